# Optimizing a Trainium2 kernel written in Bass

```python
import math
import jax, jax.numpy as jnp
from jax import lax
import numpy as np

D_MODEL = 2048
BATCH = 2
SEQ = 16384
DEPTH = 2

GRID_W = 64
CTX_LEN = 256
NORM_EPS = 1e-6
NEG_INF = -1e30

A_HEADS = 16
A_KV_HEADS = 4
A_HEAD_DIM = 64
WINDOW = 128
A_BLOCK = 128
ROPE_BASE = 10000.0

B_HEADS = 8
B_HEAD_DIM = 128
CONV_W = 5
DN_CHUNK = 64

C_HEADS = 4
C_KEY_DIM = D_MODEL // 2
C_VAL_DIM = D_MODEL
C_DK = C_KEY_DIM // C_HEADS
C_DV = C_VAL_DIM // C_HEADS
GATE_RANK = 16
GATE_NORMALIZER = 16.0
GLA_CHUNK = 64

N_EXPERTS = 32
TOP_K = 4
D_FF = D_MODEL
SWIGLU_ALPHA = 1.702
SWIGLU_LIMIT = 7.0
MOE_BLOCK = 512

A_Q = A_HEADS * A_HEAD_DIM
A_KV = A_KV_HEADS * A_HEAD_DIM
B_W = B_HEADS * B_HEAD_DIM
EVEN_IN = A_Q + 2 * A_KV + 4 * B_W + 4 * B_HEADS
EVEN_MIX = A_Q + B_W
ODD_IN = 2 * C_KEY_DIM + 2 * C_VAL_DIM + 2 * GATE_RANK
N_EVEN = (DEPTH + 1) // 2
N_ODD = DEPTH // 2

kernel_name = 'hybrid_swa_deltanet_gla_moe_dit'

f32 = jnp.float32


def rms_norm(x, g, eps=NORM_EPS):
    xf = x.astype(f32)
    y = xf * lax.rsqrt(jnp.mean(xf * xf, axis=-1, keepdims=True) + eps)
    return (y * g.astype(f32)).astype(x.dtype)


def l2norm(x, eps=1e-6):
    xf = x.astype(f32)
    return xf * lax.rsqrt(jnp.sum(xf * xf, axis=-1, keepdims=True) + eps)


def modulate(h, shift, scale):
    return h * (1.0 + scale) + shift


def split_cols(p, sizes):
    return jnp.split(p, np.cumsum(sizes)[:-1].tolist(), axis=-1)


def axial_rope_tables(rows):
    row = jnp.repeat(jnp.arange(rows, dtype=f32), GRID_W)
    col = jnp.tile(jnp.arange(GRID_W, dtype=f32), rows)
    n_freq = A_HEAD_DIM // 4
    inv = ROPE_BASE ** (-jnp.arange(n_freq, dtype=f32) / n_freq)
    ang = jnp.stack([row[:, None] * inv, col[:, None] * inv], axis=1)
    return jnp.cos(ang), jnp.sin(ang)


def apply_axial_rope(x, cos, sin):
    B, S, H, dh = x.shape
    xr = x.astype(f32).reshape(B, S, H, 2, 2, dh // 4)
    x1, x2 = xr[..., 0, :], xr[..., 1, :]
    c = cos[None, :, None]
    s = sin[None, :, None]
    out = jnp.stack([x1 * c - x2 * s, x2 * c + x1 * s], axis=-2)
    return out.reshape(B, S, H, dh).astype(x.dtype)


def centred_dwconv(x, w):
    half = CONV_W // 2
    return lax.conv_general_dilated(x, w[:, None, :].astype(x.dtype), window_strides=(1,),
                                    padding=[(half, half)], dimension_numbers=('NWC', 'WIO', 'NWC'),
                                    feature_group_count=x.shape[-1])


def window_attention(q, k, v, kc, vc, sinks):
    B, S, Hq, dh = q.shape
    Hkv = k.shape[2]
    G = Hq // Hkv
    nb = S // A_BLOCK
    L = kc.shape[1]
    nk = 3 * A_BLOCK
    scale = dh ** -0.5
    qb = q.reshape(B, nb, A_BLOCK, Hkv, G, dh)

    def band(t):
        tp = jnp.pad(t, ((0, 0), (A_BLOCK, A_BLOCK), (0, 0), (0, 0))).reshape(B, nb + 2, A_BLOCK, Hkv, dh)
        return jnp.concatenate([tp[:, :-2], tp[:, 1:-1], tp[:, 2:]], axis=2)

    kb, vb = band(k), band(v)
    s_loc = jnp.einsum('bnqhgd,bnkhd->bnhgqk', qb, kb).astype(f32) * scale
    q_pos = jnp.arange(nb)[:, None] * A_BLOCK + jnp.arange(A_BLOCK)[None, :]
    k_pos = (jnp.arange(nb)[:, None] - 1) * A_BLOCK + jnp.arange(nk)[None, :]
    kp = k_pos[:, None, :]
    allowed = (jnp.abs(kp - q_pos[:, :, None]) <= WINDOW) & (kp >= 0) & (kp < S)
    s_loc = jnp.where(allowed[None, :, None, None], s_loc, NEG_INF)
    s_ctx = jnp.einsum('bnqhgd,blhd->bnhgql', qb, kc).astype(f32) * scale
    sink = jnp.broadcast_to(sinks.reshape(Hkv, G)[None, None, :, :, None, None].astype(f32),
                            s_loc.shape[:-1] + (1,))
    p = jax.nn.softmax(jnp.concatenate([s_loc, s_ctx, sink], axis=-1), axis=-1)
    out = (jnp.einsum('bnhgqk,bnkhd->bnqhgd', p[..., :nk].astype(v.dtype), vb)
           + jnp.einsum('bnhgql,blhd->bnqhgd', p[..., nk:nk + L].astype(v.dtype), vc))
    return out.reshape(B, S, Hq * dh)


def context_attention(qc, kc, vc, sinks):
    B, L, Hq, dh = qc.shape
    Hkv = kc.shape[2]
    G = Hq // Hkv
    qg = qc.reshape(B, L, Hkv, G, dh)
    s = jnp.einsum('blhgd,bmhd->bhglm', qg, kc).astype(f32) * dh ** -0.5
    sink = jnp.broadcast_to(sinks.reshape(Hkv, G)[None, :, :, None, None].astype(f32), s.shape[:-1] + (1,))
    p = jax.nn.softmax(jnp.concatenate([s, sink], axis=-1), axis=-1)[..., :L]
    out = jnp.einsum('bhglm,bmhd->blhgd', p.astype(vc.dtype), vc)
    return out.reshape(B, L, Hq * dh)


def gated_delta_chunked(q, k, v, g, beta, s0):
    B, H, T, dk = k.shape
    dv = v.shape[-1]
    C = DN_CHUNK
    n = T // C
    q, k, v = [t.astype(f32).reshape(B, H, n, C, t.shape[-1]) for t in (q, k, v)]
    g = jnp.cumsum(g.astype(f32).reshape(B, H, n, C), axis=-1)
    beta = beta.astype(f32).reshape(B, H, n, C)
    tri_strict = jnp.tril(jnp.ones((C, C), bool), -1)
    tri_incl = jnp.tril(jnp.ones((C, C), bool))
    diff = g[..., :, None] - g[..., None, :]
    decay = jnp.where(tri_incl, jnp.exp(jnp.where(tri_incl, diff, 0.0)), 0.0)
    kb = k * beta[..., None]
    lmat = jnp.where(tri_strict, jnp.einsum('bhncd,bhnmd->bhncm', kb, k) * decay, 0.0)
    tmat = lmat + jnp.eye(C, dtype=f32)
    rhs = jnp.concatenate([v * beta[..., None], kb * jnp.exp(g)[..., None]], axis=-1)
    sol = lax.linalg.triangular_solve(tmat, rhs, left_side=True, lower=True, unit_diagonal=True)
    u, w = sol[..., :dv], sol[..., dv:]
    attn = jnp.where(tri_incl, jnp.einsum('bhncd,bhnmd->bhncm', q, k) * decay, 0.0)
    q_dec = q * jnp.exp(g)[..., None]
    k_dec = k * jnp.exp(g[..., -1:] - g)[..., None]
    g_last = jnp.exp(g[..., -1])

    def step(S, xs):
        u_i, w_i, a_i, qd_i, kd_i, gl_i = xs
        v_new = u_i - jnp.einsum('bhcd,bhde->bhce', w_i, S)
        o = jnp.einsum('bhcd,bhde->bhce', qd_i, S) + jnp.einsum('bhcm,bhme->bhce', a_i, v_new)
        S = S * gl_i[..., None, None] + jnp.einsum('bhcd,bhce->bhde', kd_i, v_new)
        return S, o

    xs = tuple(jnp.moveaxis(t, 2, 0) for t in (u, w, attn, q_dec, k_dec, g_last))
    s_fin, o = lax.scan(step, s0, xs)
    return jnp.moveaxis(o, 0, 2).reshape(B, H, T, dv), s_fin


def gla_chunked(q, k, v, gk, s0):
    B, H, T, dk = k.shape
    dv = v.shape[-1]
    C = GLA_CHUNK
    n = T // C
    q, k, v, gk = [t.astype(f32).reshape(B, H, n, C, t.shape[-1]) for t in (q, k, v, gk)]
    b = jnp.cumsum(gk, axis=3)
    b_mid = b[:, :, :, C // 2:C // 2 + 1]
    tri = jnp.tril(jnp.ones((C, C), bool))
    attn = jnp.where(tri, jnp.einsum('bhncd,bhnmd->bhncm', q * jnp.exp(b - b_mid), k * jnp.exp(b_mid - b)), 0.0)
    o_intra = jnp.einsum('bhncm,bhnme->bhnce', attn, v)
    b_last = b[:, :, :, -1:]
    q_dec = q * jnp.exp(b)
    k_dec = k * jnp.exp(b_last - b)
    dec = jnp.exp(b_last[:, :, :, 0])

    def step(S, xs):
        qd, kd, vv, dc = xs
        o = jnp.einsum('bhcd,bhde->bhce', qd, S)
        S = S * dc[..., None] + jnp.einsum('bhcd,bhce->bhde', kd, vv)
        return S, o

    xs = tuple(jnp.moveaxis(t, 2, 0) for t in (q_dec, k_dec, v, dec))
    s_fin, o_inter = lax.scan(step, s0, xs)
    o = jnp.moveaxis(o_inter, 0, 2) + o_intra
    return o.reshape(B, H, T, dv), s_fin


def bidirectional_prefix_scan(scan_fn, shared_lat, gates_lat, shared_ctx, gates_ctx, state_shape):
    B, H = shared_lat[0].shape[:2]
    s0 = jnp.zeros((B, H) + state_shape, f32)
    outs_l, outs_c = [], []
    for d in range(2):
        flip = (lambda t: jnp.flip(t, axis=2)) if d == 1 else (lambda t: t)
        o_c, s_c = scan_fn(*[flip(t) for t in shared_ctx + gates_ctx[d]], s0)
        o_l, _ = scan_fn(*[flip(t) for t in shared_lat + gates_lat[d]], s_c)
        outs_c.append(flip(o_c))
        outs_l.append(flip(o_l))
    return outs_l[0] + outs_l[1], outs_c[0] + outs_c[1]


def even_mixer(h_lat, h_ctx, w_in, w_out, q_gain, k_gain, sinks, conv_w, a_log, dt_bias, dn_norm, cos, sin, need_ctx):
    B = h_lat.shape[0]
    sizes = [A_Q, A_KV, A_KV, 3 * B_W, B_W, B_HEADS, B_HEADS, B_HEADS, B_HEADS]
    lat = split_cols(h_lat @ w_in, sizes)
    cx = split_cols(h_ctx @ w_in, sizes)

    def a_heads(qa, ka, va):
        T = qa.shape[1]
        return (rms_norm(qa.reshape(B, T, A_HEADS, A_HEAD_DIM), q_gain),
                rms_norm(ka.reshape(B, T, A_KV_HEADS, A_HEAD_DIM), k_gain),
                va.reshape(B, T, A_KV_HEADS, A_HEAD_DIM))

    q_l, k_l, v_l = a_heads(*lat[:3])
    q_l = apply_axial_rope(q_l, cos, sin)
    k_l = apply_axial_rope(k_l, cos, sin)
    q_c, k_c, v_c = a_heads(*cx[:3])
    a_lat = window_attention(q_l, k_l, v_l, k_c, v_c, sinks)

    def b_heads(qkv, a_f, a_b, b_f, b_b):
        T = qkv.shape[1]
        qkv = jax.nn.silu(centred_dwconv(qkv, conv_w))
        qb, kb, vb = [t.reshape(B, T, B_HEADS, B_HEAD_DIM).transpose(0, 2, 1, 3) for t in jnp.split(qkv, 3, axis=-1)]
        qb = l2norm(qb) * (B_HEAD_DIM ** -0.5)
        kb = l2norm(kb)
        gates = []
        for d, (a_d, b_d) in enumerate(((a_f, b_f), (a_b, b_b))):
            g = -jnp.exp(a_log[d].astype(f32)) * jax.nn.softplus(a_d.astype(f32) + dt_bias[d].astype(f32))
            beta = jax.nn.sigmoid(b_d.astype(f32))
            gates.append((g.transpose(0, 2, 1), beta.transpose(0, 2, 1)))
        return (qb, kb, vb), gates

    sh_l, gt_l = b_heads(lat[3], *lat[5:])
    sh_c, gt_c = b_heads(cx[3], *cx[5:])
    o_l, o_c = bidirectional_prefix_scan(gated_delta_chunked, sh_l, gt_l, sh_c, gt_c, (B_HEAD_DIM, B_HEAD_DIM))

    def b_out(o, z):
        T = z.shape[1]
        o = rms_norm(o.transpose(0, 2, 1, 3).astype(z.dtype), dn_norm)
        return (o * jax.nn.silu(z.reshape(B, T, B_HEADS, B_HEAD_DIM))).reshape(B, T, B_W)

    y_lat = jnp.concatenate([a_lat, b_out(o_l, lat[4])], axis=-1) @ w_out
    y_ctx = None
    if need_ctx:
        a_ctx = context_attention(q_c, k_c, v_c, sinks)
        y_ctx = jnp.concatenate([a_ctx, b_out(o_c, cx[4])], axis=-1) @ w_out
    return y_lat, y_ctx


def odd_mixer(h_lat, h_ctx, w_in, gate_w2, gate_b, gla_norm, w_out, need_ctx):
    B = h_lat.shape[0]
    sizes = [C_KEY_DIM, C_KEY_DIM, C_VAL_DIM, C_VAL_DIM, GATE_RANK, GATE_RANK]

    def heads(p):
        q, k, v, g_out, r_f, r_b = split_cols(p, sizes)
        T = q.shape[1]
        hd = lambda t, dd: t.reshape(B, T, C_HEADS, dd).transpose(0, 2, 1, 3)
        gates = []
        for d, r in enumerate((r_f, r_b)):
            gk = jax.nn.log_sigmoid((r @ gate_w2[d] + gate_b[d]).astype(f32)) / GATE_NORMALIZER
            gates.append((hd(gk, C_DK),))
        return (hd(q, C_DK) * (C_DK ** -0.5), hd(k, C_DK), hd(v, C_DV)), gates, g_out

    sh_l, gt_l, go_l = heads(h_lat @ w_in)
    sh_c, gt_c, go_c = heads(h_ctx @ w_in)
    o_l, o_c = bidirectional_prefix_scan(gla_chunked, sh_l, gt_l, sh_c, gt_c, (C_DK, C_DV))

    def out(o, g_out):
        T = g_out.shape[1]
        o = rms_norm(o.transpose(0, 2, 1, 3).astype(g_out.dtype), gla_norm)
        return (o * jax.nn.silu(g_out.reshape(B, T, C_HEADS, C_DV))).reshape(B, T, C_VAL_DIM) @ w_out

    return out(o_l, go_l), (out(o_c, go_c) if need_ctx else None)


def moe_ffn(h, w_router, b_router, w_gu, b_gu, w_down, b_down):
    N, D = h.shape
    logits = (h @ w_router + b_router).astype(f32)
    top_val, top_idx = lax.top_k(logits, TOP_K)
    gate = jax.nn.softmax(top_val, axis=-1)
    e_flat = top_idx.reshape(-1)
    tok_flat = jnp.repeat(jnp.arange(N, dtype=jnp.int32), TOP_K)
    order = jnp.argsort(e_flat)
    e_sorted = e_flat[order]
    counts = jnp.bincount(e_flat, length=N_EXPERTS)
    padded = (counts + MOE_BLOCK - 1) // MOE_BLOCK * MOE_BLOCK
    start = jnp.cumsum(counts) - counts
    pad_end = jnp.cumsum(padded)
    pad_start = pad_end - padded
    dest = pad_start[e_sorted] + jnp.arange(N * TOP_K) - start[e_sorted]
    n_blocks = -(-(N * TOP_K) // MOE_BLOCK) + N_EXPERTS
    P = n_blocks * MOE_BLOCK
    slot_tok = jnp.full((P,), N, jnp.int32).at[dest].set(tok_flat[order])
    slot_w = jnp.zeros((P,), h.dtype).at[dest].set(gate.reshape(-1)[order].astype(h.dtype))
    block_expert = jnp.minimum(jnp.searchsorted(pad_end, jnp.arange(n_blocks) * MOE_BLOCK, side='right'),
                               N_EXPERTS - 1)
    h_pad = jnp.concatenate([h, jnp.zeros((1, D), h.dtype)], axis=0)

    def expert_block(acc, blk):
        tok, wt, e = blk
        gu = h_pad[tok] @ w_gu[e] + b_gu[e]
        glu, lin = jnp.split(gu, 2, axis=-1)
        glu = jnp.minimum(glu, SWIGLU_LIMIT)
        lin = jnp.clip(lin, -SWIGLU_LIMIT, SWIGLU_LIMIT)
        act = glu * jax.nn.sigmoid(SWIGLU_ALPHA * glu) * (lin + 1.0)
        y = (act @ w_down[e] + b_down[e]) * wt[:, None]
        return acc.at[tok].add(y.astype(acc.dtype)), None

    out, _ = lax.scan(expert_block, jnp.zeros((N + 1, D), h.dtype),
                      (slot_tok.reshape(n_blocks, MOE_BLOCK), slot_w.reshape(n_blocks, MOE_BLOCK), block_expert))
    return out[:N]


def setup_inputs(seed: int = 0) -> dict:
    key = jax.random.key(seed)
    ks = iter(jax.random.split(key, 40))
    D = D_MODEL

    def nrm(shape, scale):
        return jax.random.normal(next(ks), shape, jnp.float32) * scale

    dt = jnp.exp(jax.random.uniform(next(ks), (N_EVEN, 2, B_HEADS), jnp.float32,
                                    minval=math.log(1e-3), maxval=math.log(1e-1)))
    a_log = jnp.log(jax.random.uniform(next(ks), (N_EVEN, 2, B_HEADS), jnp.float32, minval=1.0, maxval=16.0))
    return {
        'x': nrm((BATCH, SEQ, D), 1.0),
        'c': nrm((BATCH, D), 1.0),
        'ctx': nrm((BATCH, CTX_LEN, D), 1.0),
        'c_ctx': nrm((D,), 1.0),
        'w_mod': nrm((DEPTH, D, 6 * D), 0.5 * D ** -0.5),
        'b_mod': nrm((DEPTH, 6 * D), 0.02),
        'norm_g': 1.0 + nrm((DEPTH, 2, D), 0.05),
        'e_w_in': nrm((N_EVEN, D, EVEN_IN), D ** -0.5),
        'e_w_out': nrm((N_EVEN, EVEN_MIX, D), EVEN_MIX ** -0.5),
        'e_q_gain': 1.0 + nrm((N_EVEN, A_HEAD_DIM), 0.05),
        'e_k_gain': 1.0 + nrm((N_EVEN, A_HEAD_DIM), 0.05),
        'e_sinks': nrm((N_EVEN, A_HEADS), 0.5),
        'e_conv_w': nrm((N_EVEN, CONV_W, 3 * B_W), CONV_W ** -0.5),
        'e_a_log': a_log,
        'e_dt_bias': dt + jnp.log(-jnp.expm1(-dt)),
        'e_dn_norm': 1.0 + nrm((N_EVEN, B_HEAD_DIM), 0.05),
        'o_w_in': nrm((N_ODD, D, ODD_IN), D ** -0.5),
        'o_gate_w2': nrm((N_ODD, 2, GATE_RANK, C_KEY_DIM), GATE_RANK ** -0.5),
        'o_gate_b': nrm((N_ODD, 2, C_KEY_DIM), 0.1),
        'o_gla_norm': 1.0 + nrm((N_ODD, C_DV), 0.05),
        'o_w_out': nrm((N_ODD, C_VAL_DIM, D), C_VAL_DIM ** -0.5),
        'w_router': nrm((DEPTH, D, N_EXPERTS), D ** -0.5),
        'b_router': nrm((DEPTH, N_EXPERTS), 0.01),
        'w_gu': nrm((DEPTH, N_EXPERTS, D, 2 * D_FF), D ** -0.5),
        'b_gu': nrm((DEPTH, N_EXPERTS, 2 * D_FF), 0.02),
        'w_down': nrm((DEPTH, N_EXPERTS, D_FF, D), D_FF ** -0.5),
        'b_down': nrm((DEPTH, N_EXPERTS, D), 0.02),
    }


def reference(x, c, ctx, c_ctx, w_mod, b_mod, norm_g, e_w_in, e_w_out, e_q_gain, e_k_gain, e_sinks, e_conv_w,
              e_a_log, e_dt_bias, e_dn_norm, o_w_in, o_gate_w2, o_gate_b, o_gla_norm, o_w_out,
              w_router, b_router, w_gu, b_gu, w_down, b_down):
    B, S, D = x.shape
    rows = S // GRID_W
    cos, sin = axial_rope_tables(rows)
    sc = jax.nn.silu(c)
    scc = jax.nn.silu(c_ctx)
    for layer in range(DEPTH):
        need_ctx = layer < DEPTH - 1
        mod = jnp.split((sc @ w_mod[layer] + b_mod[layer])[:, None, :], 6, axis=-1)
        mod_c = jnp.split(scc @ w_mod[layer] + b_mod[layer], 6, axis=-1)
        h_lat = modulate(rms_norm(x, norm_g[layer, 0]), mod[0], mod[1])
        h_ctx = modulate(rms_norm(ctx, norm_g[layer, 0]), mod_c[0], mod_c[1])
        i = layer // 2
        if layer % 2 == 0:
            y_lat, y_ctx = even_mixer(h_lat, h_ctx, e_w_in[i], e_w_out[i], e_q_gain[i], e_k_gain[i], e_sinks[i],
                                      e_conv_w[i], e_a_log[i], e_dt_bias[i], e_dn_norm[i], cos, sin, need_ctx)
        else:
            y_lat, y_ctx = odd_mixer(h_lat, h_ctx, o_w_in[i], o_gate_w2[i], o_gate_b[i], o_gla_norm[i],
                                     o_w_out[i], need_ctx)
        x = x + mod[2] * y_lat
        h_lat = modulate(rms_norm(x, norm_g[layer, 1]), mod[3], mod[4])
        moe_w = (w_router[layer], b_router[layer], w_gu[layer], b_gu[layer], w_down[layer], b_down[layer])
        if need_ctx:
            ctx = ctx + mod_c[2] * y_ctx
            h_ctx = modulate(rms_norm(ctx, norm_g[layer, 1]), mod_c[3], mod_c[4])
            tokens = jnp.concatenate([h_lat.reshape(-1, D), h_ctx.reshape(-1, D)], axis=0)
            f = moe_ffn(tokens, *moe_w)
            x = x + mod[5] * f[:B * S].reshape(B, S, D)
            ctx = ctx + mod_c[5] * f[B * S:].reshape(ctx.shape)
        else:
            x = x + mod[5] * moe_ffn(h_lat.reshape(-1, D), *moe_w).reshape(B, S, D)
    return x
```

```python
import os
import numpy as np
import ml_dtypes
from contextlib import ExitStack
import concourse.bass as bass
import concourse.mybir as mybir
from concourse.bass_utils import run_bass_kernel_spmd

F32 = mybir.dt.float32
BF16 = mybir.dt.bfloat16
I32 = mybir.dt.int32
U32 = mybir.dt.uint32
AF = mybir.ActivationFunctionType
ALU = mybir.AluOpType
AX = mybir.AxisListType
NPBF = ml_dtypes.bfloat16

SEM_ROT = 20000


class _Op:
    __slots__ = ("stream", "fn", "r", "w", "dma", "deps", "sig", "need")

    def __init__(self, stream, fn, r, w, dma):
        self.stream = stream
        self.fn = fn
        self.r = r
        self.w = w
        self.dma = dma
        self.deps = None
        self.sig = None
        self.need = False


class Prog:
    def __init__(self):
        self.nc = bass.Bass("TRN2", target_bir_lowering=False)
        self.es = ExitStack()
        self.ops = []
        self.out_dma = []
        self.keymap = {}

    def din(self, name, shape, dt):
        return self.nc.dram_tensor(name, list(shape), dt, kind="ExternalInput").ap()

    def dout(self, name, shape, dt):
        return self.nc.dram_tensor(name, list(shape), dt, kind="ExternalOutput").ap()

    def dtmp(self, name, shape, dt):
        return self.nc.dram_tensor(name, list(shape), dt, kind="Internal").ap()

    def sb(self, name, shape, dt):
        return self.es.enter_context(self.nc.sbuf_tensor(name, list(shape), dt))

    def ps(self, name, shape, dt):
        return self.es.enter_context(self.nc.psum_tensor(name, list(shape), dt))

    def _km(self, keys):
        out = []
        for k in keys:
            k = self.keymap.get(k, k)
            if k not in out:
                out.append(k)
        return tuple(out)

    def op(self, stream, fn, r=(), w=()):
        self.ops.append(_Op(stream, fn, self._km(r), self._km(w), None))

    def dma(self, stream, out, in_, r=(), w=(), key=None, **kw):
        assert key is not None
        self.ops.append(_Op(stream, lambda e: e.dma_start(out=out, in_=in_, **kw), self._km(r), self._km(w), key))

    def dmaf(self, stream, fn, r=(), w=(), key=None):
        assert key is not None
        self.ops.append(_Op(stream, fn, self._km(r), self._km(w), key))

    def mm(self, out, lhsT, rhs, start=True, stop=True, r=(), w=(), **kw):
        self.op("pe", lambda e: e.matmul(out, lhsT, rhs, start=start, stop=stop, **kw), r, w)

    def tr(self, out, in_, ident, r=(), w=()):
        self.op("pe", lambda e: e.transpose(out, in_, ident), r, w)

    def act(self, out, in_, func, r=(), w=(), stream="act", **kw):
        self.op(stream, lambda e: e.activation(out=out, in_=in_, func=func, **kw), r, w)

    def tt(self, out, in0, in1, op, r=(), w=(), stream="dve"):
        self.op(stream, lambda e: e.tensor_tensor(out, in0, in1, op), r, w)

    def ts(self, out, in0, s1, s2, op0, op1=None, r=(), w=(), stream="dve", **kw):
        if op1 is None:
            self.op(stream, lambda e: e.tensor_scalar(out, in0, s1, None, op0, **kw), r, w)
        else:
            self.op(stream, lambda e: e.tensor_scalar(out, in0, s1, s2, op0, op1, **kw), r, w)

    def stt(self, out, in0, scalar, in1, op0, op1, r=(), w=(), stream="dve", **kw):
        self.op(stream, lambda e: e.scalar_tensor_tensor(out, in0, scalar, in1, op0, op1, **kw), r, w)

    def cp(self, out, in_, r=(), w=(), stream="dve"):
        if stream == "act":
            self.op(stream, lambda e: e.copy(out, in_), r, w)
        else:
            self.op(stream, lambda e: e.tensor_copy(out, in_), r, w)

    def memset(self, ap, val, w=(), stream="dve"):
        self.op(stream, lambda e: e.memset(ap, val), (), w)

    def finalize(self):
        nc = self.nc
        ops = self.ops
        state = {}

        def dom(o):
            return ("dma", o.dma) if o.dma is not None else o.stream

        for i, o in enumerate(ops):
            deps = set()
            d_i = dom(o)
            for k in o.r:
                st = state.setdefault(k, ({}, {}))
                deps.update(st[0].values())
                st[1][d_i] = i
            for k in o.w:
                st = state.setdefault(k, ({}, {}))
                wr, rd = st
                others_r = {d: j for d, j in rd.items() if j != i}
                if others_r:
                    for d, j in others_r.items():
                        deps.add(j)
                    for d, j in wr.items():
                        if d != d_i or (o.dma is None and d_i != "pe"):
                            deps.add(j)
                    wr.clear()
                    rd.clear()
                    wr[d_i] = i
                else:
                    for d, j in wr.items():
                        if d != d_i or (o.dma is None and d_i != "pe"):
                            deps.add(j)
                    rd.clear()
                    wr[d_i] = i
            deps.discard(i)
            o.deps = deps
            for j in deps:
                ops[j].need = True
        eng_cnt = {}
        sem_of = {}

        def get_sem(key):
            if key not in sem_of:
                sem_of[key] = self.es.enter_context(nc.semaphore("s%d" % len(sem_of)))
            return sem_of[key]

        dma_cum = {}
        for o in ops:
            if o.dma is not None:
                dma_cum[o.dma] = dma_cum.get(o.dma, 0) + 16
                o.sig = (("dma", o.dma), dma_cum[o.dma])
            elif o.need:
                c = eng_cnt.get(o.stream, 0) + 1
                eng_cnt[o.stream] = c
                o.sig = ((o.stream, (c - 1) // SEM_ROT), (c - 1) % SEM_ROT + 1)
        streams = {}
        for i, o in enumerate(ops):
            streams.setdefault(o.stream, []).append(i)
        waits = [None] * len(ops)
        waited = {}
        for i, o in enumerate(ops):
            wl = {}
            for j in o.deps:
                sk, val = ops[j].sig
                if wl.get(sk, 0) < val:
                    wl[sk] = val
            ws = waited.setdefault(o.stream, {})
            out = []
            for sk, val in wl.items():
                if ws.get(sk, 0) < val:
                    ws[sk] = val
                    out.append((sk, val))
            waits[i] = out
        for sk in set(s for w in waits for s, _ in w):
            get_sem(sk)
        for o in ops:
            if o.sig is not None:
                get_sem(o.sig[0])
        final = [(("dma", k), dma_cum[k]) for k in self.out_dma if k in dma_cum]

        def emit(stream, e):
            for i in streams.get(stream, []):
                o = ops[i]
                for sk, val in waits[i]:
                    e.wait_ge(sem_of[sk], val)
                ins = o.fn(e)
                if o.sig is not None:
                    sk, _ = o.sig
                    ins.then_inc(sem_of[sk], 16 if o.dma is not None else 1)
            if stream == "sp":
                for sk, val in final:
                    e.wait_ge(sem_of[sk], val)

        with nc.Block() as block:
            @block.sync
            def _(e):
                emit("sp", e)

            @block.tensor
            def _(e):
                emit("pe", e)

            @block.vector
            def _(e):
                emit("dve", e)

            @block.scalar
            def _(e):
                emit("act", e)

            @block.gpsimd
            def _(e):
                emit("pool", e)
        self.es.close()
        self.n_sems = len(sem_of)
        return nc


D = 2048
KD = 16
EPS = 1e-6


def build_mod():
    P = Prog()
    cT = P.din("cT", [128, KD * 3], F32)
    wm = P.din("wm", [2, D, 1536], F32)
    bm = P.din("bm", [1, 2 * 1536], F32)
    out = P.dout("modo", [2, 3, 1536], F32)
    sc = P.sb("sc", [128, KD, 3], F32)
    wms = P.sb("wms", [128, KD, 1536], F32)
    bms = P.sb("bms", [1, 2, 1536], F32)
    ones3 = P.sb("ones3", [1, 3], F32)
    res = P.sb("res", [3, 1536], F32)
    ps = P.ps("pm", [128, 512], F32)
    P.dma("sp", sc[:].rearrange("p k r -> p (k r)"), cT, w=["sc"], key="sc")
    P.act(sc[:].rearrange("p k r -> p (k r)"), sc[:].rearrange("p k r -> p (k r)"), AF.Silu, r=["sc"], w=["sc"])
    P.dma("sp", bms[:].rearrange("o l c -> o (l c)"), bm, w=["bms"], key="bms")
    P.memset(ones3[:], 1.0, w=["ones3"])
    for l in range(2):
        for k in range(KD):
            P.dma("sp", wms[:, k, :], wm[l, k * 128:(k + 1) * 128, :], w=["wms"], key="wms")
        for cc in range(3):
            cs = slice(cc * 512, (cc + 1) * 512)
            for k in range(KD):
                P.mm(ps[0:3, :], sc[:, k, :], wms[:, k, cs], start=(k == 0), stop=False, r=["sc", "wms"], w=["pm"])
            P.mm(ps[0:3, :], ones3[:, :], bms[:, l, cs], start=False, stop=True, r=["ones3", "bms"], w=["pm"])
            P.cp(res[:, cs], ps[0:3, :], r=["pm"], w=["res"])
        P.dma("sp", out[l], res[:], r=["res"], key="modo")
    P.out_dma.append("modo")
    return P.finalize()


def build_pre(NT, ctx_rows):
    P = Prog()
    R = NT * 128
    xin = P.din("xin", [R, D], F32)
    g = P.din("g", [D], F32)
    modn = P.din("modn", [2, 2, D], F32)
    hout = P.dout("hout", [R, D], BF16)
    bcB = P.sb("bcB", [128, D], F32)
    bcC = P.sb("bcC", [128, D], F32)
    xt = P.sb("xt", [128, D], F32)
    tmp = P.sb("tmp", [128, D], F32)
    hn = P.sb("hn", [128, D], BF16)
    sm = P.sb("sm", [128, 2], F32)

    def set_mod(kind):
        P.dma("sp", bcB[:], modn[kind, 1, :].partition_broadcast(128), w=["bcB"], key="bcB")
        P.dma("sp", bcC[:], g.partition_broadcast(128), w=["bcC"], key="bcC")
        P.stt(bcB[:], bcB[:], 1.0, bcC[:], ALU.add, ALU.mult, r=["bcB", "bcC"], w=["bcB"])
        P.dma("sp", bcC[:], modn[kind, 0, :].partition_broadcast(128), w=["bcC"], key="bcC")

    set_mod(0)
    for t in range(NT):
        if ctx_rows > 0 and t == NT - 1:
            set_mod(1)
        rows = slice(t * 128, (t + 1) * 128)
        P.dma("sp", xt[:], xin[rows, :], w=["xt"], key="xt")
        P.memset(sm[:, 0:1], 0.0, w=["sm"])
        P.act(tmp[:], xt[:], AF.Square, r=["xt"], w=["tmp", "sm"], accum_out=sm[:, 0:1])
        P.ts(sm[:, 0:1], sm[:, 0:1], 1.0 / D, EPS, ALU.mult, ALU.add, r=["sm"], w=["sm"])
        P.act(sm[:, 0:1], sm[:, 0:1], AF.Sqrt, r=["sm"], w=["sm"])
        P.op("dve", lambda e: e.reciprocal(sm[:, 0:1], sm[:, 0:1]), r=["sm"], w=["sm"])
        P.stt(tmp[:], xt[:], sm[:, 0:1], bcB[:], ALU.mult, ALU.mult, r=["xt", "sm", "bcB"], w=["tmp"])
        P.tt(hn[:], tmp[:], bcC[:], ALU.add, r=["tmp", "bcC"], w=["hn"])
        P.dma("sp", hout[rows, :], hn[:], r=["hn"], key="hout")
    P.out_dma.append("hout")
    return P.finalize()


D = 2048
KD = 16
NE = 32
EPS = 1e-6


def bc_row(ap_row, n):
    return ap_row.partition_broadcast(128)


def build_post(NT, DFF, last, ctx_rows):
    P = Prog()
    R = NT * 128
    KF = DFF // 128
    NCG = DFF // 256
    xin = P.din("xin", [R, D], F32)
    mix = P.din("mix", [R, D], BF16)
    modv = P.din("modv", [2, 6, D], F32)
    g2 = P.din("g2", [D], F32)
    wout = P.din("wout", [D, D], F32)
    wr = P.din("wr", [D, NE], F32)
    br = P.din("br", [1, NE], F32)
    wgu = P.din("wgu", [NE, D, 2 * DFF], F32)
    bgu = P.din("bgu", [NE, 2 * DFF], F32)
    wdn = P.din("wdn", [NE, DFF, D], F32)
    bdn = P.din("bdn", [NE, D], F32)
    identb_d = P.din("identb", [128, 128], BF16)
    identf_d = P.din("identf", [128, 128], F32)
    if not last:
        gn = P.din("gn", [D], F32)
        modn = P.din("modn", [2, 2, D], F32)
        hnext = P.dout("hnext", [R, D], BF16)
    xout = P.dout("xout", [R, D], F32)
    wo_s = P.dtmp("wo_s", [4, 128, KD * 512], BF16)
    wg_l = [P.dtmp("wg_s%d" % e, [NCG, 128, KD * 512], BF16) for e in range(NE)]
    wd_l = [P.dtmp("wd_s%d" % e, [4, 128, KF * 512], BF16) for e in range(NE)]
    x1_s = P.dtmp("x1_s", [R, D], F32)
    h2_s = P.dtmp("h2_s", [R, D], BF16)
    wbuf = P.sb("wbuf", [128, 2, KD * 512], BF16)
    BA = P.sb("BA", [128, 32768], BF16)
    FA = P.sb("FA", [128, 8192], F32)
    bcA = P.sb("bcA", [128, D], F32)
    bcB = P.sb("bcB", [128, D], F32)
    bcC = P.sb("bcC", [128, D], F32)
    xt3 = P.sb("xt3", [128, D], F32)
    hn = P.sb("hn", [128, D], BF16)
    Bg = P.sb("Bg", [NE, 2 * DFF], BF16)
    Bd = P.sb("Bd", [NE, D], F32)
    G = P.sb("G", [128, NT, NE], F32)
    GT = P.sb("GT", [NE, 512], F32)
    GTb = P.sb("GTb", [NE, 512], BF16)
    identb = P.sb("identb_s", [128, 128], BF16)
    identf = P.sb("identf_s", [128, 128], F32)
    wrs = P.sb("wrs", [128, KD, NE], F32)
    brs = P.sb("brs", [1, NE], F32)
    ones1 = P.sb("ones1", [1, 128], F32)
    sm = P.sb("sm", [128, 16], F32)
    mx8 = P.sb("mx8", [128, 8], F32)
    lg = P.sb("lg", [128, NE], F32)
    msk = P.sb("msk", [128, NE], F32)
    glu = P.sb("glu", [128, 256], F32)
    sig = P.sb("sig", [128, 256], F32)
    lin = P.sb("lin", [128, 256], F32)
    ptb = [P.ps("ptb%d" % i, [128, 1024], BF16) for i in range(2)]
    pf = [P.ps("pf%d" % i, [128, 512], F32) for i in range(2)]
    pg = [P.ps("pg%d" % i, [128, 512], F32) for i in range(2)]
    py = [P.ps("py%d" % i, [128, 512], F32) for i in range(2)]

    mixt = BA[:, 0:2048]
    mixT = BA[:, 2048:4096]
    h2b = BA[:, 4096:6144]
    xt = FA[:, 0:2048]
    h2 = FA[:, 2048:4096]
    h2T32 = FA[:, 4096:6144]
    P1KEYS = ["mixt", "mixT", "h2b", "xt", "h2", "h2T32"]

    P.dma("sp", identb[:], identb_d, w=["identb"], key="identb")
    P.dma("sp", identf[:], identf_d, w=["identf"], key="identf")
    P.dma("sp", wrs[:], wr.rearrange("(k p) e -> p k e", p=128), w=["wrs"], key="wrs")
    P.dma("sp", brs[:], br, w=["brs"], key="brs")
    P.dma("sp", Bd[:], bdn, w=["Bd"], key="Bd")
    P.dma("pool", Bg[:], bgu, w=["Bg"], key="Bg")
    P.memset(ones1[:], 1.0, w=["ones1"])
    P.memset(G[:], 0.0, w=["G"])

    stg = 0

    def cast_piece(dst, srcs):
        nonlocal stg
        s = stg % 2
        stg += 1
        key = "wp%d" % s
        for (lo, hi, src) in srcs:
            P.dma("pool", wbuf[:, s, :].rearrange("p (k c) -> p k c", c=512)[:, :src.shape[1], lo:hi], src,
                  w=[key], key=key + "l")
        P.dma("sp", dst, wbuf[:, s, 0:dst.shape[1]], r=[key], w=["wscr"], key=key + "s")

    for dc in range(4):
        cast_piece(wo_s[dc], [(0, 512, wout[:, dc * 512:(dc + 1) * 512].rearrange("(k p) c -> p k c", p=128))])
    for e in range(NE):
        for cg in range(NCG):
            cast_piece(wg_l[e][cg], [
                (0, 256, wgu[e, :, cg * 256:(cg + 1) * 256].rearrange("(k p) c -> p k c", p=128)),
                (256, 512, wgu[e, :, DFF + cg * 256:DFF + (cg + 1) * 256].rearrange("(k p) c -> p k c", p=128))])
        for dc in range(4):
            cast_piece(wd_l[e][dc], [(0, 512, wdn[e, :, dc * 512:(dc + 1) * 512].rearrange("(k p) c -> p k c", p=128))])

    wslot = [0]

    def load_piece(src, extra_w=()):
        s = wslot[0] % 2
        wslot[0] += 1
        key = "wp%d" % s
        P.dma("sp", wbuf[:, s, 0:src.shape[1]], src, r=["wscr"], w=[key] + list(extra_w), key=key + "l")
        return s, key

    def load_bc(tile, row_ap, key):
        P.dma("sp", tile[:], row_ap.partition_broadcast(128), w=[key], key=key)

    def set_mod(kind):
        load_bc(bcA, modv[kind, 2, :], "bcA")
        load_bc(bcB, modv[kind, 4, :], "bcB")
        load_bc(bcC, g2, "bcC")
        P.stt(bcB[:], bcB[:], 1.0, bcC[:], ALU.add, ALU.mult, r=["bcB", "bcC"], w=["bcB"])
        load_bc(bcC, modv[kind, 3, :], "bcC")

    def rstd_from(src, key_src, col):
        P.memset(sm[:, col:col + 1], 0.0, w=["sm%d" % col])
        P.act(h2[:], src, AF.Square, r=[key_src], w=["h2", "sm%d" % col], accum_out=sm[:, col:col + 1])
        P.ts(sm[:, col:col + 1], sm[:, col:col + 1], 1.0 / D, EPS, ALU.mult, ALU.add, r=["sm%d" % col], w=["sm%d" % col])
        P.act(sm[:, col:col + 1], sm[:, col:col + 1], AF.Sqrt, r=["sm%d" % col], w=["sm%d" % col])
        P.op("dve", lambda e: e.reciprocal(sm[:, col:col + 1], sm[:, col:col + 1]), r=["sm%d" % col], w=["sm%d" % col])

    set_mod(0)
    for t in range(NT):
        is_ctx = ctx_rows > 0 and t == NT - 1
        if is_ctx:
            set_mod(1)
        rows = slice(t * 128, (t + 1) * 128)
        P.dma("sp", mixt, mix[rows, :], w=["mixt"], key="mixt")
        P.dma("sp", xt, xin[rows, :], w=["xt"], key="xt")
        for j in range(2):
            for i in range(8):
                k = j * 8 + i
                P.tr(ptb[j][:, i * 128:(i + 1) * 128], mixt[:, k * 128:(k + 1) * 128], identb[:],
                     r=["mixt", "identb"], w=["ptb%d" % j])
            P.cp(mixT[:, j * 1024:(j + 1) * 1024], ptb[j][:], r=["ptb%d" % j], w=["mixT"], stream="act" if j else "dve")
        for dc in range(4):
            s, key = load_piece(wo_s[dc])
            wv = wbuf[:, s, :].rearrange("p (k c) -> p k c", c=512)
            ps = pf[dc % 2]
            pk = "pf%d" % (dc % 2)
            for k in range(KD):
                P.mm(ps[:], mixT[:, k * 128:(k + 1) * 128], wv[:, k, :], start=(k == 0), stop=(k == KD - 1),
                     r=["mixT", key], w=[pk])
            cs = slice(dc * 512, (dc + 1) * 512)
            P.tt(h2[:, cs], ps[:], bcA[:, cs], ALU.mult, r=[pk, "bcA"], w=["h2"])
            P.tt(xt[:, cs], h2[:, cs], xt[:, cs], ALU.add, r=["h2", "xt"], w=["xt"])
        P.dma("sp", x1_s[rows, :], xt, r=["xt"], w=["x1_s"], key="xt_st")
        rstd_from(xt, "xt", 0)
        P.stt(h2[:], xt, sm[:, 0:1], bcB[:], ALU.mult, ALU.mult, r=["xt", "sm0", "bcB"], w=["h2"])
        P.tt(h2[:], h2[:], bcC[:], ALU.add, r=["h2", "bcC"], w=["h2"])
        P.cp(h2b, h2[:], r=["h2"], w=["h2b"], stream="act")
        P.dma("sp", h2_s[rows, :], h2b, r=["h2b"], w=["h2_s"], key="h2b_st")
        if last and False:
            pass
        for j in range(4):
            for i in range(4):
                k = j * 4 + i
                P.tr(pf[j % 2][:, i * 128:(i + 1) * 128], h2[:, k * 128:(k + 1) * 128], identf[:],
                     r=["h2", "identf"], w=["pf%d" % (j % 2)])
            P.cp(h2T32[:, j * 512:(j + 1) * 512], pf[j % 2][:], r=["pf%d" % (j % 2)], w=["h2T32"],
                 stream="act" if j % 2 else "dve")
        for k in range(KD):
            P.mm(pg[0][:, 0:NE], h2T32[:, k * 128:(k + 1) * 128], wrs[:, k, :], start=(k == 0), stop=False,
                 r=["h2T32", "wrs"], w=["pg0"])
        P.mm(pg[0][:, 0:NE], ones1[:, :], brs[:, :], start=False, stop=True, r=["ones1", "brs"], w=["pg0"])
        P.cp(lg[:], pg[0][:, 0:NE], r=["pg0"], w=["lg"])
        P.op("dve", lambda e: e.max(mx8[:], lg[:]), r=["lg"], w=["mx8"])
        P.ts(msk[:], lg[:], mx8[:, 3:4], None, ALU.is_ge, r=["lg", "mx8"], w=["msk"])
        P.ts(sm[:, 2:3], mx8[:, 0:1], -1.0, None, ALU.mult, r=["mx8"], w=["sm2"])
        P.act(lg[:], lg[:], AF.Exp, r=["lg", "sm2"], w=["lg"], bias=sm[:, 2:3], scale=1.0)
        P.tt(lg[:], lg[:], msk[:], ALU.mult, r=["lg", "msk"], w=["lg"])
        P.op("dve", lambda e: e.reduce_sum(sm[:, 3:4], lg[:], AX.X), r=["lg"], w=["sm3"])
        P.op("dve", lambda e: e.reciprocal(sm[:, 3:4], sm[:, 3:4]), r=["sm3"], w=["sm3"])
        nv = ctx_rows if is_ctx else 128
        P.ts(G[0:nv, t, :], lg[0:nv, :], sm[0:nv, 3:4], None, ALU.mult, r=["lg", "sm3"], w=["G"])

    h2g = BA[:, 0:8192]
    h2T = BA[:, 8192:16384]
    actb = BA[:, 16384:24576]
    actT = BA[:, 24576:32768]
    acc = FA[:, 0:8192]
    first = True
    ngroups = (NT + 3) // 4
    for g in range(ngroups):
        t0 = g * 4
        nt = min(4, NT - t0)
        NTOK = nt * 128
        extra = P1KEYS if first else []
        for tt in range(nt):
            P.dma("sp", h2g[:, tt * 2048:(tt + 1) * 2048], h2_s[(t0 + tt) * 128:(t0 + tt + 1) * 128, :],
                  r=["h2_s"], w=["h2g"] + (extra if tt == 0 else []), key="h2g")
        h2Tv = h2T.rearrange("p (k t) -> p k t", t=512)
        for tt in range(nt):
            for j in range(2):
                for i in range(8):
                    k = j * 8 + i
                    P.tr(ptb[j][:, i * 128:(i + 1) * 128], h2g[:, tt * 2048 + k * 128: tt * 2048 + (k + 1) * 128],
                         identb[:], r=["h2g", "identb"], w=["ptb%d" % j])
                P.cp(h2Tv[:, j * 8:(j + 1) * 8, tt * 128:(tt + 1) * 128],
                     ptb[j][:].rearrange("p (k t) -> p k t", t=128), r=["ptb%d" % j],
                     w=["h2T"] + (extra if (tt == 0 and j == 0) else []), stream="act" if j else "dve")
        for tt in range(nt):
            P.tr(pf[0][0:NE, tt * 128:(tt + 1) * 128], G[:, t0 + tt, :], identf[:], r=["G", "identf"], w=["pf0"])
        P.cp(GT[:, 0:NTOK], pf[0][0:NE, 0:NTOK], r=["pf0"], w=["GT"])
        for tt in range(nt):
            for dc in range(4):
                ps = py[dc % 2]
                pk = "py%d" % (dc % 2)
                P.mm(ps[:], GT[:, tt * 128:(tt + 1) * 128], Bd[:, dc * 512:(dc + 1) * 512], r=["GT", "Bd"], w=[pk])
                P.cp(acc[:, tt * 2048 + dc * 512: tt * 2048 + (dc + 1) * 512], ps[:], r=[pk],
                     w=["acc"] + (extra if (tt == 0 and dc == 0) else []), stream="act" if dc % 2 else "dve")
        first = False
        for e in range(NE):
            for cg in range(NCG):
                s, key = load_piece(wg_l[e][cg])
                wv = wbuf[:, s, :].rearrange("p (k c) -> p k c", c=512)
                for tt in range(nt):
                    ps = pg[tt % 2]
                    pk = "pg%d" % (tt % 2)
                    for k in range(KD):
                        P.mm(ps[:], h2Tv[:, k, tt * 128:(tt + 1) * 128], wv[:, k, :], start=(k == 0), stop=False,
                             r=["h2T", key], w=[pk])
                    oh = identb[0:NE, e:e + 1].to_broadcast([NE, 128])
                    P.mm(ps[:, 0:256], oh, Bg[:, cg * 256:(cg + 1) * 256], start=False, stop=False,
                         r=["identb", "Bg"], w=[pk])
                    P.mm(ps[:, 256:512], oh, Bg[:, DFF + cg * 256:DFF + (cg + 1) * 256], start=False, stop=True,
                         r=["identb", "Bg"], w=[pk])
                    P.ts(glu[:], ps[:, 0:256], 7.0, None, ALU.min, r=[pk], w=["glu"])
                    P.act(sig[:], glu[:], AF.Sigmoid, r=["glu"], w=["sig"], scale=1.702)
                    P.ts(lin[:], ps[:, 256:512], 7.0, -7.0, ALU.min, ALU.max, r=[pk], w=["lin"], stream="pool" if False else "dve")
                    P.stt(glu[:], glu[:], G[:, t0 + tt, e:e + 1], sig[:], ALU.mult, ALU.mult, r=["glu", "G", "sig"], w=["glu"])
                    P.stt(actb[:, tt * 2048 + cg * 256: tt * 2048 + (cg + 1) * 256], lin[:], 1.0, glu[:], ALU.add, ALU.mult,
                          r=["lin", "glu"], w=["actb"])
            actTv = actT.rearrange("p (k t) -> p k t", t=512)
            for tt in range(nt):
                for j0 in range(0, KF, 8):
                    nj = min(8, KF - j0)
                    b = (j0 // 8) % 2
                    for i in range(nj):
                        kf = j0 + i
                        P.tr(ptb[b][:, i * 128:(i + 1) * 128], actb[:, tt * 2048 + kf * 128: tt * 2048 + (kf + 1) * 128],
                             identb[:], r=["actb", "identb"], w=["ptb%d" % b])
                    P.cp(actTv[:, j0:j0 + nj, tt * 128:(tt + 1) * 128],
                         ptb[b][:, 0:nj * 128].rearrange("p (k t) -> p k t", t=128), r=["ptb%d" % b], w=["actT"],
                         stream="act" if b else "dve")
            for dc in range(4):
                s, key = load_piece(wd_l[e][dc])
                wv = wbuf[:, s, 0:KF * 512].rearrange("p (k c) -> p k c", c=512)
                for tt in range(nt):
                    ps = py[tt % 2]
                    pk = "py%d" % (tt % 2)
                    for kf in range(KF):
                        P.mm(ps[:], actTv[:, kf, tt * 128:(tt + 1) * 128], wv[:, kf, :], start=(kf == 0), stop=(kf == KF - 1),
                             r=["actT", key], w=[pk])
                    a = acc[:, tt * 2048 + dc * 512: tt * 2048 + (dc + 1) * 512]
                    P.tt(a, ps[:], a, ALU.add, r=[pk, "acc"], w=["acc"], stream="pool" if False else "dve")
        for tt in range(nt):
            t = t0 + tt
            is_ctx = ctx_rows > 0 and t == NT - 1
            kind = 1 if is_ctx else 0
            rows = slice(t * 128, (t + 1) * 128)
            if tt == 0 or is_ctx:
                load_bc(bcA, modv[kind, 5, :], "bcA")
                if not last:
                    load_bc(bcB, modn[kind, 1, :], "bcB")
                    load_bc(bcC, gn, "bcC")
                    P.stt(bcB[:], bcB[:], 1.0, bcC[:], ALU.add, ALU.mult, r=["bcB", "bcC"], w=["bcB"])
                    load_bc(bcC, modn[kind, 0, :], "bcC")
            P.dma("sp", xt3[:], x1_s[rows, :], r=["x1_s"], w=["xt3"], key="xt3")
            a = acc[:, tt * 2048:(tt + 1) * 2048]
            P.tt(a, a, bcA[:], ALU.mult, r=["acc", "bcA"], w=["acc"])
            P.tt(xt3[:], xt3[:], a, ALU.add, r=["xt3", "acc"], w=["xt3"])
            P.dma("sp", xout[rows, :], xt3[:], r=["xt3"], key="xout")
            if not last:
                P.memset(sm[:, 5:6], 0.0, w=["sm5"])
                P.act(a, xt3[:], AF.Square, r=["xt3"], w=["acc", "sm5"], accum_out=sm[:, 5:6])
                P.ts(sm[:, 5:6], sm[:, 5:6], 1.0 / D, EPS, ALU.mult, ALU.add, r=["sm5"], w=["sm5"])
                P.act(sm[:, 5:6], sm[:, 5:6], AF.Sqrt, r=["sm5"], w=["sm5"])
                P.op("dve", lambda e: e.reciprocal(sm[:, 5:6], sm[:, 5:6]), r=["sm5"], w=["sm5"])
                P.stt(a, xt3[:], sm[:, 5:6], bcB[:], ALU.mult, ALU.mult, r=["xt3", "sm5", "bcB"], w=["acc"])
                P.tt(hn[:], a, bcC[:], ALU.add, r=["acc", "bcC"], w=["hn"])
                P.dma("sp", hnext[rows, :], hn[:], r=["hn"], key="hnext")
    P.out_dma.append("xout")
    if not last:
        P.out_dma.append("hnext")
    return P.finalize()


D = 2048
KD = 16


import os
GSTAGE = int(os.environ.get('GSTAGE', '99'))
GSUB = int(os.environ.get('GSUB', '9'))


def build_gla(NLT, NCT):
    P = Prog()
    TB = (NLT + NCT) * 128
    NC = 1568
    h = P.din("h", [TB, D], BF16)
    wc = P.din("wc", [D, NC], F32)
    w2 = P.din("w2", [2, 17, 256], F32)
    gnorm = P.din("gnorm", [512], F32)
    identb_d = P.din("identb", [128, 128], BF16)
    U_d = P.din("U", [2, 128, 128], F32)
    mixp = P.dout("mixp", [TB, 512], BF16)
    o_s = P.dtmp("o_s", [TB, 512], F32)

    W = P.sb("W", [128, KD, NC], BF16)
    identb = P.sb("identb_s", [128, 128], BF16)
    U = P.sb("U_s", [128, 2, 128], F32)
    w2s = P.sb("w2s", [17, 2, 256], F32)
    gbc = P.sb("gbc", [128, 512], F32)
    ones = P.sb("ones", [128, 128], F32)
    ht = P.sb("ht", [128, D], BF16)
    hT = P.sb("hT", [128, KD, 128], BF16)
    qT = P.sb("qT", [128, 2, 128], F32)
    kT = P.sb("kT", [128, 2, 128], F32)
    vb = P.sb("vb", [128, 512], BF16)
    r17 = P.sb("r17", [17, 128], F32)
    gk = P.sb("gk", [128, 256], F32)
    tmpa = P.sb("tmpa", [128, 256], F32)
    tmpb = P.sb("tmpb", [128, 256], F32)
    bTs = P.sb("bTs", [128, 2, 128], F32)
    ee = P.sb("ee", [128, 3, 2, 128], F32)
    qs = P.sb("qs", [128, 2, 128], BF16)
    ks = P.sb("ks", [128, 2, 128], BF16)
    qd = P.sb("qd", [128, 2, 128], BF16)
    attnT = P.sb("attnT", [128, 128], BF16)
    kd = P.sb("kd", [128, 256], BF16)
    S = P.sb("S", [128, 2, 512], F32)
    Sb = P.sb("Sb", [128, 2, 512], BF16)
    ot = P.sb("ot", [128, 512], F32)
    ot2 = P.sb("ot2", [128, 512], F32)
    go = P.sb("go", [128, 512], F32)
    yb = P.sb("yb", [128, 512], BF16)
    sm = P.sb("sm", [128, 8], F32)
    ptb = [P.ps("ptb%d" % i, [128, 1024], BF16) for i in range(2)]
    p_qk = P.ps("p_qk", [128, 512], F32)
    p_v = P.ps("p_v", [128, 512], F32)
    p_kr = P.ps("p_kr", [128, 512], F32)
    p_b = P.ps("p_b", [128, 512], F32)
    p_bT = P.ps("p_bT", [128, 512], F32)
    p_o = P.ps("p_o", [128, 512], F32)

    for k in range(KD):
        P.dma("pool", W[:, k, :], wc[k * 128:(k + 1) * 128, :], w=["W"], key="W")
    P.dma("sp", identb[:], identb_d, w=["identb"], key="identb")
    P.dma("sp", U[:], U_d.rearrange("d m c -> m d c"), w=["U"], key="U")
    P.dma("sp", w2s[:], w2.rearrange("d r c -> r d c"), w=["w2s"], key="w2s")
    P.dma("sp", gbc[:], gnorm.partition_broadcast(128), w=["gbc"], key="gbc")
    P.memset(ones[:], 1.0, w=["ones"])
    P.memset(r17[:], 1.0, w=["r17"])

    for d in range(2):
        P.memset(S[:], 0.0, w=["S"])
        P.memset(Sb[:], 0.0, w=["Sb"])
        last = 127 if d == 0 else 0
        order = [("c", i) for i in range(NCT)] + [("l", i) for i in range(NLT)]
        if d == 1:
            order = [("c", i) for i in reversed(range(NCT))] + [("l", i) for i in reversed(range(NLT))]
        for kind, i in order:
            r0 = (i if kind == "l" else NLT + i) * 128
            rows = slice(r0, r0 + 128)
            need_o = kind == "l"
            if GSTAGE < -3:
                continue
            P.dma("sp", ht[:], h[rows, :], w=["ht"], key="ht")
            for j in range(2):
                for q in range(8):
                    k = j * 8 + q
                    P.tr(ptb[j][:, q * 128:(q + 1) * 128], ht[:, k * 128:(k + 1) * 128], identb[:],
                         r=["ht", "identb"], w=["ptb%d" % j])
                P.cp(hT[:, j * 8:(j + 1) * 8, :], ptb[j][:].rearrange("p (k t) -> p k t", t=128), r=["ptb%d" % j],
                     w=["hT"], stream="act" if j else "dve")
            if GSUB < 1:
                continue
            for f in range(4):
                for k in range(KD):
                    P.mm(p_qk[:, f * 128:(f + 1) * 128], W[:, k, f * 128:(f + 1) * 128], hT[:, k, :],
                         start=(k == 0), stop=(k == KD - 1), r=["W", "hT"], w=["p_qk"])
            if GSUB < 2:
                continue
            P.ts(qT[:], p_qk[:, 0:256].rearrange("p (f t) -> p f t", t=128), 1.0 / 16, None, ALU.mult, r=["p_qk"], w=["qT"])
            P.cp(kT[:], p_qk[:, 256:512].rearrange("p (f t) -> p f t", t=128), r=["p_qk"], w=["kT"])
            if GSTAGE < -2:
                continue
            for k in range(KD):
                P.mm(p_v[:], hT[:, k, :], W[:, k, 512:1024], start=(k == 0), stop=(k == KD - 1), r=["W", "hT"], w=["p_v"])
            P.cp(vb[:], p_v[:], r=["p_v"], w=["vb"], stream="act")
            for k in range(KD):
                P.mm(p_kr[:, 0:256], hT[:, k, :], W[:, k, 256:512], start=(k == 0), stop=(k == KD - 1),
                     r=["W", "hT"], w=["p_kr"])
            if GSTAGE < -1:
                continue
            for k in range(KD):
                P.mm(p_bT[0:16, 384:512], W[:, k, 1536 + 16 * d:1552 + 16 * d], hT[:, k, :], start=(k == 0),
                     stop=(k == KD - 1), r=["W", "hT"], w=["p_rT"])
            P.cp(r17[0:16, :], p_bT[0:16, 384:512], r=["p_rT"], w=["r17"])
            P.mm(p_kr[:, 256:512], r17[:, :], w2s[:, d, :], r=["r17", "w2s"], w=["p_gk"])
            P.act(tmpa[:], p_kr[:, 256:512], AF.Exp, r=["p_gk"], w=["tmpa"], scale=-1.0)
            P.act(tmpa[:], tmpa[:], AF.Ln, r=["tmpa"], w=["tmpa"], bias=1.0)
            P.ts(gk[:], tmpa[:], -1.0 / 16, None, ALU.mult, r=["tmpa"], w=["gk"])
            if GSTAGE < 1:
                continue
            P.mm(p_b[:, 0:256], U[:, d, :], gk[:], r=["U", "gk"], w=["p_btok"])
            P.mm(p_b[:, 256:512], ones[:], gk[:], r=["ones", "gk"], w=["p_blast"])
            for f in range(2):
                P.mm(p_bT[:, f * 128:(f + 1) * 128], gk[:, f * 128:(f + 1) * 128], U[:, d, :], r=["gk", "U"], w=["p_bT"])
            P.cp(bTs[:], p_bT[:, 0:256].rearrange("p (f t) -> p f t", t=128), r=["p_bT"], w=["bTs"])
            P.ts(sm[:, 0:2], bTs[:, :, 64], -1.0, None, ALU.mult, r=["bTs"], w=["sm01"])
            for f in range(2):
                P.act(ee[:, 0, f, :], bTs[:, f, :], AF.Exp, r=["bTs", "sm01"], w=["ee"], bias=sm[:, f:f + 1], scale=1.0)
                P.act(ee[:, 1, f, :], bTs[:, f, :], AF.Exp, r=["bTs"], w=["ee"], bias=bTs[:, f, 64:65], scale=-1.0)
                P.act(ee[:, 2, f, :], bTs[:, f, :], AF.Exp, r=["bTs"], w=["ee"])
                P.act(sm[:, 2 + f:3 + f], bTs[:, f, last:last + 1], AF.Exp, r=["bTs"], w=["sm23"])
            P.tt(qs[:], qT[:], ee[:, 0], ALU.mult, r=["qT", "ee"], w=["qs"])
            P.tt(ks[:], kT[:], ee[:, 1], ALU.mult, r=["kT", "ee"], w=["ks"])
            P.tt(qd[:], qT[:], ee[:, 2], ALU.mult, r=["qT", "ee"], w=["qd"])
            if GSTAGE < 2:
                continue
            for f in range(2):
                P.mm(p_bT[:, 256:384], ks[:, f, :], qs[:, f, :], start=(f == 0), stop=(f == 1), r=["ks", "qs"], w=["p_at"])
            P.tt(attnT[:], p_bT[:, 256:384], U[:, d, :], ALU.mult, r=["p_at", "U"], w=["attnT"])
            P.cp(tmpb[:], p_b[:, 256:512], r=["p_blast"], w=["tmpb"], stream="act")
            P.tt(tmpb[:], tmpb[:], p_b[:, 0:256], ALU.subtract, r=["tmpb", "p_btok"], w=["tmpb"])
            P.act(tmpb[:], tmpb[:], AF.Exp, r=["tmpb"], w=["tmpb"])
            P.tt(kd[:], p_kr[:, 0:256], tmpb[:], ALU.mult, r=["p_kr", "tmpb"], w=["kd"])
            if GSTAGE < 3:
                continue
            if need_o:
                P.mm(p_o[:], attnT[:], vb[:], start=True, stop=False, r=["attnT", "vb"], w=["p_o"])
                for f in range(2):
                    P.mm(p_o[:], qd[:, f, :], Sb[:, f, :], start=False, stop=(f == 1), r=["qd", "Sb"], w=["p_o"])
                if d == 0:
                    P.cp(ot[:], p_o[:], r=["p_o"], w=["ot"])
                    P.dma("sp", o_s[rows, :], ot[:], r=["ot"], w=["o_s"], key="ot_st")
                else:
                    P.dma("sp", ot2[:], o_s[rows, :], r=["o_s"], w=["ot2"], key="ot2")
                    P.tt(ot[:], p_o[:], ot2[:], ALU.add, r=["p_o", "ot2"], w=["ot"])
                    for k in range(KD):
                        P.mm(p_v[:], hT[:, k, :], W[:, k, 1024:1536], start=(k == 0), stop=(k == KD - 1),
                             r=["W", "hT"], w=["p_v"])
                    P.act(go[:], p_v[:], AF.Silu, r=["p_v"], w=["go"])
                    P.memset(sm[:, 4:5], 0.0, w=["sm4"])
                    P.act(ot2[:], ot[:], AF.Square, r=["ot"], w=["ot2", "sm4"], accum_out=sm[:, 4:5])
                    P.ts(sm[:, 4:5], sm[:, 4:5], 1.0 / 512, 1e-6, ALU.mult, ALU.add, r=["sm4"], w=["sm4"])
                    P.act(sm[:, 4:5], sm[:, 4:5], AF.Sqrt, r=["sm4"], w=["sm4"])
                    P.op("dve", lambda e: e.reciprocal(sm[:, 4:5], sm[:, 4:5]), r=["sm4"], w=["sm4"])
                    P.stt(ot[:], ot[:], sm[:, 4:5], gbc[:], ALU.mult, ALU.mult, r=["ot", "sm4", "gbc"], w=["ot"])
                    P.tt(yb[:], ot[:], go[:], ALU.mult, r=["ot", "go"], w=["yb"])
                    P.dma("sp", mixp[rows, :], yb[:], r=["yb"], key="mixp")
            if GSTAGE < 4:
                continue
            for f in range(2):
                P.mm(p_o[:], kd[:, f * 128:(f + 1) * 128], vb[:], r=["kd", "vb"], w=["p_o"])
                P.stt(S[:, f, :], S[:, f, :], sm[:, 2 + f:3 + f], p_o[:], ALU.mult, ALU.add, r=["S", "sm23", "p_o"], w=["S"])
                P.cp(Sb[:, f, :], S[:, f, :], r=["S"], w=["Sb"], stream="act")
    P.memset(yb[:], 0.0, w=["yb"])
    for i in range(NCT):
        r0 = (NLT + i) * 128
        P.dma("sp", mixp[r0:r0 + 128, :], yb[:], r=["yb"], key="mixp")
    P.out_dma.append("mixp")
    return P.finalize()


D = 2048
KD = 16


import os
STAGE = int(os.environ.get('STAGE', '9'))
VAR = os.environ.get('VAR', '')
SUB = float(os.environ.get('SUB', '9'))


def build_mix0(NLT, NCT):
    P = Prog()
    P.keymap = {"B0a": "B0", "B0x": "B0", "B1z": "B1", "B2b": "B2", "B2c": "B2", "B2d": "B2", "B3a": "B3", "B3b": "B3",
                "B3c": "B3", "B4c": "B4", "B4d": "B4", "B5a": "B5", "B5b": "B5", "B5c": "B5", "B5d": "B5"}
    TB = (NLT + NCT) * 128
    S = NLT * 128
    CT = NCT * 128
    TBP = S + 4 + CT + 4
    NC = 1416
    NTT = NLT + NCT
    h = P.din("h", [TB, D], BF16)
    wc = P.din("wc", [D, NC], F32)
    qkg = P.din("qkg", [128, 320], F32)
    sinks = P.din("sinks", [128, 4], F32)
    cw = P.din("cw", [128, 30], F32)
    gpar = P.din("gpar", [128, 8], F32)
    dnn = P.din("dnn", [128, 128], F32)
    M_d = P.din("M", [128, 512 + NLT * 64], F32)
    identb_d = P.din("identb", [128, 128], BF16)
    identf_d = P.din("identf", [128, 128], F32)
    mixp = P.dout("mixp", [TB, 512], BF16)
    xs = P.dtmp("xs", [6, 128, TBP], F32)
    zs = P.dtmp("zs", [TB, 256], F32)
    gs = P.dtmp("gs", [TB, 8], F32)
    o_s = P.dtmp("o_s", [TB, 256], F32)

    W = P.sb("W", [128, KD, NC], BF16)
    identb = P.sb("identb_s", [128, 128], BF16)
    identf = P.sb("identf_s", [128, 128], F32)
    Mm = P.sb("Mm", [128, 4, 128], F32)
    gq = P.sb("gq", [128, 320], F32)
    esink = P.sb("esink", [128, 4], F32)
    cws = P.sb("cws", [128, 6, 5], F32)
    gp = P.sb("gp", [128, 8], F32)
    dnbc = P.sb("dnbc", [128, 128], F32)
    ones = P.sb("ones", [128, 128], F32)
    zero6 = P.sb("zero6", [128, 6, 2], F32)
    ht = P.sb("ht", [128, D], BF16)
    hT = P.sb("hT", [128, KD, 128], BF16)
    qkv = P.sb("qkv", [128, 384], F32)
    sq = P.sb("sq", [128, 320], F32)
    ss = P.sb("ss", [128, 8], F32)
    qkn = P.sb("qkn", [128, 320], F32)
    qkr = P.sb("qkr", [128, 320], F32)
    rt = P.sb("ropetmp", [128, 64], F32)
    rpa = P.sb("rpa", [128, NLT, 64], F32)
    qrb = P.sb("qrb", [128, 320], BF16)
    qTr = [P.sb("qTr%d" % i, [64, 512], BF16) for i in range(2)]
    kTa = P.sb("kTa", [64, NTT * 128], BF16)
    V1 = P.sb("V1", [128, NTT, 66], BF16)
    xst = P.sb("xst", [128, 6, 128], F32)
    zt = P.sb("zt", [128, 256], F32)
    gts = P.sb("gts", [128, 8], F32)
    gtm = P.sb("gtm", [128, 4], F32)
    eT = P.sb("eT", [128, 5, 512], BF16)
    den = P.sb("den", [128, 4], F32)
    aout = P.sb("aout", [128, 256], BF16)
    xw = P.sb("xw", [128, 6, 132], F32)
    yc = P.sb("yc", [128, 6, 128], F32)
    sq4 = P.sb("sq4", [128, 4, 128], F32)
    rs4 = P.sb("rs4", [128, 4, 128], F32)
    qk = P.sb("qk", [128, 4, 128], F32)
    ktv = P.sb("ktv", [128, 4, 128], F32)
    gt8 = P.sb("gt8", [128, 8], F32)
    gbc = P.sb("gbc", [128, 128], F32)
    gct = P.sb("gct", [128, 1], F32)
    egs = P.sb("egs", [128, 4], F32)
    t1 = P.sb("t1", [128, 128], F32)
    t2 = P.sb("t2", [128, 128], F32)
    Dcm = P.sb("Dcm", [128, 128], F32)
    DTs = P.sb("DTs", [128, 128], F32)
    DTi = P.sb("DTi", [128, 128], F32)
    kb = P.sb("kb", [128, 128], F32)
    kbT = P.sb("kbT", [128, 128], F32)
    Mb = [P.sb("Mb%d" % i, [128, 128], F32) for i in range(2)]
    MTb = [P.sb("MTb%d" % i, [128, 128], F32) for i in range(2)]
    attnT = P.sb("attnT", [128, 128], F32)
    R = P.sb("R", [128, 256], F32)
    wT = P.sb("wT", [128, 128], F32)
    vn = P.sb("vn", [128, 128], F32)
    o2 = P.sb("o2", [128, 128], F32)
    od = P.sb("od", [128, 256], F32)
    oprev = P.sb("oprev", [128, 256], F32)
    kdk = P.sb("kdk", [128, 128], F32)
    Sst = P.sb("Sst", [128, 2, 128], F32)
    yb = P.sb("yb", [128, 256], BF16)

    ptb = [P.ps("ptb%d" % i, [128, 1024], BF16) for i in range(2)]
    B0 = P.ps("B0", [128, 512], F32)
    B1 = P.ps("B1", [128, 512], F32)
    B2 = P.ps("B2", [128, 512], F32)
    B3 = P.ps("B3", [128, 512], F32)
    B4 = P.ps("B4", [128, 512], F32)
    B5 = P.ps("B5", [128, 512], F32)

    for k in range(KD):
        P.dma("pool", W[:, k, :], wc[k * 128:(k + 1) * 128, :], w=["W"], key="W")
    P.dma("sp", identb[:], identb_d, w=["identb"], key="identb")
    P.dma("sp", identf[:], identf_d, w=["identf"], key="identf")
    P.dma("sp", Mm[:].rearrange("p d c -> p (d c)"), M_d[:, 0:512], w=["Mm"], key="Mm")
    P.dma("sp", gq[:], qkg, w=["gq"], key="gq")
    P.dma("sp", esink[:], sinks, w=["esink"], key="esink")
    P.act(esink[:], esink[:], AF.Exp, r=["esink"], w=["esink"])
    P.dma("sp", cws[:].rearrange("p c j -> p (c j)"), cw, w=["cws"], key="cws")
    P.dma("sp", gp[:], gpar, w=["gp"], key="gp")
    P.act(gp[:, 0:4], gp[:, 0:4], AF.Exp, r=["gp"], w=["gp"])
    P.ts(gp[:, 0:4], gp[:, 0:4], -1.0, None, ALU.mult, r=["gp"], w=["gp"])
    P.dma("sp", dnbc[:], dnn, w=["dnbc"], key="dnbc")
    P.dma("sp", rpa[:].rearrange("p n c -> p (n c)"), M_d[:, 512:512 + NLT * 64], w=["rp"], key="rp")
    P.memset(ones[:], 1.0, w=["ones"])
    P.memset(zero6[:], 0.0, w=["zero6"])
    P.memset(V1[:], 1.0, w=["V1"])

    def tile_pos(kind, i):
        if kind == "l":
            return i * 128, 2 + i * 128, i
        return S + i * 128, S + 4 + 2 + i * 128, NLT + i

    def proj_tile(kind, i, qslot):
        if STAGE < 1:
            return
        r0, col0, gt = tile_pos(kind, i)
        rows = slice(r0, r0 + 128)
        P.dma("sp", ht[:], h[rows, :], r=["identb", "identf", "Mm", "gq", "esink", "cws", "gp", "dnbc", "rp"], w=["ht"], key="ht")
        for j in range(2):
            for q in range(8):
                k = j * 8 + q
                P.tr(ptb[j][:, q * 128:(q + 1) * 128], ht[:, k * 128:(k + 1) * 128], identb[:],
                     r=["ht", "identb"], w=["ptb%d" % j])
            P.cp(hT[:, j * 8:(j + 1) * 8, :], ptb[j][:].rearrange("p (k t) -> p k t", t=128), r=["ptb%d" % j],
                 w=["hT"], stream="act" if j else "dve")
        if SUB < 2:
            return
        for k in range(KD):
            P.mm(B0[:, 0:384], hT[:, k, :], W[:, k, 0:384], start=(k == 0), stop=(k == KD - 1), r=["W", "hT"], w=["B0a"])
        P.cp(qkv[:], B0[:, 0:384], r=["B0a"], w=["qkv"])
        P.tt(sq[:], qkv[:, 0:320], qkv[:, 0:320], ALU.mult, r=["qkv"], w=["sq"])
        P.op("dve", lambda e: e.reduce_sum(ss[:, 0:5], sq[:].rearrange("p (h f) -> p h f", f=64), AX.X), r=["sq"], w=["ss"])
        P.ts(ss[:, 0:5], ss[:, 0:5], 1.0 / 64, 1e-6, ALU.mult, ALU.add, r=["ss"], w=["ss"])
        P.act(ss[:, 0:5], ss[:, 0:5], AF.Sqrt, r=["ss"], w=["ss"])
        P.op("dve", lambda e: e.reciprocal(ss[:, 0:5], ss[:, 0:5]), r=["ss"], w=["ss"])
        for hh in range(5):
            cs = slice(hh * 64, (hh + 1) * 64)
            P.stt(qkn[:, cs], qkv[:, cs], ss[:, hh:hh + 1], gq[:, cs], ALU.mult, ALU.mult, r=["qkv", "ss", "gq"], w=["qkn"])
        if SUB < 3:
            return
        src = qkn
        if kind == "l":
            rp = rpa[:, i, :]
            if VAR == 'xdma':
                P.dma("sp", qkr[:, 0:64], M_d[0, :, 0:64], w=["qkr"], key="xd")
            for hh in range(0 if VAR == "noops" else 5):
                for a in range(2):
                    b0 = hh * 64 + a * 32
                    x1 = qkn[:, b0:b0 + 16]
                    x2 = qkn[:, b0 + 16:b0 + 32]
                    cv = rp[:, a * 16:(a + 1) * 16]
                    sv = rp[:, 32 + a * 16:32 + (a + 1) * 16]
                    P.tt(rt[:, 0:16], x1, cv, ALU.mult, r=["qkn", "rp"], w=["rt"])
                    P.tt(rt[:, 16:32], x2, sv, ALU.mult, r=["qkn", "rp"], w=["rt"])
                    P.tt(rt[:, 32:48], x2, cv, ALU.mult, r=["qkn", "rp"], w=["rt"])
                    P.tt(rt[:, 48:64], x1, sv, ALU.mult, r=["qkn", "rp"], w=["rt"])
                    P.tt(qkr[:, b0:b0 + 16], rt[:, 0:16], rt[:, 16:32], ALU.subtract, r=["rt"], w=["qkr"])
                    P.tt(qkr[:, b0 + 16:b0 + 32], rt[:, 32:48], rt[:, 48:64], ALU.add, r=["rt"], w=["qkr"])
            src = qkr
        if SUB < 4.1:
            return
        P.cp(qrb[:], src[:], r=["qkn", "qkr"], w=["qrb"], stream="act")
        for hh in range(5):
            P.tr(ptb[1][0:64, hh * 128:(hh + 1) * 128], qrb[:, hh * 64:(hh + 1) * 64], identb[:],
                 r=["qrb", "identb"], w=["ptb1"])
        if SUB < 4.2:
            return
        P.cp(qTr[qslot][:, :], ptb[1][0:64, 0:512], r=["ptb1"], w=["qT%d" % qslot])
        if SUB < 4.3:
            return
        P.cp(kTa[:, gt * 128:(gt + 1) * 128], ptb[1][0:64, 512:640], r=["ptb1"], w=["kTa"])
        if SUB < 4.4:
            return
        P.cp(V1[:, gt, 0:64], qkv[:, 320:384], r=["qkv"], w=["V1"])
        if SUB < 5:
            return
        for c in range(6):
            for k in range(KD):
                P.mm(B0[:, 384:512], W[:, k, 384 + c * 128:384 + (c + 1) * 128], hT[:, k, :], start=(k == 0),
                     stop=(k == KD - 1), r=["W", "hT"], w=["B0x"])
            P.cp(xst[:, c, :], B0[:, 384:512], r=["B0x"], w=["xst"], stream="act" if c % 2 else "dve")
        P.dma("sp", xs[:, :, col0:col0 + 128].rearrange("c p t -> p c t"), xst[:], r=["xst"], w=["xs"], key="xst_st")
        if SUB < 6:
            return
        for k in range(KD):
            P.mm(B1[:, 0:264], hT[:, k, :], W[:, k, 1152:1416], start=(k == 0), stop=(k == KD - 1), r=["W", "hT"], w=["B1z"])
        P.act(zt[:], B1[:, 0:256], AF.Silu, r=["B1z"], w=["zt"])
        P.dma("sp", zs[rows, :], zt[:], r=["zt"], w=["zs"], key="zt_st")
        P.tt(gtm[:], B1[:, 256:260], gp[:, 4:8], ALU.add, r=["B1z", "gp"], w=["gtm"])
        P.act(gtm[:], gtm[:], AF.Exp, r=["gtm"], w=["gtm"])
        P.act(gtm[:], gtm[:], AF.Ln, r=["gtm"], w=["gtm"], bias=1.0)
        P.tt(gts[:, 0:4], gtm[:], gp[:, 0:4], ALU.mult, r=["gtm", "gp"], w=["gts"])
        P.act(gts[:, 4:8], B1[:, 260:264], AF.Sigmoid, r=["B1z"], w=["gts"])
        P.dma("sp", gs[rows, :], gts[:], r=["gts"], w=["gs"], key="gts_st")

    def attn_block(kind, i, qslot):
        if STAGE < 2:
            return
        r0, col0, gt = tile_pos(kind, i)
        kbs = []
        if kind == "l":
            if i > 0:
                kbs.append((i - 1, 1))
            kbs.append((i, None))
            if i < NLT - 1:
                kbs.append((i + 1, 0))
        for c in range(NCT):
            kbs.append((NLT + c, None))
        for idx, (kg, mk) in enumerate(kbs):
            ps = B2 if idx % 2 == 0 else B3
            pk = "B2" if idx % 2 == 0 else "B3"
            P.mm(ps[:], kTa[:, kg * 128:(kg + 1) * 128], qTr[qslot][:, :], r=["kTa", "qT%d" % qslot], w=[pk])
            P.act(eT[:, idx, :], ps[:], AF.Exp, r=[pk], w=["eT"], scale=0.125)
            if mk is not None:
                for hh in range(4):
                    P.tt(eT[:, idx, hh * 128:(hh + 1) * 128], eT[:, idx, hh * 128:(hh + 1) * 128], Mm[:, mk, :], ALU.mult,
                         r=["eT", "Mm"], w=["eT"])
        for hh in range(4):
            for idx, (kg, mk) in enumerate(kbs):
                P.mm(B4[:, hh * 128:hh * 128 + 65], eT[:, idx, hh * 128:(hh + 1) * 128], V1[:, kg, 0:65], start=(idx == 0),
                     stop=(idx == len(kbs) - 1), r=["eT", "V1"], w=["B4"])
        pv = B4[:, :].rearrange("p (h f) -> p h f", f=128)
        P.tt(den[:], pv[:, :, 64], esink[:], ALU.add, r=["B4", "esink"], w=["den"])
        P.op("dve", lambda e: e.reciprocal(den[:], den[:]), r=["den"], w=["den"])
        for hh in range(4):
            P.ts(aout[:, hh * 64:(hh + 1) * 64], B4[:, hh * 128:hh * 128 + 64], den[:, hh:hh + 1], None, ALU.mult,
                 r=["B4", "den"], w=["aout"])
        P.dma("sp", mixp[r0:r0 + 128, 0:256], aout[:], r=["aout"], key="mixp")

    for c in range(NCT):
        proj_tile("c", c, c % 2)
    for c in range(NCT):
        attn_block("c", c, c % 2)
    for i in range(NLT):
        proj_tile("l", i, i % 2)
        if i >= 1:
            attn_block("l", i - 1, (i - 1) % 2)
    attn_block("l", NLT - 1, (NLT - 1) % 2)

    PSK = ["B0a", "B0x", "B1z", "B2", "B3", "B4", "B2b", "B2c", "B2d", "B3a", "B3b", "B3c", "B4c", "B4d",
           "B5a", "B5b", "B5c", "B5d"]
    P.memset(ss[:, 7:8], 0.0, w=PSK + ["ss7"])
    for d in range(2 if STAGE >= 3 else 0):
        P.memset(Sst[:], 0.0, w=["Sst"])
        iMin = 0 if d == 0 else 1
        iMst = 2 if d == 0 else 3
        iLst = 3 if d == 0 else 2
        last = 127 if d == 0 else 0
        order = [("c", i) for i in range(NCT)] + [("l", i) for i in range(NLT)]
        if d == 1:
            order = [("c", i) for i in reversed(range(NCT))] + [("l", i) for i in reversed(range(NLT))]
        for kind, i in order:
            r0, col0, gt = tile_pos(kind, i)
            rows = slice(r0, r0 + 128)
            nlast = (NLT if kind == "l" else NCT) - 1
            lo = 2 if i == 0 else 0
            hi = 130 if i == nlast else 132
            if lo:
                P.memset(xw[:, :, 0:2], 0.0, w=["xw"])
            if hi < 132:
                P.memset(xw[:, :, 130:132], 0.0, w=["xw"])
            P.dma("sp", xw[:, :, lo:hi], xs[:, :, col0 - 2 + lo:col0 - 2 + hi].rearrange("c p t -> p c t"), r=["xs"], w=["xw"], key="xw")
            P.dma("sp", gt8[:], gs[rows, :], r=["gs"], w=["gt8"], key="gt8")
            for c in range(6):
                P.ts(yc[:, c, :], xw[:, c, 0:128], cws[:, c, 0:1], None, ALU.mult, r=["xw", "cws"], w=["yc"])
                for j in range(1, 5):
                    P.stt(yc[:, c, :], xw[:, c, j:j + 128], cws[:, c, j:j + 1], yc[:, c, :], ALU.mult, ALU.add,
                          r=["xw", "cws", "yc"], w=["yc"])
            P.act(yc[:], yc[:], AF.Silu, r=["yc"], w=["yc"])
            P.tt(sq4[:], yc[:, 0:4, :], yc[:, 0:4, :], ALU.mult, r=["yc"], w=["sq4"])
            P.mm(B0[:], ones[:], sq4[:].rearrange("p c t -> p (c t)"), r=["ones", "sq4"], w=["B0a", "B0x"])
            P.ts(rs4[:].rearrange("p c t -> p (c t)"), B0[:], 1e-6, None, ALU.add, r=["B0a"], w=["rs4"])
            P.act(rs4[:], rs4[:], AF.Sqrt, r=["rs4"], w=["rs4"])
            P.op("dve", lambda e: e.reciprocal(rs4[:], rs4[:]), r=["rs4"], w=["rs4"])
            P.ts(rs4[:, 0:2, :], rs4[:, 0:2, :], 128 ** -0.5, None, ALU.mult, r=["rs4"], w=["rs4"])
            P.tt(qk[:], yc[:, 0:4, :], rs4[:], ALU.mult, r=["yc", "rs4"], w=["qk"])
            for hh in range(2):
                P.tr(B1[:, hh * 128:(hh + 1) * 128], qk[:, 2 + hh, :], identf[:], r=["qk", "identf"], w=["B1z"])
                P.tr(B1[:, 256 + hh * 128:256 + (hh + 1) * 128], yc[:, 4 + hh, :], identf[:], r=["yc", "identf"], w=["B1z"])
            P.cp(ktv[:].rearrange("p c t -> p (c t)"), B1[:], r=["B1z"], w=["ktv"])
            for hh in range(2 if STAGE >= 4 else 0):
                g = gt8[:, 2 * d + hh:2 * d + hh + 1]
                beta = gt8[:, 4 + 2 * d + hh:4 + 2 * d + hh + 1]
                qT = qk[:, hh, :]
                kT = qk[:, 2 + hh, :]
                k_tok = ktv[:, hh, :]
                v_tok = ktv[:, 2 + hh, :]
                P.ts(gbc[:], ones[:], g, None, ALU.mult, r=["ones", "gt8"], w=["gbc"])
                P.mm(B2[:, 0:128], gbc[:], Mm[:, iMin, :], r=["gbc", "Mm"], w=["B2"])
                P.mm(B3[:, 256:384], Mm[:, iMin, :], gbc[:], r=["Mm", "gbc"], w=["B3c"])
                P.cp(gct[:], B3[:, 256:257], r=["B3c"], w=["gct"])
                P.act(egs[:, 0:1], gct[:, 0:1], AF.Exp, r=["gct"], w=["egs0"])
                P.cp(egs[:, 3:4], B2[:, last:last + 1], r=["B2"], w=["egs3"])
                P.act(egs[:, 1:2], egs[:, 3:4], AF.Exp, r=["egs3"], w=["egs1"])
                P.ts(egs[:, 2:3], B2[:, last:last + 1], gct[:, 0:1], None, ALU.subtract, r=["B2", "gct"], w=["egs2"])
                P.act(egs[:, 2:3], egs[:, 2:3], AF.Exp, r=["egs2"], w=["egs2"])
                P.ts(t1[:], B2[:, 0:128], gct[:, 0:1], 0.0, ALU.subtract, ALU.max, r=["B2", "gct"], w=["t1"])
                P.act(Dcm[:], t1[:], AF.Exp, r=["t1"], w=["Dcm"], scale=-1.0)
                P.tt(Dcm[:], Dcm[:], Mm[:, iLst, :], ALU.mult, r=["Dcm", "Mm"], w=["Dcm"])
                P.ts(t2[:], B2[:, 0:128], gct[:, 0:1], 0.0, ALU.subtract, ALU.min, r=["B2", "gct"], w=["t2"])
                P.act(t2[:], t2[:], AF.Exp, r=["t2"], w=["t2"])
                P.tt(DTs[:], t2[:], Mm[:, iMst, :], ALU.mult, r=["t2", "Mm"], w=["DTs"])
                P.tt(DTi[:], t2[:], Mm[:, iMin, :], ALU.mult, r=["t2", "Mm"], w=["DTi"])
                P.ts(kb[:], k_tok, beta, None, ALU.mult, r=["ktv", "gt8"], w=["kb"])
                P.tr(B3[:, 0:128], kb[:], identf[:], r=["kb", "identf"], w=["B3a"])
                P.cp(kbT[:], B3[:, 0:128], r=["B3a"], w=["kbT"])
                P.mm(B2[:, 128:256], kbT[:], kT, r=["kbT", "qk"], w=["B2b"])
                P.mm(B2[:, 256:384], kT, kbT[:], r=["kbT", "qk"], w=["B2c"])
                P.mm(B2[:, 384:512], kT, qT, r=["qk"], w=["B2d"])
                P.stt(Mb[0][:], B2[:, 128:256], -1.0, Dcm[:], ALU.mult, ALU.mult, r=["B2b", "Dcm"], w=["Mb0"])
                P.stt(MTb[0][:], B2[:, 256:384], -1.0, DTs[:], ALU.mult, ALU.mult, r=["B2c", "DTs"], w=["MTb0"])
                P.tt(attnT[:], B2[:, 384:512], DTi[:], ALU.mult, r=["B2d", "DTi"], w=["attnT"])
                P.ts(R[:, 0:128], v_tok, beta, None, ALU.mult, r=["ktv", "gt8"], w=["R"])
                P.ts(R[:, 128:256], kb[:], egs[:, 0:1], None, ALU.mult, r=["kb", "egs0"], w=["R"])
                cur = 0
                for it in range(7):
                    P.mm(B4[:, 0:256], MTb[cur][:], R[:], r=["MTb%d" % cur, "R"], w=["B4"])
                    if it < 6:
                        P.mm(B5[:, 0:128], MTb[cur][:], Mb[cur][:], r=["MTb%d" % cur, "Mb%d" % cur], w=["B5a"])
                        P.mm(B5[:, 128:256], Mb[cur][:], MTb[cur][:], r=["MTb%d" % cur, "Mb%d" % cur], w=["B5b"])
                    P.tt(R[:], B4[:, 0:256], R[:], ALU.add, r=["B4", "R"], w=["R"])
                    if it < 6:
                        nx = 1 - cur
                        P.cp(Mb[nx][:], B5[:, 0:128], r=["B5a"], w=["Mb%d" % nx])
                        P.cp(MTb[nx][:], B5[:, 128:256], r=["B5b"], w=["MTb%d" % nx])
                        cur = nx
                P.tr(B3[:, 128:256], R[:, 128:256], identf[:], r=["R", "identf"], w=["B3b"])
                P.cp(wT[:], B3[:, 128:256], r=["B3b"], w=["wT"])
                P.mm(B5[:, 256:384], wT[:], Sst[:, hh, :], r=["wT", "Sst"], w=["B5c"])
                P.tt(vn[:], R[:, 0:128], B5[:, 256:384], ALU.subtract, r=["R", "B5c"], w=["vn"])
                P.mm(B5[:, 384:512], qT, Sst[:, hh, :], r=["qk", "Sst"], w=["B5d"])
                P.mm(B4[:, 256:384], attnT[:], vn[:], r=["attnT", "vn"], w=["B4c"])
                P.cp(o2[:], B4[:, 256:384], r=["B4c"], w=["o2"])
                P.stt(od[:, hh * 128:(hh + 1) * 128], B5[:, 384:512], egs[:, 0:1], o2[:], ALU.mult, ALU.add,
                      r=["B5d", "egs0", "o2"], w=["od"])
                P.ts(kdk[:], k_tok, egs[:, 2:3], None, ALU.mult, r=["ktv", "egs2"], w=["kdk"])
                P.mm(B4[:, 384:512], kdk[:], vn[:], r=["kdk", "vn"], w=["B4d"])
                P.stt(Sst[:, hh, :], Sst[:, hh, :], egs[:, 1:2], B4[:, 384:512], ALU.mult, ALU.add,
                      r=["Sst", "egs1", "B4d"], w=["Sst"])
            if d == 0:
                P.dma("sp", o_s[rows, :], od[:], r=["od"], w=["o_s"], key="od_st")
            else:
                P.dma("sp", oprev[:], o_s[rows, :], r=["o_s"], w=["oprev"], key="oprev")
                P.dma("sp", zt[:], zs[rows, :], r=["zs"], w=["zt"], key="zt_ld")
                P.tt(od[:], od[:], oprev[:], ALU.add, r=["od", "oprev"], w=["od"])
                P.tt(oprev[:], od[:], od[:], ALU.mult, r=["od"], w=["oprev"])
                P.op("dve", lambda e: e.reduce_sum(ss[:, 5:7], oprev[:].rearrange("p (h f) -> p h f", f=128), AX.X),
                     r=["oprev"], w=["ss57"])
                P.ts(ss[:, 5:7], ss[:, 5:7], 1.0 / 128, 1e-6, ALU.mult, ALU.add, r=["ss57"], w=["ss57"])
                P.act(ss[:, 5:7], ss[:, 5:7], AF.Sqrt, r=["ss57"], w=["ss57"])
                P.op("dve", lambda e: e.reciprocal(ss[:, 5:7], ss[:, 5:7]), r=["ss57"], w=["ss57"])
                for hh in range(2):
                    cs = slice(hh * 128, (hh + 1) * 128)
                    P.stt(od[:, cs], od[:, cs], ss[:, 5 + hh:6 + hh], dnbc[:], ALU.mult, ALU.mult, r=["od", "ss57", "dnbc"], w=["od"])
                P.tt(yb[:], od[:], zt[:], ALU.mult, r=["od", "zt"], w=["yb"])
                P.dma("sp", mixp[rows, 256:512], yb[:], r=["yb"], key="mixp")
    P.out_dma.append("mixp")
    return P.finalize()


D = 2048


def _run(nc, in_maps):
    return run_bass_kernel_spmd(nc, in_maps, core_ids=list(range(8))).results


def _bc(v):
    v = np.asarray(v, np.float32).reshape(1, -1)
    return np.ascontiguousarray(np.broadcast_to(v, (128, v.shape[1])))


def _rope_table(S):
    GRID_W = 64
    rows = S // GRID_W
    row = np.repeat(np.arange(rows, dtype=np.float32), GRID_W)
    col = np.tile(np.arange(GRID_W, dtype=np.float32), rows)
    inv = (np.float32(10000.0) ** (-np.arange(16, dtype=np.float32) / np.float32(16))).astype(np.float32)
    ang = np.stack([row[:, None] * inv, col[:, None] * inv], axis=1).astype(np.float32)
    return np.concatenate([np.cos(ang).reshape(S, 32), np.sin(ang).reshape(S, 32)], axis=1).astype(np.float32)


def kernel(x, c, ctx, c_ctx, w_mod, b_mod, norm_g, e_w_in, e_w_out, e_q_gain, e_k_gain, e_sinks, e_conv_w,
           e_a_log, e_dt_bias, e_dn_norm, o_w_in, o_gate_w2, o_gate_b, o_gla_norm, o_w_out,
           w_router, b_router, w_gu, b_gu, w_down, b_down):
    f32 = lambda a: np.ascontiguousarray(np.asarray(a, dtype=np.float32))
    x, c, ctx, c_ctx, w_mod, b_mod, norm_g = map(f32, (x, c, ctx, c_ctx, w_mod, b_mod, norm_g))
    B, S, _ = x.shape
    CTXL = ctx.shape[1]
    DFF = w_gu.shape[-1] // 2
    NLc = S // 4 // 128
    CR = CTXL // 4
    NLT, NCT = S // 128, CTXL // 128
    TB = S + CTXL
    identb = np.eye(128).astype(NPBF)
    identf = np.eye(128, dtype=np.float32)
    o = np.ones((128, 128), np.float32)
    M4 = np.stack([np.triu(o), np.tril(o), np.triu(o, 1), np.tril(o, -1)])
    cv = np.stack([c[0], c[1], c_ctx])
    cT = np.ascontiguousarray(cv.reshape(3, 16, 128).transpose(2, 1, 0).reshape(128, 48))
    ims = []
    for cc in range(8):
        cs = slice(cc * 1536, (cc + 1) * 1536)
        ims.append(dict(cT=cT, wm=np.ascontiguousarray(w_mod[:, :, cs]), bm=np.ascontiguousarray(b_mod[:, cs]).reshape(1, 2 * 1536)))
    r = _run(build_mod(), ims)
    mod = np.concatenate([r[cc]["modo"] for cc in range(8)], axis=2).reshape(2, 3, 6, D)

    def tok_rows(cc):
        b, q = cc // 4, cc % 4
        return b, slice(q * (S // 4), (q + 1) * (S // 4)), slice(q * CR, (q + 1) * CR)

    NT0 = NLc + 1
    xins, ims = [], []
    for cc in range(8):
        b, ls, cs = tok_rows(cc)
        xin = np.zeros((NT0 * 128, D), np.float32)
        xin[:NLc * 128] = x[b, ls]
        xin[NLc * 128:NLc * 128 + CR] = ctx[b, cs]
        xins.append(xin)
        modn = np.ascontiguousarray(np.stack([mod[0, b, 0:2], mod[0, 2, 0:2]]))
        ims.append(dict(xin=xin, g=norm_g[0, 0], modn=modn))
    r = _run(build_pre(NT0, CR), ims)

    def gather_h(res, key):
        hb = np.zeros((B, TB, D), NPBF)
        for cc in range(8):
            b, ls, cs = tok_rows(cc)
            hb[b, ls] = res[cc][key][:NLc * 128]
            hb[b, S + cs.start:S + cs.stop] = res[cc][key][NLc * 128:NLc * 128 + CR]
        return hb

    hb = gather_h(r, "hout")
    W0 = f32(e_w_in[0])
    cwT = f32(e_conv_w[0])
    rope = _rope_table(S)
    Mc = np.ascontiguousarray(np.concatenate([M4.transpose(1, 0, 2).reshape(128, 512),
                                              rope.reshape(NLT, 128, 64).transpose(1, 0, 2).reshape(128, NLT * 64)], axis=1))
    ims = []
    for cc in range(8):
        b, j = cc // 4, cc % 4
        base = 1536
        cols = np.concatenate([np.arange(j * 256, (j + 1) * 256), 1024 + np.arange(j * 64, (j + 1) * 64),
                               1280 + np.arange(j * 64, (j + 1) * 64),
                               base + np.arange(2 * j * 128, (2 * j + 2) * 128),
                               base + 1024 + np.arange(2 * j * 128, (2 * j + 2) * 128),
                               base + 2048 + np.arange(2 * j * 128, (2 * j + 2) * 128),
                               base + 3072 + np.arange(2 * j * 128, (2 * j + 2) * 128),
                               5632 + np.arange(2 * j, 2 * j + 2), 5640 + np.arange(2 * j, 2 * j + 2),
                               5648 + np.arange(2 * j, 2 * j + 2), 5656 + np.arange(2 * j, 2 * j + 2)])
        ccols = np.concatenate([np.arange(2 * j * 128, (2 * j + 2) * 128), 1024 + np.arange(2 * j * 128, (2 * j + 2) * 128),
                                2048 + np.arange(2 * j * 128, (2 * j + 2) * 128)])
        cw = np.ascontiguousarray(cwT[:, ccols].T.reshape(6, 128, 5).transpose(1, 0, 2).reshape(128, 30))
        gpar = np.concatenate([np.asarray(e_a_log, np.float32)[0, 0, 2 * j:2 * j + 2], np.asarray(e_a_log, np.float32)[0, 1, 2 * j:2 * j + 2],
                               np.asarray(e_dt_bias, np.float32)[0, 0, 2 * j:2 * j + 2], np.asarray(e_dt_bias, np.float32)[0, 1, 2 * j:2 * j + 2]])
        ims.append(dict(h=np.ascontiguousarray(hb[b]), wc=np.ascontiguousarray(W0[:, cols]),
                        qkg=_bc(np.concatenate([np.asarray(e_q_gain, np.float32)[0]] * 4 + [np.asarray(e_k_gain, np.float32)[0]])),
                        sinks=_bc(np.asarray(e_sinks, np.float32)[0, 4 * j:4 * j + 4]), cw=cw, gpar=_bc(gpar),
                        dnn=_bc(np.asarray(e_dn_norm, np.float32)[0]), M=Mc, identb=identb, identf=identf))
    r = _run(build_mix0(NLT, NCT), ims)
    mixf = np.zeros((B, TB, D), NPBF)
    for cc in range(8):
        b, j = cc // 4, cc % 4
        mixf[b, :, j * 256:(j + 1) * 256] = r[cc]["mixp"][:, 0:256]
        mixf[b, :, 1024 + j * 256:1024 + (j + 1) * 256] = r[cc]["mixp"][:, 256:512]

    def post(layer, last, xin_list, mixfull, wout):
        NT = NLc if last else NLc + 1
        wr = f32(w_router[layer]); br = f32(b_router[layer]).reshape(1, 32)
        wgu = f32(w_gu[layer]); bgu = f32(b_gu[layer]); wdn = f32(w_down[layer]); bdn = f32(b_down[layer])
        wo = f32(wout)
        ims = []
        for cc in range(8):
            b, ls, cs = tok_rows(cc)
            mix = np.zeros((NT * 128, D), NPBF)
            mix[:NLc * 128] = mixfull[b, ls]
            if not last:
                mix[NLc * 128:NLc * 128 + CR] = mixfull[b, S + cs.start:S + cs.stop]
            modv = np.ascontiguousarray(np.stack([mod[layer, b], mod[layer, 2]]))
            im = dict(xin=xin_list[cc], mix=mix, modv=modv, g2=norm_g[layer, 1], wout=wo, wr=wr, br=br, wgu=wgu, bgu=bgu,
                      wdn=wdn, bdn=bdn, identb=identb, identf=identf)
            if not last:
                im["gn"] = norm_g[layer + 1, 0]
                im["modn"] = np.ascontiguousarray(np.stack([mod[layer + 1, b, 0:2], mod[layer + 1, 2, 0:2]]))
            ims.append(im)
        return _run(build_post(NT, DFF, last, 0 if last else CR), ims)

    r = post(0, False, xins, mixf, e_w_out[0])
    x1 = [np.ascontiguousarray(r[cc]["xout"][:NLc * 128]) for cc in range(8)]
    hb = gather_h(r, "hnext")
    W1 = f32(o_w_in[0]); gw2 = f32(o_gate_w2[0]); gb = f32(o_gate_b[0])
    U2 = np.ascontiguousarray(M4[0:2])
    ims = []
    for cc in range(8):
        b, j = cc // 4, cc % 4
        cols = np.concatenate([np.arange(j * 256, (j + 1) * 256), 1024 + np.arange(j * 256, (j + 1) * 256),
                               2048 + np.arange(j * 512, (j + 1) * 512), 4096 + np.arange(j * 512, (j + 1) * 512),
                               np.arange(6144, 6176)])
        w2 = np.ascontiguousarray(np.concatenate([gw2[:, :, j * 256:(j + 1) * 256], gb[:, None, j * 256:(j + 1) * 256]], axis=1))
        ims.append(dict(h=np.ascontiguousarray(hb[b]), wc=np.ascontiguousarray(W1[:, cols]), w2=w2,
                        gnorm=f32(o_gla_norm[0]), identb=identb, U=U2))
    r = _run(build_gla(NLT, NCT), ims)
    mixf = np.zeros((B, TB, D), NPBF)
    for cc in range(8):
        b, j = cc // 4, cc % 4
        mixf[b, :, j * 512:(j + 1) * 512] = r[cc]["mixp"]
    r = post(1, True, x1, mixf, o_w_out[0])
    out = np.zeros((B, S, D), np.float32)
    for cc in range(8):
        b, ls, cs = tok_rows(cc)
        out[b, ls] = r[cc]["xout"][:NLc * 128]
    return out
```

```python
import os
import numpy as np
import ml_dtypes
from contextlib import ExitStack
import concourse.bass as bass
import concourse.mybir as mybir
from concourse.bass_utils import run_bass_kernel_spmd

F32 = mybir.dt.float32
BF16 = mybir.dt.bfloat16
I32 = mybir.dt.int32
U32 = mybir.dt.uint32
AF = mybir.ActivationFunctionType
ALU = mybir.AluOpType
AX = mybir.AxisListType
NPBF = ml_dtypes.bfloat16

SEM_ROT = 20000


class _Op:
    __slots__ = ("stream", "fn", "r", "w", "dma", "deps", "sig", "need")

    def __init__(self, stream, fn, r, w, dma):
        self.stream = stream
        self.fn = fn
        self.r = r
        self.w = w
        self.dma = dma
        self.deps = None
        self.sig = None
        self.need = False


class Prog:
    def __init__(self):
        self.nc = bass.Bass("TRN2", target_bir_lowering=False)
        self.es = ExitStack()
        self.ops = []
        self.out_dma = []
        self.keymap = {}
        self.ksuf = ""
        self.ksuf_keys = set()

    def din(self, name, shape, dt):
        return self.nc.dram_tensor(name, list(shape), dt, kind="ExternalInput").ap()

    def dout(self, name, shape, dt):
        return self.nc.dram_tensor(name, list(shape), dt, kind="ExternalOutput").ap()

    def dtmp(self, name, shape, dt):
        return self.nc.dram_tensor(name, list(shape), dt, kind="Internal").ap()

    def sb(self, name, shape, dt):
        return self.es.enter_context(self.nc.sbuf_tensor(name, list(shape), dt))

    def ps(self, name, shape, dt):
        return self.es.enter_context(self.nc.psum_tensor(name, list(shape), dt))

    def _km(self, keys):
        out = []
        for k in keys:
            if self.ksuf and k in self.ksuf_keys:
                k = k + self.ksuf
            k = self.keymap.get(k, k)
            if k not in out:
                out.append(k)
        return tuple(out)

    def op(self, stream, fn, r=(), w=()):
        self.ops.append(_Op(stream, fn, self._km(r), self._km(w), None))

    def dma(self, stream, out, in_, r=(), w=(), key=None, **kw):
        assert key is not None
        self.ops.append(_Op(stream, lambda e: e.dma_start(out=out, in_=in_, **kw), self._km(r), self._km(w), key))

    def dmaf(self, stream, fn, r=(), w=(), key=None):
        assert key is not None
        self.ops.append(_Op(stream, fn, self._km(r), self._km(w), key))

    def mm(self, out, lhsT, rhs, start=True, stop=True, r=(), w=(), **kw):
        self.op("pe", lambda e: e.matmul(out, lhsT, rhs, start=start, stop=stop, **kw), r, w)

    def tr(self, out, in_, ident, r=(), w=()):
        self.op("pe", lambda e: e.transpose(out, in_, ident), r, w)

    def act(self, out, in_, func, r=(), w=(), stream="act", **kw):
        self.op(stream, lambda e: e.activation(out=out, in_=in_, func=func, **kw), r, w)

    def tt(self, out, in0, in1, op, r=(), w=(), stream="dve"):
        self.op(stream, lambda e: e.tensor_tensor(out, in0, in1, op), r, w)

    def ts(self, out, in0, s1, s2, op0, op1=None, r=(), w=(), stream="dve", **kw):
        if op1 is None:
            self.op(stream, lambda e: e.tensor_scalar(out, in0, s1, None, op0, **kw), r, w)
        else:
            self.op(stream, lambda e: e.tensor_scalar(out, in0, s1, s2, op0, op1, **kw), r, w)

    def stt(self, out, in0, scalar, in1, op0, op1, r=(), w=(), stream="dve", **kw):
        self.op(stream, lambda e: e.scalar_tensor_tensor(out, in0, scalar, in1, op0, op1, **kw), r, w)

    def cp(self, out, in_, r=(), w=(), stream="dve"):
        if stream == "act":
            self.op(stream, lambda e: e.copy(out, in_), r, w)
        else:
            self.op(stream, lambda e: e.tensor_copy(out, in_), r, w)

    def memset(self, ap, val, w=(), stream="dve"):
        self.op(stream, lambda e: e.memset(ap, val), (), w)

    def finalize(self):
        nc = self.nc
        ops = self.ops
        state = {}

        def dom(o):
            return ("dma", o.dma) if o.dma is not None else o.stream

        for i, o in enumerate(ops):
            deps = set()
            d_i = dom(o)
            for k in o.r:
                st = state.setdefault(k, ({}, {}))
                deps.update(st[0].values())
                st[1][d_i] = i
            for k in o.w:
                st = state.setdefault(k, ({}, {}))
                wr, rd = st
                others_r = {d: j for d, j in rd.items() if j != i}
                if others_r:
                    for d, j in others_r.items():
                        deps.add(j)
                    for d, j in wr.items():
                        if d != d_i or (o.dma is None and d_i != "pe"):
                            deps.add(j)
                    wr.clear()
                    rd.clear()
                    wr[d_i] = i
                else:
                    for d, j in wr.items():
                        if d != d_i or (o.dma is None and d_i != "pe"):
                            deps.add(j)
                    rd.clear()
                    wr[d_i] = i
            deps.discard(i)
            o.deps = deps
            for j in deps:
                ops[j].need = True
        eng_cnt = {}
        sem_of = {}

        def get_sem(key):
            if key not in sem_of:
                sem_of[key] = self.es.enter_context(nc.semaphore("s%d" % len(sem_of)))
            return sem_of[key]

        dma_cum = {}
        for o in ops:
            if o.dma is not None:
                dma_cum[o.dma] = dma_cum.get(o.dma, 0) + 16
                o.sig = (("dma", o.dma), dma_cum[o.dma])
            elif o.need:
                c = eng_cnt.get(o.stream, 0) + 1
                eng_cnt[o.stream] = c
                o.sig = ((o.stream, (c - 1) // SEM_ROT), (c - 1) % SEM_ROT + 1)
        streams = {}
        for i, o in enumerate(ops):
            streams.setdefault(o.stream, []).append(i)
        waits = [None] * len(ops)
        waited = {}
        for i, o in enumerate(ops):
            wl = {}
            for j in o.deps:
                sk, val = ops[j].sig
                if wl.get(sk, 0) < val:
                    wl[sk] = val
            ws = waited.setdefault(o.stream, {})
            out = []
            for sk, val in wl.items():
                if ws.get(sk, 0) < val:
                    ws[sk] = val
                    out.append((sk, val))
            waits[i] = out
        for sk in set(s for w in waits for s, _ in w):
            get_sem(sk)
        for o in ops:
            if o.sig is not None:
                get_sem(o.sig[0])
        final = [(("dma", k), dma_cum[k]) for k in self.out_dma if k in dma_cum]

        def emit(stream, e):
            for i in streams.get(stream, []):
                o = ops[i]
                for sk, val in waits[i]:
                    e.wait_ge(sem_of[sk], val)
                ins = o.fn(e)
                if o.sig is not None:
                    sk, _ = o.sig
                    ins.then_inc(sem_of[sk], 16 if o.dma is not None else 1)
            if stream == "sp":
                for sk, val in final:
                    e.wait_ge(sem_of[sk], val)

        with nc.Block() as block:
            @block.sync
            def _(e):
                emit("sp", e)

            @block.tensor
            def _(e):
                emit("pe", e)

            @block.vector
            def _(e):
                emit("dve", e)

            @block.scalar
            def _(e):
                emit("act", e)

            @block.gpsimd
            def _(e):
                emit("pool", e)
        self.es.close()
        self.n_sems = len(sem_of)
        return nc


D = 2048
KD = 16
EPS = 1e-6


def build_mod():
    P = Prog()
    cT = P.din("cT", [128, KD * 3], F32)
    wm = P.din("wm", [2, D, 1536], F32)
    bm = P.din("bm", [1, 2 * 1536], F32)
    out = P.dout("modo", [2, 3, 1536], F32)
    sc = P.sb("sc", [128, KD, 3], F32)
    wms = P.sb("wms", [128, KD, 1536], F32)
    bms = P.sb("bms", [1, 2, 1536], F32)
    ones3 = P.sb("ones3", [1, 3], F32)
    res = P.sb("res", [3, 1536], F32)
    ps = P.ps("pm", [128, 512], F32)
    P.dma("sp", sc[:].rearrange("p k r -> p (k r)"), cT, w=["sc"], key="sc")
    P.act(sc[:].rearrange("p k r -> p (k r)"), sc[:].rearrange("p k r -> p (k r)"), AF.Silu, r=["sc"], w=["sc"])
    P.dma("sp", bms[:].rearrange("o l c -> o (l c)"), bm, w=["bms"], key="bms")
    P.memset(ones3[:], 1.0, w=["ones3"])
    for l in range(2):
        for k in range(KD):
            P.dma("sp", wms[:, k, :], wm[l, k * 128:(k + 1) * 128, :], w=["wms"], key="wms")
        for cc in range(3):
            cs = slice(cc * 512, (cc + 1) * 512)
            for k in range(KD):
                P.mm(ps[0:3, :], sc[:, k, :], wms[:, k, cs], start=(k == 0), stop=False, r=["sc", "wms"], w=["pm"])
            P.mm(ps[0:3, :], ones3[:, :], bms[:, l, cs], start=False, stop=True, r=["ones3", "bms"], w=["pm"])
            P.cp(res[:, cs], ps[0:3, :], r=["pm"], w=["res"])
        P.dma("sp", out[l], res[:], r=["res"], key="modo")
    P.out_dma.append("modo")
    return P.finalize()


def build_pre(NT, ctx_rows):
    P = Prog()
    R = NT * 128
    xin = P.din("xin", [R, D], F32)
    g = P.din("g", [D], F32)
    modn = P.din("modn", [2, 2, D], F32)
    hout = P.dout("hout", [R, D], BF16)
    bcB = P.sb("bcB", [128, D], F32)
    bcC = P.sb("bcC", [128, D], F32)
    xt = P.sb("xt", [128, D], F32)
    tmp = P.sb("tmp", [128, D], F32)
    hn = P.sb("hn", [128, D], BF16)
    sm = P.sb("sm", [128, 2], F32)

    def set_mod(kind):
        P.dma("sp", bcB[:], modn[kind, 1, :].partition_broadcast(128), w=["bcB"], key="bcB")
        P.dma("sp", bcC[:], g.partition_broadcast(128), w=["bcC"], key="bcC")
        P.stt(bcB[:], bcB[:], 1.0, bcC[:], ALU.add, ALU.mult, r=["bcB", "bcC"], w=["bcB"])
        P.dma("sp", bcC[:], modn[kind, 0, :].partition_broadcast(128), w=["bcC"], key="bcC")

    set_mod(0)
    for t in range(NT):
        if ctx_rows > 0 and t == NT - 1:
            set_mod(1)
        rows = slice(t * 128, (t + 1) * 128)
        P.dma("sp", xt[:], xin[rows, :], w=["xt"], key="xt")
        P.memset(sm[:, 0:1], 0.0, w=["sm"])
        P.act(tmp[:], xt[:], AF.Square, r=["xt"], w=["tmp", "sm"], accum_out=sm[:, 0:1])
        P.ts(sm[:, 0:1], sm[:, 0:1], 1.0 / D, EPS, ALU.mult, ALU.add, r=["sm"], w=["sm"])
        P.act(sm[:, 0:1], sm[:, 0:1], AF.Sqrt, r=["sm"], w=["sm"])
        P.op("dve", lambda e: e.reciprocal(sm[:, 0:1], sm[:, 0:1]), r=["sm"], w=["sm"])
        P.stt(tmp[:], xt[:], sm[:, 0:1], bcB[:], ALU.mult, ALU.mult, r=["xt", "sm", "bcB"], w=["tmp"])
        P.tt(hn[:], tmp[:], bcC[:], ALU.add, r=["tmp", "bcC"], w=["hn"])
        P.dma("sp", hout[rows, :], hn[:], r=["hn"], key="hout")
    P.out_dma.append("hout")
    return P.finalize()


D = 2048
KD = 16
NE = 32
EPS = 1e-6


def bc_row(ap_row, n):
    return ap_row.partition_broadcast(128)


def build_post(NT, DFF, last, ctx_rows):
    P = Prog()
    R = NT * 128
    KF = DFF // 128
    NCG = DFF // 256
    xin = P.din("xin", [R, D], F32)
    mix = P.din("mix", [R, D], BF16)
    modv = P.din("modv", [2, 6, D], F32)
    g2 = P.din("g2", [D], F32)
    wout = P.din("wout", [D, D], F32)
    wr = P.din("wr", [D, NE], F32)
    br = P.din("br", [1, NE], F32)
    wgu = P.din("wgu", [NE, D, 2 * DFF], F32)
    bgu = P.din("bgu", [NE, 2 * DFF], F32)
    wdn = P.din("wdn", [NE, DFF, D], F32)
    bdn = P.din("bdn", [NE, D], F32)
    identb_d = P.din("identb", [128, 128], BF16)
    identf_d = P.din("identf", [128, 128], F32)
    if not last:
        gn = P.din("gn", [D], F32)
        modn = P.din("modn", [2, 2, D], F32)
        hnext = P.dout("hnext", [R, D], BF16)
    xout = P.dout("xout", [R, D], F32)
    wo_s = P.dtmp("wo_s", [4, 128, KD * 512], BF16)
    wg_l = [P.dtmp("wg_s%d" % e, [NCG, 128, KD * 512], BF16) for e in range(NE)]
    wd_l = [P.dtmp("wd_s%d" % e, [4, 128, KF * 512], BF16) for e in range(NE)]
    x1_s = P.dtmp("x1_s", [R, D], F32)
    h2_s = P.dtmp("h2_s", [R, D], BF16)
    wbuf = P.sb("wbuf", [128, 2, KD * 512], BF16)
    BA = P.sb("BA", [128, 32768], BF16)
    FA = P.sb("FA", [128, 8192], F32)
    bcA = P.sb("bcA", [128, D], F32)
    bcB = P.sb("bcB", [128, D], F32)
    bcC = P.sb("bcC", [128, D], F32)
    xt3 = P.sb("xt3", [128, D], F32)
    hn = P.sb("hn", [128, D], BF16)
    Bg = P.sb("Bg", [NE, 2 * DFF], BF16)
    Bd = P.sb("Bd", [NE, D], F32)
    G = P.sb("G", [128, NT, NE], F32)
    GT = P.sb("GT", [NE, 512], F32)
    GTb = P.sb("GTb", [NE, 512], BF16)
    identb = P.sb("identb_s", [128, 128], BF16)
    identf = P.sb("identf_s", [128, 128], F32)
    wrs = P.sb("wrs", [128, KD, NE], F32)
    brs = P.sb("brs", [1, NE], F32)
    ones1 = P.sb("ones1", [1, 128], F32)
    sm = P.sb("sm", [128, 16], F32)
    mx8 = P.sb("mx8", [128, 8], F32)
    lg = P.sb("lg", [128, NE], F32)
    msk = P.sb("msk", [128, NE], F32)
    glu = P.sb("glu", [128, 256], F32)
    sig = P.sb("sig", [128, 256], F32)
    lin = P.sb("lin", [128, 256], F32)
    ptb = [P.ps("ptb%d" % i, [128, 1024], BF16) for i in range(2)]
    pf = [P.ps("pf%d" % i, [128, 512], F32) for i in range(2)]
    pg = [P.ps("pg%d" % i, [128, 512], F32) for i in range(2)]
    py = [P.ps("py%d" % i, [128, 512], F32) for i in range(2)]

    mixt = BA[:, 0:2048]
    mixT = BA[:, 2048:4096]
    h2b = BA[:, 4096:6144]
    xt = FA[:, 0:2048]
    h2 = FA[:, 2048:4096]
    h2T32 = FA[:, 4096:6144]
    P1KEYS = ["mixt", "mixT", "h2b", "xt", "h2", "h2T32"]

    P.dma("sp", identb[:], identb_d, w=["identb"], key="identb")
    P.dma("sp", identf[:], identf_d, w=["identf"], key="identf")
    P.dma("sp", wrs[:], wr.rearrange("(k p) e -> p k e", p=128), w=["wrs"], key="wrs")
    P.dma("sp", brs[:], br, w=["brs"], key="brs")
    P.dma("sp", Bd[:], bdn, w=["Bd"], key="Bd")
    P.dma("pool", Bg[:], bgu, w=["Bg"], key="Bg")
    P.memset(ones1[:], 1.0, w=["ones1"])
    P.memset(G[:], 0.0, w=["G"])

    stg = 0

    def cast_piece(dst, srcs):
        nonlocal stg
        s = stg % 2
        stg += 1
        key = "wp%d" % s
        for (lo, hi, src) in srcs:
            P.dma("pool", wbuf[:, s, :].rearrange("p (k c) -> p k c", c=512)[:, :src.shape[1], lo:hi], src,
                  w=[key], key=key + "l")
        P.dma("sp", dst, wbuf[:, s, 0:dst.shape[1]], r=[key], w=["wscr"], key=key + "s")

    for dc in range(4):
        cast_piece(wo_s[dc], [(0, 512, wout[:, dc * 512:(dc + 1) * 512].rearrange("(k p) c -> p k c", p=128))])
    for e in range(NE):
        for cg in range(NCG):
            cast_piece(wg_l[e][cg], [
                (0, 256, wgu[e, :, cg * 256:(cg + 1) * 256].rearrange("(k p) c -> p k c", p=128)),
                (256, 512, wgu[e, :, DFF + cg * 256:DFF + (cg + 1) * 256].rearrange("(k p) c -> p k c", p=128))])
        for dc in range(4):
            cast_piece(wd_l[e][dc], [(0, 512, wdn[e, :, dc * 512:(dc + 1) * 512].rearrange("(k p) c -> p k c", p=128))])

    wslot = [0]

    def load_piece(src, extra_w=()):
        s = wslot[0] % 2
        wslot[0] += 1
        key = "wp%d" % s
        P.dma("sp", wbuf[:, s, 0:src.shape[1]], src, r=["wscr"], w=[key] + list(extra_w), key=key + "l")
        return s, key

    def load_bc(tile, row_ap, key):
        P.dma("sp", tile[:], row_ap.partition_broadcast(128), w=[key], key=key)

    def set_mod(kind):
        load_bc(bcA, modv[kind, 2, :], "bcA")
        load_bc(bcB, modv[kind, 4, :], "bcB")
        load_bc(bcC, g2, "bcC")
        P.stt(bcB[:], bcB[:], 1.0, bcC[:], ALU.add, ALU.mult, r=["bcB", "bcC"], w=["bcB"])
        load_bc(bcC, modv[kind, 3, :], "bcC")

    def rstd_from(src, key_src, col):
        P.memset(sm[:, col:col + 1], 0.0, w=["sm%d" % col])
        P.act(h2[:], src, AF.Square, r=[key_src], w=["h2", "sm%d" % col], accum_out=sm[:, col:col + 1])
        P.ts(sm[:, col:col + 1], sm[:, col:col + 1], 1.0 / D, EPS, ALU.mult, ALU.add, r=["sm%d" % col], w=["sm%d" % col])
        P.act(sm[:, col:col + 1], sm[:, col:col + 1], AF.Sqrt, r=["sm%d" % col], w=["sm%d" % col])
        P.op("dve", lambda e: e.reciprocal(sm[:, col:col + 1], sm[:, col:col + 1]), r=["sm%d" % col], w=["sm%d" % col])

    set_mod(0)
    for t in range(NT):
        is_ctx = ctx_rows > 0 and t == NT - 1
        if is_ctx:
            set_mod(1)
        rows = slice(t * 128, (t + 1) * 128)
        P.dma("sp", mixt, mix[rows, :], w=["mixt"], key="mixt")
        P.dma("sp", xt, xin[rows, :], w=["xt"], key="xt")
        for j in range(2):
            for i in range(8):
                k = j * 8 + i
                P.tr(ptb[j][:, i * 128:(i + 1) * 128], mixt[:, k * 128:(k + 1) * 128], identb[:],
                     r=["mixt", "identb"], w=["ptb%d" % j])
            P.cp(mixT[:, j * 1024:(j + 1) * 1024], ptb[j][:], r=["ptb%d" % j], w=["mixT"], stream="act" if j else "dve")
        for dc in range(4):
            s, key = load_piece(wo_s[dc])
            wv = wbuf[:, s, :].rearrange("p (k c) -> p k c", c=512)
            ps = pf[dc % 2]
            pk = "pf%d" % (dc % 2)
            for k in range(KD):
                P.mm(ps[:], mixT[:, k * 128:(k + 1) * 128], wv[:, k, :], start=(k == 0), stop=(k == KD - 1),
                     r=["mixT", key], w=[pk])
            cs = slice(dc * 512, (dc + 1) * 512)
            P.tt(h2[:, cs], ps[:], bcA[:, cs], ALU.mult, r=[pk, "bcA"], w=["h2"])
            P.tt(xt[:, cs], h2[:, cs], xt[:, cs], ALU.add, r=["h2", "xt"], w=["xt"])
        P.dma("sp", x1_s[rows, :], xt, r=["xt"], w=["x1_s"], key="xt_st")
        rstd_from(xt, "xt", 0)
        P.stt(h2[:], xt, sm[:, 0:1], bcB[:], ALU.mult, ALU.mult, r=["xt", "sm0", "bcB"], w=["h2"])
        P.tt(h2[:], h2[:], bcC[:], ALU.add, r=["h2", "bcC"], w=["h2"])
        P.cp(h2b, h2[:], r=["h2"], w=["h2b"], stream="act")
        P.dma("sp", h2_s[rows, :], h2b, r=["h2b"], w=["h2_s"], key="h2b_st")
        if last and False:
            pass
        for j in range(4):
            for i in range(4):
                k = j * 4 + i
                P.tr(pf[j % 2][:, i * 128:(i + 1) * 128], h2[:, k * 128:(k + 1) * 128], identf[:],
                     r=["h2", "identf"], w=["pf%d" % (j % 2)])
            P.cp(h2T32[:, j * 512:(j + 1) * 512], pf[j % 2][:], r=["pf%d" % (j % 2)], w=["h2T32"],
                 stream="act" if j % 2 else "dve")
        for k in range(KD):
            P.mm(pg[0][:, 0:NE], h2T32[:, k * 128:(k + 1) * 128], wrs[:, k, :], start=(k == 0), stop=False,
                 r=["h2T32", "wrs"], w=["pg0"])
        P.mm(pg[0][:, 0:NE], ones1[:, :], brs[:, :], start=False, stop=True, r=["ones1", "brs"], w=["pg0"])
        P.cp(lg[:], pg[0][:, 0:NE], r=["pg0"], w=["lg"])
        P.op("dve", lambda e: e.max(mx8[:], lg[:]), r=["lg"], w=["mx8"])
        P.ts(msk[:], lg[:], mx8[:, 3:4], None, ALU.is_ge, r=["lg", "mx8"], w=["msk"])
        P.ts(sm[:, 2:3], mx8[:, 0:1], -1.0, None, ALU.mult, r=["mx8"], w=["sm2"])
        P.act(lg[:], lg[:], AF.Exp, r=["lg", "sm2"], w=["lg"], bias=sm[:, 2:3], scale=1.0)
        P.tt(lg[:], lg[:], msk[:], ALU.mult, r=["lg", "msk"], w=["lg"])
        P.op("dve", lambda e: e.reduce_sum(sm[:, 3:4], lg[:], AX.X), r=["lg"], w=["sm3"])
        P.op("dve", lambda e: e.reciprocal(sm[:, 3:4], sm[:, 3:4]), r=["sm3"], w=["sm3"])
        nv = ctx_rows if is_ctx else 128
        P.ts(G[0:nv, t, :], lg[0:nv, :], sm[0:nv, 3:4], None, ALU.mult, r=["lg", "sm3"], w=["G"])

    h2g = BA[:, 0:8192]
    h2T = BA[:, 8192:16384]
    actb = BA[:, 16384:24576]
    actT = BA[:, 24576:32768]
    acc = FA[:, 0:8192]
    first = True
    ngroups = (NT + 3) // 4
    for g in range(ngroups):
        t0 = g * 4
        nt = min(4, NT - t0)
        NTOK = nt * 128
        extra = P1KEYS if first else []
        for tt in range(nt):
            P.dma("sp", h2g[:, tt * 2048:(tt + 1) * 2048], h2_s[(t0 + tt) * 128:(t0 + tt + 1) * 128, :],
                  r=["h2_s"], w=["h2g"] + (extra if tt == 0 else []), key="h2g")
        h2Tv = h2T.rearrange("p (k t) -> p k t", t=512)
        for tt in range(nt):
            for j in range(2):
                for i in range(8):
                    k = j * 8 + i
                    P.tr(ptb[j][:, i * 128:(i + 1) * 128], h2g[:, tt * 2048 + k * 128: tt * 2048 + (k + 1) * 128],
                         identb[:], r=["h2g", "identb"], w=["ptb%d" % j])
                P.cp(h2Tv[:, j * 8:(j + 1) * 8, tt * 128:(tt + 1) * 128],
                     ptb[j][:].rearrange("p (k t) -> p k t", t=128), r=["ptb%d" % j],
                     w=["h2T"] + (extra if (tt == 0 and j == 0) else []), stream="act" if j else "dve")
        for tt in range(nt):
            P.tr(pf[0][0:NE, tt * 128:(tt + 1) * 128], G[:, t0 + tt, :], identf[:], r=["G", "identf"], w=["pf0"])
        P.cp(GT[:, 0:NTOK], pf[0][0:NE, 0:NTOK], r=["pf0"], w=["GT"])
        for tt in range(nt):
            for dc in range(4):
                ps = py[dc % 2]
                pk = "py%d" % (dc % 2)
                P.mm(ps[:], GT[:, tt * 128:(tt + 1) * 128], Bd[:, dc * 512:(dc + 1) * 512], r=["GT", "Bd"], w=[pk])
                P.cp(acc[:, tt * 2048 + dc * 512: tt * 2048 + (dc + 1) * 512], ps[:], r=[pk],
                     w=["acc"] + (extra if (tt == 0 and dc == 0) else []), stream="act" if dc % 2 else "dve")
        first = False
        for e in range(NE):
            for cg in range(NCG):
                s, key = load_piece(wg_l[e][cg])
                wv = wbuf[:, s, :].rearrange("p (k c) -> p k c", c=512)
                for tt in range(nt):
                    ps = pg[tt % 2]
                    pk = "pg%d" % (tt % 2)
                    for k in range(KD):
                        P.mm(ps[:], h2Tv[:, k, tt * 128:(tt + 1) * 128], wv[:, k, :], start=(k == 0), stop=False,
                             r=["h2T", key], w=[pk])
                    oh = identb[0:NE, e:e + 1].to_broadcast([NE, 128])
                    P.mm(ps[:, 0:256], oh, Bg[:, cg * 256:(cg + 1) * 256], start=False, stop=False,
                         r=["identb", "Bg"], w=[pk])
                    P.mm(ps[:, 256:512], oh, Bg[:, DFF + cg * 256:DFF + (cg + 1) * 256], start=False, stop=True,
                         r=["identb", "Bg"], w=[pk])
                    P.ts(glu[:], ps[:, 0:256], 7.0, None, ALU.min, r=[pk], w=["glu"])
                    P.act(sig[:], glu[:], AF.Sigmoid, r=["glu"], w=["sig"], scale=1.702)
                    P.ts(lin[:], ps[:, 256:512], 7.0, -7.0, ALU.min, ALU.max, r=[pk], w=["lin"], stream="pool" if False else "dve")
                    P.stt(glu[:], glu[:], G[:, t0 + tt, e:e + 1], sig[:], ALU.mult, ALU.mult, r=["glu", "G", "sig"], w=["glu"])
                    P.stt(actb[:, tt * 2048 + cg * 256: tt * 2048 + (cg + 1) * 256], lin[:], 1.0, glu[:], ALU.add, ALU.mult,
                          r=["lin", "glu"], w=["actb"])
            actTv = actT.rearrange("p (k t) -> p k t", t=512)
            for tt in range(nt):
                for j0 in range(0, KF, 8):
                    nj = min(8, KF - j0)
                    b = (j0 // 8) % 2
                    for i in range(nj):
                        kf = j0 + i
                        P.tr(ptb[b][:, i * 128:(i + 1) * 128], actb[:, tt * 2048 + kf * 128: tt * 2048 + (kf + 1) * 128],
                             identb[:], r=["actb", "identb"], w=["ptb%d" % b])
                    P.cp(actTv[:, j0:j0 + nj, tt * 128:(tt + 1) * 128],
                         ptb[b][:, 0:nj * 128].rearrange("p (k t) -> p k t", t=128), r=["ptb%d" % b], w=["actT"],
                         stream="act" if b else "dve")
            for dc in range(4):
                s, key = load_piece(wd_l[e][dc])
                wv = wbuf[:, s, 0:KF * 512].rearrange("p (k c) -> p k c", c=512)
                for tt in range(nt):
                    ps = py[tt % 2]
                    pk = "py%d" % (tt % 2)
                    for kf in range(KF):
                        P.mm(ps[:], actTv[:, kf, tt * 128:(tt + 1) * 128], wv[:, kf, :], start=(kf == 0), stop=(kf == KF - 1),
                             r=["actT", key], w=[pk])
                    a = acc[:, tt * 2048 + dc * 512: tt * 2048 + (dc + 1) * 512]
                    P.tt(a, ps[:], a, ALU.add, r=[pk, "acc"], w=["acc"], stream="pool" if False else "dve")
        for tt in range(nt):
            t = t0 + tt
            is_ctx = ctx_rows > 0 and t == NT - 1
            kind = 1 if is_ctx else 0
            rows = slice(t * 128, (t + 1) * 128)
            if tt == 0 or is_ctx:
                load_bc(bcA, modv[kind, 5, :], "bcA")
                if not last:
                    load_bc(bcB, modn[kind, 1, :], "bcB")
                    load_bc(bcC, gn, "bcC")
                    P.stt(bcB[:], bcB[:], 1.0, bcC[:], ALU.add, ALU.mult, r=["bcB", "bcC"], w=["bcB"])
                    load_bc(bcC, modn[kind, 0, :], "bcC")
            P.dma("sp", xt3[:], x1_s[rows, :], r=["x1_s"], w=["xt3"], key="xt3")
            a = acc[:, tt * 2048:(tt + 1) * 2048]
            P.tt(a, a, bcA[:], ALU.mult, r=["acc", "bcA"], w=["acc"])
            P.tt(xt3[:], xt3[:], a, ALU.add, r=["xt3", "acc"], w=["xt3"])
            P.dma("sp", xout[rows, :], xt3[:], r=["xt3"], key="xout")
            if not last:
                P.memset(sm[:, 5:6], 0.0, w=["sm5"])
                P.act(a, xt3[:], AF.Square, r=["xt3"], w=["acc", "sm5"], accum_out=sm[:, 5:6])
                P.ts(sm[:, 5:6], sm[:, 5:6], 1.0 / D, EPS, ALU.mult, ALU.add, r=["sm5"], w=["sm5"])
                P.act(sm[:, 5:6], sm[:, 5:6], AF.Sqrt, r=["sm5"], w=["sm5"])
                P.op("dve", lambda e: e.reciprocal(sm[:, 5:6], sm[:, 5:6]), r=["sm5"], w=["sm5"])
                P.stt(a, xt3[:], sm[:, 5:6], bcB[:], ALU.mult, ALU.mult, r=["xt3", "sm5", "bcB"], w=["acc"])
                P.tt(hn[:], a, bcC[:], ALU.add, r=["acc", "bcC"], w=["hn"])
                P.dma("sp", hnext[rows, :], hn[:], r=["hn"], key="hnext")
    P.out_dma.append("xout")
    if not last:
        P.out_dma.append("hnext")
    return P.finalize()


D = 2048
KD = 16


import os
GSTAGE = int(os.environ.get('GSTAGE', '99'))
GSUB = int(os.environ.get('GSUB', '9'))


def build_gla(NLT, NCT):
    P = Prog()
    TB = (NLT + NCT) * 128
    NC = 1568
    h = P.din("h", [TB, D], BF16)
    wc = P.din("wc", [D, NC], F32)
    w2 = P.din("w2", [2, 17, 256], F32)
    gnorm = P.din("gnorm", [512], F32)
    identb_d = P.din("identb", [128, 128], BF16)
    U_d = P.din("U", [2, 128, 128], F32)
    mixp = P.dout("mixp", [TB, 512], BF16)
    o_s = P.dtmp("o_s", [TB, 512], F32)

    W = P.sb("W", [128, KD, NC], BF16)
    identb = P.sb("identb_s", [128, 128], BF16)
    U = P.sb("U_s", [128, 2, 128], F32)
    w2s = P.sb("w2s", [17, 2, 256], F32)
    gbc = P.sb("gbc", [128, 512], F32)
    ones = P.sb("ones", [128, 128], F32)
    ht = P.sb("ht", [128, D], BF16)
    hT = P.sb("hT", [128, KD, 128], BF16)
    qT = P.sb("qT", [128, 2, 128], F32)
    kT = P.sb("kT", [128, 2, 128], F32)
    vb = P.sb("vb", [128, 512], BF16)
    r17 = P.sb("r17", [17, 128], F32)
    gk = P.sb("gk", [128, 256], F32)
    tmpa = P.sb("tmpa", [128, 256], F32)
    tmpb = P.sb("tmpb", [128, 256], F32)
    bTs = P.sb("bTs", [128, 2, 128], F32)
    ee = P.sb("ee", [128, 3, 2, 128], F32)
    qs = P.sb("qs", [128, 2, 128], BF16)
    ks = P.sb("ks", [128, 2, 128], BF16)
    qd = P.sb("qd", [128, 2, 128], BF16)
    attnT = P.sb("attnT", [128, 128], BF16)
    kd = P.sb("kd", [128, 256], BF16)
    S = P.sb("S", [128, 2, 512], F32)
    Sb = P.sb("Sb", [128, 2, 512], BF16)
    ot = P.sb("ot", [128, 512], F32)
    ot2 = P.sb("ot2", [128, 512], F32)
    go = P.sb("go", [128, 512], F32)
    yb = P.sb("yb", [128, 512], BF16)
    sm = P.sb("sm", [128, 8], F32)
    ptb = [P.ps("ptb%d" % i, [128, 1024], BF16) for i in range(2)]
    p_qk = P.ps("p_qk", [128, 512], F32)
    p_v = P.ps("p_v", [128, 512], F32)
    p_kr = P.ps("p_kr", [128, 512], F32)
    p_b = P.ps("p_b", [128, 512], F32)
    p_bT = P.ps("p_bT", [128, 512], F32)
    p_o = P.ps("p_o", [128, 512], F32)

    for k in range(KD):
        P.dma("pool", W[:, k, :], wc[k * 128:(k + 1) * 128, :], w=["W"], key="W")
    P.dma("sp", identb[:], identb_d, w=["identb"], key="identb")
    P.dma("sp", U[:], U_d.rearrange("d m c -> m d c"), w=["U"], key="U")
    P.dma("sp", w2s[:], w2.rearrange("d r c -> r d c"), w=["w2s"], key="w2s")
    P.dma("sp", gbc[:], gnorm.partition_broadcast(128), w=["gbc"], key="gbc")
    P.memset(ones[:], 1.0, w=["ones"])
    P.memset(r17[:], 1.0, w=["r17"])

    for d in range(2):
        P.memset(S[:], 0.0, w=["S"])
        P.memset(Sb[:], 0.0, w=["Sb"])
        last = 127 if d == 0 else 0
        order = [("c", i) for i in range(NCT)] + [("l", i) for i in range(NLT)]
        if d == 1:
            order = [("c", i) for i in reversed(range(NCT))] + [("l", i) for i in reversed(range(NLT))]
        for kind, i in order:
            r0 = (i if kind == "l" else NLT + i) * 128
            rows = slice(r0, r0 + 128)
            need_o = kind == "l"
            if GSTAGE < -3:
                continue
            P.dma("sp", ht[:], h[rows, :], w=["ht"], key="ht")
            for j in range(2):
                for q in range(8):
                    k = j * 8 + q
                    P.tr(ptb[j][:, q * 128:(q + 1) * 128], ht[:, k * 128:(k + 1) * 128], identb[:],
                         r=["ht", "identb"], w=["ptb%d" % j])
                P.cp(hT[:, j * 8:(j + 1) * 8, :], ptb[j][:].rearrange("p (k t) -> p k t", t=128), r=["ptb%d" % j],
                     w=["hT"], stream="act" if j else "dve")
            if GSUB < 1:
                continue
            for f in range(4):
                for k in range(KD):
                    P.mm(p_qk[:, f * 128:(f + 1) * 128], W[:, k, f * 128:(f + 1) * 128], hT[:, k, :],
                         start=(k == 0), stop=(k == KD - 1), r=["W", "hT"], w=["p_qk"])
            if GSUB < 2:
                continue
            P.ts(qT[:], p_qk[:, 0:256].rearrange("p (f t) -> p f t", t=128), 1.0 / 16, None, ALU.mult, r=["p_qk"], w=["qT"])
            P.cp(kT[:], p_qk[:, 256:512].rearrange("p (f t) -> p f t", t=128), r=["p_qk"], w=["kT"])
            if GSTAGE < -2:
                continue
            for k in range(KD):
                P.mm(p_v[:], hT[:, k, :], W[:, k, 512:1024], start=(k == 0), stop=(k == KD - 1), r=["W", "hT"], w=["p_v"])
            P.cp(vb[:], p_v[:], r=["p_v"], w=["vb"], stream="act")
            for k in range(KD):
                P.mm(p_kr[:, 0:256], hT[:, k, :], W[:, k, 256:512], start=(k == 0), stop=(k == KD - 1),
                     r=["W", "hT"], w=["p_kr"])
            if GSTAGE < -1:
                continue
            for k in range(KD):
                P.mm(p_bT[0:16, 384:512], W[:, k, 1536 + 16 * d:1552 + 16 * d], hT[:, k, :], start=(k == 0),
                     stop=(k == KD - 1), r=["W", "hT"], w=["p_rT"])
            P.cp(r17[0:16, :], p_bT[0:16, 384:512], r=["p_rT"], w=["r17"])
            P.mm(p_kr[:, 256:512], r17[:, :], w2s[:, d, :], r=["r17", "w2s"], w=["p_gk"])
            P.act(tmpa[:], p_kr[:, 256:512], AF.Exp, r=["p_gk"], w=["tmpa"], scale=-1.0)
            P.act(tmpa[:], tmpa[:], AF.Ln, r=["tmpa"], w=["tmpa"], bias=1.0)
            P.ts(gk[:], tmpa[:], -1.0 / 16, None, ALU.mult, r=["tmpa"], w=["gk"])
            if GSTAGE < 1:
                continue
            P.mm(p_b[:, 0:256], U[:, d, :], gk[:], r=["U", "gk"], w=["p_btok"])
            P.mm(p_b[:, 256:512], ones[:], gk[:], r=["ones", "gk"], w=["p_blast"])
            for f in range(2):
                P.mm(p_bT[:, f * 128:(f + 1) * 128], gk[:, f * 128:(f + 1) * 128], U[:, d, :], r=["gk", "U"], w=["p_bT"])
            P.cp(bTs[:], p_bT[:, 0:256].rearrange("p (f t) -> p f t", t=128), r=["p_bT"], w=["bTs"])
            P.ts(sm[:, 0:2], bTs[:, :, 64], -1.0, None, ALU.mult, r=["bTs"], w=["sm01"])
            for f in range(2):
                P.act(ee[:, 0, f, :], bTs[:, f, :], AF.Exp, r=["bTs", "sm01"], w=["ee"], bias=sm[:, f:f + 1], scale=1.0)
                P.act(ee[:, 1, f, :], bTs[:, f, :], AF.Exp, r=["bTs"], w=["ee"], bias=bTs[:, f, 64:65], scale=-1.0)
                P.act(ee[:, 2, f, :], bTs[:, f, :], AF.Exp, r=["bTs"], w=["ee"])
                P.act(sm[:, 2 + f:3 + f], bTs[:, f, last:last + 1], AF.Exp, r=["bTs"], w=["sm23"])
            P.tt(qs[:], qT[:], ee[:, 0], ALU.mult, r=["qT", "ee"], w=["qs"])
            P.tt(ks[:], kT[:], ee[:, 1], ALU.mult, r=["kT", "ee"], w=["ks"])
            P.tt(qd[:], qT[:], ee[:, 2], ALU.mult, r=["qT", "ee"], w=["qd"])
            if GSTAGE < 2:
                continue
            for f in range(2):
                P.mm(p_bT[:, 256:384], ks[:, f, :], qs[:, f, :], start=(f == 0), stop=(f == 1), r=["ks", "qs"], w=["p_at"])
            P.tt(attnT[:], p_bT[:, 256:384], U[:, d, :], ALU.mult, r=["p_at", "U"], w=["attnT"])
            P.cp(tmpb[:], p_b[:, 256:512], r=["p_blast"], w=["tmpb"], stream="act")
            P.tt(tmpb[:], tmpb[:], p_b[:, 0:256], ALU.subtract, r=["tmpb", "p_btok"], w=["tmpb"])
            P.act(tmpb[:], tmpb[:], AF.Exp, r=["tmpb"], w=["tmpb"])
            P.tt(kd[:], p_kr[:, 0:256], tmpb[:], ALU.mult, r=["p_kr", "tmpb"], w=["kd"])
            if GSTAGE < 3:
                continue
            if need_o:
                P.mm(p_o[:], attnT[:], vb[:], start=True, stop=False, r=["attnT", "vb"], w=["p_o"])
                for f in range(2):
                    P.mm(p_o[:], qd[:, f, :], Sb[:, f, :], start=False, stop=(f == 1), r=["qd", "Sb"], w=["p_o"])
                if d == 0:
                    P.cp(ot[:], p_o[:], r=["p_o"], w=["ot"])
                    P.dma("sp", o_s[rows, :], ot[:], r=["ot"], w=["o_s"], key="ot_st")
                else:
                    P.dma("sp", ot2[:], o_s[rows, :], r=["o_s"], w=["ot2"], key="ot2")
                    P.tt(ot[:], p_o[:], ot2[:], ALU.add, r=["p_o", "ot2"], w=["ot"])
                    for k in range(KD):
                        P.mm(p_v[:], hT[:, k, :], W[:, k, 1024:1536], start=(k == 0), stop=(k == KD - 1),
                             r=["W", "hT"], w=["p_v"])
                    P.act(go[:], p_v[:], AF.Silu, r=["p_v"], w=["go"])
                    P.memset(sm[:, 4:5], 0.0, w=["sm4"])
                    P.act(ot2[:], ot[:], AF.Square, r=["ot"], w=["ot2", "sm4"], accum_out=sm[:, 4:5])
                    P.ts(sm[:, 4:5], sm[:, 4:5], 1.0 / 512, 1e-6, ALU.mult, ALU.add, r=["sm4"], w=["sm4"])
                    P.act(sm[:, 4:5], sm[:, 4:5], AF.Sqrt, r=["sm4"], w=["sm4"])
                    P.op("dve", lambda e: e.reciprocal(sm[:, 4:5], sm[:, 4:5]), r=["sm4"], w=["sm4"])
                    P.stt(ot[:], ot[:], sm[:, 4:5], gbc[:], ALU.mult, ALU.mult, r=["ot", "sm4", "gbc"], w=["ot"])
                    P.tt(yb[:], ot[:], go[:], ALU.mult, r=["ot", "go"], w=["yb"])
                    P.dma("sp", mixp[rows, :], yb[:], r=["yb"], key="mixp")
            if GSTAGE < 4:
                continue
            for f in range(2):
                P.mm(p_o[:], kd[:, f * 128:(f + 1) * 128], vb[:], r=["kd", "vb"], w=["p_o"])
                P.stt(S[:, f, :], S[:, f, :], sm[:, 2 + f:3 + f], p_o[:], ALU.mult, ALU.add, r=["S", "sm23", "p_o"], w=["S"])
                P.cp(Sb[:, f, :], S[:, f, :], r=["S"], w=["Sb"], stream="act")
    P.memset(yb[:], 0.0, w=["yb"])
    for i in range(NCT):
        r0 = (NLT + i) * 128
        P.dma("sp", mixp[r0:r0 + 128, :], yb[:], r=["yb"], key="mixp")
    P.out_dma.append("mixp")
    return P.finalize()


D = 2048
KD = 16


import os
STAGE = int(os.environ.get('STAGE', '9'))
VAR = os.environ.get('VAR', '')
SUB = float(os.environ.get('SUB', '9'))


def build_mix0(NLT, NCT):
    P = Prog()
    P.keymap = {"B0a": "B0", "B0x": "B0", "B1z": "B1", "B2b": "B2", "B2c": "B2", "B2d": "B2", "B3a": "B3", "B3b": "B3",
                "B3c": "B3", "B4c": "B4", "B4d": "B4", "B5a": "B5", "B5b": "B5", "B5c": "B5", "B5d": "B5"}
    HKEYS = ["gbc", "gct", "egs0", "egs1", "egs2", "egs3", "t1", "t2", "Dcm", "DTs", "DTi", "kb", "kbT", "Mb0", "Mb1", "MTb0", "MTb1",
             "attnT", "R", "wT", "vn", "o2", "kdk", "Sst", "od", "B2", "B2b", "B2c", "B2d", "B3a", "B3b", "B3c", "B4", "B4c", "B4d",
             "B5a", "B5b", "B5c", "B5d"]
    P.ksuf_keys = set(HKEYS)
    for kk_ in ("B2", "B2b", "B2c", "B2d"):
        P.keymap[kk_ + "_0"] = "B2"; P.keymap[kk_ + "_1"] = "B0"
    for kk_ in ("B3a", "B3b", "B3c"):
        P.keymap[kk_ + "_0"] = "B3"; P.keymap[kk_ + "_1"] = "B1"
    for kk_ in ("B4", "B4c", "B4d"):
        P.keymap[kk_ + "_0"] = "B4"; P.keymap[kk_ + "_1"] = "ptb0"
    for kk_ in ("B5a", "B5b", "B5c", "B5d"):
        P.keymap[kk_ + "_0"] = "B5"; P.keymap[kk_ + "_1"] = "ptb1"
    TB = (NLT + NCT) * 128
    S = NLT * 128
    CT = NCT * 128
    TBP = S + 4 + CT + 4
    NC = 1416
    NTT = NLT + NCT
    h = P.din("h", [TB, D], BF16)
    wc = P.din("wc", [D, NC], F32)
    qkg = P.din("qkg", [128, 320], F32)
    sinks = P.din("sinks", [128, 4], F32)
    cw = P.din("cw", [128, 30], F32)
    gpar = P.din("gpar", [128, 8], F32)
    dnn = P.din("dnn", [128, 128], F32)
    M_d = P.din("M", [128, 512 + NLT * 64], F32)
    identb_d = P.din("identb", [128, 128], BF16)
    identf_d = P.din("identf", [128, 128], F32)
    mixp = P.dout("mixp", [TB, 512], BF16)
    xs = P.dtmp("xs", [6, 128, TBP], F32)
    zs = P.dtmp("zs", [TB, 256], F32)
    gs = P.dtmp("gs", [TB, 8], F32)
    o_s = P.dtmp("o_s", [TB, 256], F32)

    W = P.sb("W", [128, KD, NC], BF16)
    identb = P.sb("identb_s", [128, 128], BF16)
    identf = P.sb("identf_s", [128, 128], F32)
    Mm = P.sb("Mm", [128, 4, 128], F32)
    gq = P.sb("gq", [128, 320], F32)
    esink = P.sb("esink", [128, 4], F32)
    cws = P.sb("cws", [128, 6, 5], F32)
    gp = P.sb("gp", [128, 8], F32)
    dnbc = P.sb("dnbc", [128, 128], F32)
    ones = P.sb("ones", [128, 128], F32)
    zero6 = P.sb("zero6", [128, 6, 2], F32)
    ht = P.sb("ht", [128, D], BF16)
    hT = P.sb("hT", [128, KD, 128], BF16)
    qkv = P.sb("qkv", [128, 384], F32)
    sq = P.sb("sq", [128, 320], F32)
    ss = P.sb("ss", [128, 8], F32)
    qkn = P.sb("qkn", [128, 320], F32)
    qkr = P.sb("qkr", [128, 320], F32)
    rt = P.sb("ropetmp", [128, 64], F32)
    rpa = P.sb("rpa", [128, NLT, 64], F32)
    qrb = P.sb("qrb", [128, 320], BF16)
    qTr = [P.sb("qTr%d" % i, [64, 512], BF16) for i in range(2)]
    kTa = P.sb("kTa", [64, NTT * 128], BF16)
    V1 = P.sb("V1", [128, NTT, 66], BF16)
    xst = P.sb("xst", [128, 6, 128], F32)
    zt = P.sb("zt", [128, 256], F32)
    gts = P.sb("gts", [128, 8], F32)
    gtm = P.sb("gtm", [128, 4], F32)
    eT = P.sb("eT", [128, 5, 512], BF16)
    den = P.sb("den", [128, 4], F32)
    aout = P.sb("aout", [128, 256], BF16)
    xw = P.sb("xw", [128, 6, 132], F32)
    yc = P.sb("yc", [128, 6, 128], F32)
    sq4 = P.sb("sq4", [128, 4, 128], F32)
    rs4 = P.sb("rs4", [128, 4, 128], F32)
    qk = P.sb("qk", [128, 4, 128], F32)
    ktv = P.sb("ktv", [128, 4, 128], F32)
    gt8 = P.sb("gt8", [128, 8], F32)
    gbc = P.sb("gbc", [128, 128], F32)
    gct = P.sb("gct", [128, 1], F32)
    egs = P.sb("egs", [128, 4], F32)
    t1 = P.sb("t1", [128, 128], F32)
    t2 = P.sb("t2", [128, 128], F32)
    Dcm = P.sb("Dcm", [128, 128], F32)
    DTs = P.sb("DTs", [128, 128], F32)
    DTi = P.sb("DTi", [128, 128], F32)
    kb = P.sb("kb", [128, 128], F32)
    kbT = P.sb("kbT", [128, 128], F32)
    Mb = [P.sb("Mb%d" % i, [128, 128], F32) for i in range(2)]
    MTb = [P.sb("MTb%d" % i, [128, 128], F32) for i in range(2)]
    attnT = P.sb("attnT", [128, 128], F32)
    R = P.sb("R", [128, 256], F32)
    wT = P.sb("wT", [128, 128], F32)
    vn = P.sb("vn", [128, 128], F32)
    o2 = P.sb("o2", [128, 128], F32)
    od = P.sb("od", [128, 256], F32)
    oprev = P.sb("oprev", [128, 256], F32)
    kdk = P.sb("kdk", [128, 128], F32)
    Sst = P.sb("Sst", [128, 2, 128], F32)
    yb = P.sb("yb", [128, 256], BF16)

    HT_NAMES = ["gbc", "gct", "egs", "t1", "t2", "Dcm", "DTs", "DTi", "kb", "kbT", "Mb0", "Mb1", "MTb0", "MTb1", "attnT", "R",
                "wT", "vn", "o2", "kdk"]
    HT0 = dict(gbc=gbc, gct=gct, egs=egs, t1=t1, t2=t2, Dcm=Dcm, DTs=DTs, DTi=DTi, kb=kb, kbT=kbT, Mb0=Mb[0], Mb1=Mb[1],
               MTb0=MTb[0], MTb1=MTb[1], attnT=attnT, R=R, wT=wT, vn=vn, o2=o2, kdk=kdk)
    HT1 = {}
    for nm_ in HT_NAMES:
        shp_ = [128, 256] if nm_ == "R" else ([128, 1] if nm_ == "gct" else ([128, 4] if nm_ == "egs" else [128, 128]))
        HT1[nm_] = P.sb(nm_ + "_h1", shp_, F32)
    ptb = [P.ps("ptb%d" % i, [128, 1024], BF16) for i in range(2)]
    B0 = P.ps("B0", [128, 512], F32)
    B1 = P.ps("B1", [128, 512], F32)
    B2 = P.ps("B2", [128, 512], F32)
    B3 = P.ps("B3", [128, 512], F32)
    B4 = P.ps("B4", [128, 512], F32)
    B5 = P.ps("B5", [128, 512], F32)
    PB2, PB3, PB4, PB5 = B2, B3, B4, B5
    ptbf = [ptb[i][:].bitcast(F32) for i in range(2)]

    for k in range(KD):
        P.dma("pool", W[:, k, :], wc[k * 128:(k + 1) * 128, :], w=["W"], key="W")
    P.dma("sp", identb[:], identb_d, w=["identb"], key="identb")
    P.dma("sp", identf[:], identf_d, w=["identf"], key="identf")
    P.dma("sp", Mm[:].rearrange("p d c -> p (d c)"), M_d[:, 0:512], w=["Mm"], key="Mm")
    P.dma("sp", gq[:], qkg, w=["gq"], key="gq")
    P.dma("sp", esink[:], sinks, w=["esink"], key="esink")
    P.act(esink[:], esink[:], AF.Exp, r=["esink"], w=["esink"])
    P.dma("sp", cws[:].rearrange("p c j -> p (c j)"), cw, w=["cws"], key="cws")
    P.dma("sp", gp[:], gpar, w=["gp"], key="gp")
    P.act(gp[:, 0:4], gp[:, 0:4], AF.Exp, r=["gp"], w=["gp"])
    P.ts(gp[:, 0:4], gp[:, 0:4], -1.0, None, ALU.mult, r=["gp"], w=["gp"])
    P.dma("sp", dnbc[:], dnn, w=["dnbc"], key="dnbc")
    P.dma("sp", rpa[:].rearrange("p n c -> p (n c)"), M_d[:, 512:512 + NLT * 64], w=["rp"], key="rp")
    P.memset(ones[:], 1.0, w=["ones"])
    P.memset(zero6[:], 0.0, w=["zero6"])
    P.memset(V1[:], 1.0, w=["V1"])

    def tile_pos(kind, i):
        if kind == "l":
            return i * 128, 2 + i * 128, i
        return S + i * 128, S + 4 + 2 + i * 128, NLT + i

    def proj_tile(kind, i, qslot):
        if STAGE < 1:
            return
        r0, col0, gt = tile_pos(kind, i)
        rows = slice(r0, r0 + 128)
        P.dma("sp", ht[:], h[rows, :], r=["identb", "identf", "Mm", "gq", "esink", "cws", "gp", "dnbc", "rp"], w=["ht"], key="ht")
        for j in range(2):
            for q in range(8):
                k = j * 8 + q
                P.tr(ptb[j][:, q * 128:(q + 1) * 128], ht[:, k * 128:(k + 1) * 128], identb[:],
                     r=["ht", "identb"], w=["ptb%d" % j])
            P.cp(hT[:, j * 8:(j + 1) * 8, :], ptb[j][:].rearrange("p (k t) -> p k t", t=128), r=["ptb%d" % j],
                 w=["hT"], stream="act" if j else "dve")
        if SUB < 2:
            return
        for k in range(KD):
            P.mm(B0[:, 0:384], hT[:, k, :], W[:, k, 0:384], start=(k == 0), stop=(k == KD - 1), r=["W", "hT"], w=["B0a"])
        P.cp(qkv[:], B0[:, 0:384], r=["B0a"], w=["qkv"])
        P.tt(sq[:], qkv[:, 0:320], qkv[:, 0:320], ALU.mult, r=["qkv"], w=["sq"])
        P.op("dve", lambda e: e.reduce_sum(ss[:, 0:5], sq[:].rearrange("p (h f) -> p h f", f=64), AX.X), r=["sq"], w=["ss"])
        P.ts(ss[:, 0:5], ss[:, 0:5], 1.0 / 64, 1e-6, ALU.mult, ALU.add, r=["ss"], w=["ss"])
        P.act(ss[:, 0:5], ss[:, 0:5], AF.Sqrt, r=["ss"], w=["ss"])
        P.op("dve", lambda e: e.reciprocal(ss[:, 0:5], ss[:, 0:5]), r=["ss"], w=["ss"])
        for hh in range(5):
            cs = slice(hh * 64, (hh + 1) * 64)
            P.stt(qkn[:, cs], qkv[:, cs], ss[:, hh:hh + 1], gq[:, cs], ALU.mult, ALU.mult, r=["qkv", "ss", "gq"], w=["qkn"])
        if SUB < 3:
            return
        src = qkn
        if kind == "l":
            rp = rpa[:, i, :]
            if VAR == 'xdma':
                P.dma("sp", qkr[:, 0:64], M_d[0, :, 0:64], w=["qkr"], key="xd")
            for hh in range(0 if VAR == "noops" else 5):
                for a in range(2):
                    b0 = hh * 64 + a * 32
                    x1 = qkn[:, b0:b0 + 16]
                    x2 = qkn[:, b0 + 16:b0 + 32]
                    cv = rp[:, a * 16:(a + 1) * 16]
                    sv = rp[:, 32 + a * 16:32 + (a + 1) * 16]
                    P.tt(rt[:, 0:16], x1, cv, ALU.mult, r=["qkn", "rp"], w=["rt"])
                    P.tt(rt[:, 16:32], x2, sv, ALU.mult, r=["qkn", "rp"], w=["rt"])
                    P.tt(rt[:, 32:48], x2, cv, ALU.mult, r=["qkn", "rp"], w=["rt"])
                    P.tt(rt[:, 48:64], x1, sv, ALU.mult, r=["qkn", "rp"], w=["rt"])
                    P.tt(qkr[:, b0:b0 + 16], rt[:, 0:16], rt[:, 16:32], ALU.subtract, r=["rt"], w=["qkr"])
                    P.tt(qkr[:, b0 + 16:b0 + 32], rt[:, 32:48], rt[:, 48:64], ALU.add, r=["rt"], w=["qkr"])
            src = qkr
        if SUB < 4.1:
            return
        P.cp(qrb[:], src[:], r=["qkn", "qkr"], w=["qrb"], stream="act")
        for hh in range(5):
            P.tr(ptb[1][0:64, hh * 128:(hh + 1) * 128], qrb[:, hh * 64:(hh + 1) * 64], identb[:],
                 r=["qrb", "identb"], w=["ptb1"])
        if SUB < 4.2:
            return
        P.cp(qTr[qslot][:, :], ptb[1][0:64, 0:512], r=["ptb1"], w=["qT%d" % qslot])
        if SUB < 4.3:
            return
        P.cp(kTa[:, gt * 128:(gt + 1) * 128], ptb[1][0:64, 512:640], r=["ptb1"], w=["kTa"])
        if SUB < 4.4:
            return
        P.cp(V1[:, gt, 0:64], qkv[:, 320:384], r=["qkv"], w=["V1"])
        if SUB < 5:
            return
        for c in range(6):
            for k in range(KD):
                P.mm(B0[:, 384:512], W[:, k, 384 + c * 128:384 + (c + 1) * 128], hT[:, k, :], start=(k == 0),
                     stop=(k == KD - 1), r=["W", "hT"], w=["B0x"])
            P.cp(xst[:, c, :], B0[:, 384:512], r=["B0x"], w=["xst"], stream="act" if c % 2 else "dve")
        P.dma("sp", xs[:, :, col0:col0 + 128].rearrange("c p t -> p c t"), xst[:], r=["xst"], w=["xs"], key="xst_st")
        if SUB < 6:
            return
        for k in range(KD):
            P.mm(B1[:, 0:264], hT[:, k, :], W[:, k, 1152:1416], start=(k == 0), stop=(k == KD - 1), r=["W", "hT"], w=["B1z"])
        P.act(zt[:], B1[:, 0:256], AF.Silu, r=["B1z"], w=["zt"])
        P.dma("sp", zs[rows, :], zt[:], r=["zt"], w=["zs"], key="zt_st")
        P.tt(gtm[:], B1[:, 256:260], gp[:, 4:8], ALU.add, r=["B1z", "gp"], w=["gtm"])
        P.act(gtm[:], gtm[:], AF.Exp, r=["gtm"], w=["gtm"])
        P.act(gtm[:], gtm[:], AF.Ln, r=["gtm"], w=["gtm"], bias=1.0)
        P.tt(gts[:, 0:4], gtm[:], gp[:, 0:4], ALU.mult, r=["gtm", "gp"], w=["gts"])
        P.act(gts[:, 4:8], B1[:, 260:264], AF.Sigmoid, r=["B1z"], w=["gts"])
        P.dma("sp", gs[rows, :], gts[:], r=["gts"], w=["gs"], key="gts_st")

    def attn_block(kind, i, qslot):
        if STAGE < 2:
            return
        r0, col0, gt = tile_pos(kind, i)
        kbs = []
        if kind == "l":
            if i > 0:
                kbs.append((i - 1, 1))
            kbs.append((i, None))
            if i < NLT - 1:
                kbs.append((i + 1, 0))
        for c in range(NCT):
            kbs.append((NLT + c, None))
        for idx, (kg, mk) in enumerate(kbs):
            ps = B2 if idx % 2 == 0 else B3
            pk = "B2" if idx % 2 == 0 else "B3"
            P.mm(ps[:], kTa[:, kg * 128:(kg + 1) * 128], qTr[qslot][:, :], r=["kTa", "qT%d" % qslot], w=[pk])
            P.act(eT[:, idx, :], ps[:], AF.Exp, r=[pk], w=["eT"], scale=0.125)
            if mk is not None:
                for hh in range(4):
                    P.tt(eT[:, idx, hh * 128:(hh + 1) * 128], eT[:, idx, hh * 128:(hh + 1) * 128], Mm[:, mk, :], ALU.mult,
                         r=["eT", "Mm"], w=["eT"])
        for hh in range(4):
            for idx, (kg, mk) in enumerate(kbs):
                P.mm(B4[:, hh * 128:hh * 128 + 65], eT[:, idx, hh * 128:(hh + 1) * 128], V1[:, kg, 0:65], start=(idx == 0),
                     stop=(idx == len(kbs) - 1), r=["eT", "V1"], w=["B4"])
        pv = B4[:, :].rearrange("p (h f) -> p h f", f=128)
        P.tt(den[:], pv[:, :, 64], esink[:], ALU.add, r=["B4", "esink"], w=["den"])
        P.op("dve", lambda e: e.reciprocal(den[:], den[:]), r=["den"], w=["den"])
        for hh in range(4):
            P.ts(aout[:, hh * 64:(hh + 1) * 64], B4[:, hh * 128:hh * 128 + 64], den[:, hh:hh + 1], None, ALU.mult,
                 r=["B4", "den"], w=["aout"])
        P.dma("sp", mixp[r0:r0 + 128, 0:256], aout[:], r=["aout"], key="mixp")

    for c in range(NCT):
        proj_tile("c", c, c % 2)
    for c in range(NCT):
        attn_block("c", c, c % 2)
    for i in range(NLT):
        proj_tile("l", i, i % 2)
        if i >= 1:
            attn_block("l", i - 1, (i - 1) % 2)
    attn_block("l", NLT - 1, (NLT - 1) % 2)

    PSK = ["B0a", "B0x", "B1z", "B2", "B3", "B4", "B2b", "B2c", "B2d", "B3a", "B3b", "B3c", "B4c", "B4d",
           "B5a", "B5b", "B5c", "B5d"]
    P.memset(ss[:, 7:8], 0.0, w=PSK + ["ss7"])
    for d in range(2 if STAGE >= 3 else 0):
        P.memset(Sst[:], 0.0, w=["Sst_0", "Sst_1"])
        iMin = 0 if d == 0 else 1
        iMst = 2 if d == 0 else 3
        iLst = 3 if d == 0 else 2
        last = 127 if d == 0 else 0
        order = [("c", i) for i in range(NCT)] + [("l", i) for i in range(NLT)]
        if d == 1:
            order = [("c", i) for i in reversed(range(NCT))] + [("l", i) for i in reversed(range(NLT))]
        for kind, i in order:
            r0, col0, gt = tile_pos(kind, i)
            rows = slice(r0, r0 + 128)
            nlast = (NLT if kind == "l" else NCT) - 1
            lo = 2 if i == 0 else 0
            hi = 130 if i == nlast else 132
            if lo:
                P.memset(xw[:, :, 0:2], 0.0, w=["xw"])
            if hi < 132:
                P.memset(xw[:, :, 130:132], 0.0, w=["xw"])
            P.dma("sp", xw[:, :, lo:hi], xs[:, :, col0 - 2 + lo:col0 - 2 + hi].rearrange("c p t -> p c t"), r=["xs"], w=["xw"], key="xw")
            P.dma("sp", gt8[:], gs[rows, :], r=["gs"], w=["gt8"], key="gt8")
            for c in range(6):
                P.ts(yc[:, c, :], xw[:, c, 0:128], cws[:, c, 0:1], None, ALU.mult, r=["xw", "cws"], w=["yc"])
                for j in range(1, 5):
                    P.stt(yc[:, c, :], xw[:, c, j:j + 128], cws[:, c, j:j + 1], yc[:, c, :], ALU.mult, ALU.add,
                          r=["xw", "cws", "yc"], w=["yc"])
            P.act(yc[:], yc[:], AF.Silu, r=["yc"], w=["yc"])
            P.tt(sq4[:], yc[:, 0:4, :], yc[:, 0:4, :], ALU.mult, r=["yc"], w=["sq4"])
            P.mm(B0[:], ones[:], sq4[:].rearrange("p c t -> p (c t)"), r=["ones", "sq4"], w=["B0a", "B0x"])
            P.ts(rs4[:].rearrange("p c t -> p (c t)"), B0[:], 1e-6, None, ALU.add, r=["B0a"], w=["rs4"])
            P.act(rs4[:], rs4[:], AF.Sqrt, r=["rs4"], w=["rs4"])
            P.op("dve", lambda e: e.reciprocal(rs4[:], rs4[:]), r=["rs4"], w=["rs4"])
            P.ts(rs4[:, 0:2, :], rs4[:, 0:2, :], 128 ** -0.5, None, ALU.mult, r=["rs4"], w=["rs4"])
            P.tt(qk[:], yc[:, 0:4, :], rs4[:], ALU.mult, r=["yc", "rs4"], w=["qk"])
            for hh in range(2):
                P.tr(B1[:, hh * 128:(hh + 1) * 128], qk[:, 2 + hh, :], identf[:], r=["qk", "identf"], w=["B1z"])
                P.tr(B1[:, 256 + hh * 128:256 + (hh + 1) * 128], yc[:, 4 + hh, :], identf[:], r=["yc", "identf"], w=["B1z"])
            P.cp(ktv[:].rearrange("p c t -> p (c t)"), B1[:], r=["B1z"], w=["ktv"])
            segs = []
            for hh in range(2 if STAGE >= 4 else 0):
                seg_start = len(P.ops)
                P.ksuf = "_%d" % hh
                HB = HT0 if hh == 0 else HT1
                gbc, gct, egs, t1, t2, Dcm, DTs, DTi, kb, kbT = (HB[n_] for n_ in HT_NAMES[:10])
                Mb = [HB["Mb0"], HB["Mb1"]]
                MTb = [HB["MTb0"], HB["MTb1"]]
                attnT, R, wT, vn, o2, kdk = (HB[n_] for n_ in HT_NAMES[14:])
                if hh == 0:
                    B2, B3, B4, B5 = PB2, PB3, PB4, PB5
                else:
                    B2, B3, B4, B5 = B0, B1, ptbf[0], ptbf[1]
                g = gt8[:, 2 * d + hh:2 * d + hh + 1]
                beta = gt8[:, 4 + 2 * d + hh:4 + 2 * d + hh + 1]
                qT = qk[:, hh, :]
                kT = qk[:, 2 + hh, :]
                k_tok = ktv[:, hh, :]
                v_tok = ktv[:, 2 + hh, :]
                P.ts(gbc[:], ones[:], g, None, ALU.mult, r=["ones", "gt8"], w=["gbc"])
                P.mm(B2[:, 0:128], gbc[:], Mm[:, iMin, :], r=["gbc", "Mm"], w=["B2"])
                P.mm(B3[:, 256:384], Mm[:, iMin, :], gbc[:], r=["Mm", "gbc"], w=["B3c"])
                P.cp(gct[:], B3[:, 256:257], r=["B3c"], w=["gct"])
                P.act(egs[:, 0:1], gct[:, 0:1], AF.Exp, r=["gct"], w=["egs0"])
                P.cp(egs[:, 3:4], B2[:, last:last + 1], r=["B2"], w=["egs3"])
                P.act(egs[:, 1:2], egs[:, 3:4], AF.Exp, r=["egs3"], w=["egs1"])
                P.ts(egs[:, 2:3], B2[:, last:last + 1], gct[:, 0:1], None, ALU.subtract, r=["B2", "gct"], w=["egs2"])
                P.act(egs[:, 2:3], egs[:, 2:3], AF.Exp, r=["egs2"], w=["egs2"])
                P.ts(t1[:], B2[:, 0:128], gct[:, 0:1], 0.0, ALU.subtract, ALU.max, r=["B2", "gct"], w=["t1"])
                P.act(Dcm[:], t1[:], AF.Exp, r=["t1"], w=["Dcm"], scale=-1.0)
                P.tt(Dcm[:], Dcm[:], Mm[:, iLst, :], ALU.mult, r=["Dcm", "Mm"], w=["Dcm"])
                P.ts(t2[:], B2[:, 0:128], gct[:, 0:1], 0.0, ALU.subtract, ALU.min, r=["B2", "gct"], w=["t2"])
                P.act(t2[:], t2[:], AF.Exp, r=["t2"], w=["t2"])
                P.tt(DTs[:], t2[:], Mm[:, iMst, :], ALU.mult, r=["t2", "Mm"], w=["DTs"])
                P.tt(DTi[:], t2[:], Mm[:, iMin, :], ALU.mult, r=["t2", "Mm"], w=["DTi"])
                P.ts(kb[:], k_tok, beta, None, ALU.mult, r=["ktv", "gt8"], w=["kb"])
                P.tr(B3[:, 0:128], kb[:], identf[:], r=["kb", "identf"], w=["B3a"])
                P.cp(kbT[:], B3[:, 0:128], r=["B3a"], w=["kbT"])
                P.mm(B2[:, 128:256], kbT[:], kT, r=["kbT", "qk"], w=["B2b"])
                P.mm(B2[:, 256:384], kT, kbT[:], r=["kbT", "qk"], w=["B2c"])
                P.mm(B2[:, 384:512], kT, qT, r=["qk"], w=["B2d"])
                P.stt(Mb[0][:], B2[:, 128:256], -1.0, Dcm[:], ALU.mult, ALU.mult, r=["B2b", "Dcm"], w=["Mb0"])
                P.stt(MTb[0][:], B2[:, 256:384], -1.0, DTs[:], ALU.mult, ALU.mult, r=["B2c", "DTs"], w=["MTb0"])
                P.tt(attnT[:], B2[:, 384:512], DTi[:], ALU.mult, r=["B2d", "DTi"], w=["attnT"])
                P.ts(R[:, 0:128], v_tok, beta, None, ALU.mult, r=["ktv", "gt8"], w=["R"])
                P.ts(R[:, 128:256], kb[:], egs[:, 0:1], None, ALU.mult, r=["kb", "egs0"], w=["R"])
                cur = 0
                for it in range(7):
                    P.mm(B4[:, 0:256], MTb[cur][:], R[:], r=["MTb%d" % cur, "R"], w=["B4"])
                    if it < 6:
                        P.mm(B5[:, 0:128], MTb[cur][:], Mb[cur][:], r=["MTb%d" % cur, "Mb%d" % cur], w=["B5a"])
                        P.mm(B5[:, 128:256], Mb[cur][:], MTb[cur][:], r=["MTb%d" % cur, "Mb%d" % cur], w=["B5b"])
                    P.tt(R[:], B4[:, 0:256], R[:], ALU.add, r=["B4", "R"], w=["R"])
                    if it < 6:
                        nx = 1 - cur
                        P.cp(Mb[nx][:], B5[:, 0:128], r=["B5a"], w=["Mb%d" % nx])
                        P.cp(MTb[nx][:], B5[:, 128:256], r=["B5b"], w=["MTb%d" % nx])
                        cur = nx
                P.tr(B3[:, 128:256], R[:, 128:256], identf[:], r=["R", "identf"], w=["B3b"])
                P.cp(wT[:], B3[:, 128:256], r=["B3b"], w=["wT"])
                P.mm(B5[:, 256:384], wT[:], Sst[:, hh, :], r=["wT", "Sst"], w=["B5c"])
                P.tt(vn[:], R[:, 0:128], B5[:, 256:384], ALU.subtract, r=["R", "B5c"], w=["vn"])
                P.mm(B5[:, 384:512], qT, Sst[:, hh, :], r=["qk", "Sst"], w=["B5d"])
                P.mm(B4[:, 256:384], attnT[:], vn[:], r=["attnT", "vn"], w=["B4c"])
                P.cp(o2[:], B4[:, 256:384], r=["B4c"], w=["o2"])
                P.stt(od[:, hh * 128:(hh + 1) * 128], B5[:, 384:512], egs[:, 0:1], o2[:], ALU.mult, ALU.add,
                      r=["B5d", "egs0", "o2"], w=["od"])
                P.ts(kdk[:], k_tok, egs[:, 2:3], None, ALU.mult, r=["ktv", "egs2"], w=["kdk"])
                P.mm(B4[:, 384:512], kdk[:], vn[:], r=["kdk", "vn"], w=["B4d"])
                P.stt(Sst[:, hh, :], Sst[:, hh, :], egs[:, 1:2], B4[:, 384:512], ALU.mult, ALU.add,
                      r=["Sst", "egs1", "B4d"], w=["Sst"])
                segs.append(P.ops[seg_start:])
                del P.ops[seg_start:]
                P.ksuf = ""
            if segs:
                B2, B3, B4, B5 = PB2, PB3, PB4, PB5
                for i_ in range(max(len(s_) for s_ in segs)):
                    for s_ in segs:
                        if i_ < len(s_):
                            P.ops.append(s_[i_])
            if d == 0:
                P.dma("sp", o_s[rows, :], od[:], r=["od_0", "od_1"], w=["o_s"], key="od_st")
            else:
                P.dma("sp", oprev[:], o_s[rows, :], r=["o_s"], w=["oprev"], key="oprev")
                P.dma("sp", zt[:], zs[rows, :], r=["zs"], w=["zt"], key="zt_ld")
                P.tt(od[:], od[:], oprev[:], ALU.add, r=["od_0", "od_1", "oprev"], w=["od_0", "od_1", "odf"])
                P.tt(oprev[:], od[:], od[:], ALU.mult, r=["odf"], w=["oprev"])
                P.op("dve", lambda e: e.reduce_sum(ss[:, 5:7], oprev[:].rearrange("p (h f) -> p h f", f=128), AX.X),
                     r=["oprev"], w=["ss57"])
                P.ts(ss[:, 5:7], ss[:, 5:7], 1.0 / 128, 1e-6, ALU.mult, ALU.add, r=["ss57"], w=["ss57"])
                P.act(ss[:, 5:7], ss[:, 5:7], AF.Sqrt, r=["ss57"], w=["ss57"])
                P.op("dve", lambda e: e.reciprocal(ss[:, 5:7], ss[:, 5:7]), r=["ss57"], w=["ss57"])
                for hh in range(2):
                    cs = slice(hh * 128, (hh + 1) * 128)
                    P.stt(od[:, cs], od[:, cs], ss[:, 5 + hh:6 + hh], dnbc[:], ALU.mult, ALU.mult, r=["odf", "ss57", "dnbc"], w=["odf", "od_0", "od_1"])
                P.tt(yb[:], od[:], zt[:], ALU.mult, r=["odf", "zt"], w=["yb"])
                P.dma("sp", mixp[rows, 256:512], yb[:], r=["yb"], key="mixp")
    P.out_dma.append("mixp")
    return P.finalize()


D = 2048


def _run(nc, in_maps):
    return run_bass_kernel_spmd(nc, in_maps, core_ids=list(range(8))).results


def _bc(v):
    v = np.asarray(v, np.float32).reshape(1, -1)
    return np.ascontiguousarray(np.broadcast_to(v, (128, v.shape[1])))


def _rope_table(S):
    GRID_W = 64
    rows = S // GRID_W
    row = np.repeat(np.arange(rows, dtype=np.float32), GRID_W)
    col = np.tile(np.arange(GRID_W, dtype=np.float32), rows)
    inv = (np.float32(10000.0) ** (-np.arange(16, dtype=np.float32) / np.float32(16))).astype(np.float32)
    ang = np.stack([row[:, None] * inv, col[:, None] * inv], axis=1).astype(np.float32)
    return np.concatenate([np.cos(ang).reshape(S, 32), np.sin(ang).reshape(S, 32)], axis=1).astype(np.float32)


def kernel(x, c, ctx, c_ctx, w_mod, b_mod, norm_g, e_w_in, e_w_out, e_q_gain, e_k_gain, e_sinks, e_conv_w,
           e_a_log, e_dt_bias, e_dn_norm, o_w_in, o_gate_w2, o_gate_b, o_gla_norm, o_w_out,
           w_router, b_router, w_gu, b_gu, w_down, b_down):
    f32 = lambda a: np.ascontiguousarray(np.asarray(a, dtype=np.float32))
    x, c, ctx, c_ctx, w_mod, b_mod, norm_g = map(f32, (x, c, ctx, c_ctx, w_mod, b_mod, norm_g))
    B, S, _ = x.shape
    CTXL = ctx.shape[1]
    DFF = w_gu.shape[-1] // 2
    NLc = S // 4 // 128
    CR = CTXL // 4
    NLT, NCT = S // 128, CTXL // 128
    TB = S + CTXL
    identb = np.eye(128).astype(NPBF)
    identf = np.eye(128, dtype=np.float32)
    o = np.ones((128, 128), np.float32)
    M4 = np.stack([np.triu(o), np.tril(o), np.triu(o, 1), np.tril(o, -1)])
    cv = np.stack([c[0], c[1], c_ctx])
    cT = np.ascontiguousarray(cv.reshape(3, 16, 128).transpose(2, 1, 0).reshape(128, 48))
    ims = []
    for cc in range(8):
        cs = slice(cc * 1536, (cc + 1) * 1536)
        ims.append(dict(cT=cT, wm=np.ascontiguousarray(w_mod[:, :, cs]), bm=np.ascontiguousarray(b_mod[:, cs]).reshape(1, 2 * 1536)))
    r = _run(build_mod(), ims)
    mod = np.concatenate([r[cc]["modo"] for cc in range(8)], axis=2).reshape(2, 3, 6, D)

    def tok_rows(cc):
        b, q = cc // 4, cc % 4
        return b, slice(q * (S // 4), (q + 1) * (S // 4)), slice(q * CR, (q + 1) * CR)

    NT0 = NLc + 1
    xins, ims = [], []
    for cc in range(8):
        b, ls, cs = tok_rows(cc)
        xin = np.zeros((NT0 * 128, D), np.float32)
        xin[:NLc * 128] = x[b, ls]
        xin[NLc * 128:NLc * 128 + CR] = ctx[b, cs]
        xins.append(xin)
        modn = np.ascontiguousarray(np.stack([mod[0, b, 0:2], mod[0, 2, 0:2]]))
        ims.append(dict(xin=xin, g=norm_g[0, 0], modn=modn))
    r = _run(build_pre(NT0, CR), ims)

    def gather_h(res, key):
        hb = np.zeros((B, TB, D), NPBF)
        for cc in range(8):
            b, ls, cs = tok_rows(cc)
            hb[b, ls] = res[cc][key][:NLc * 128]
            hb[b, S + cs.start:S + cs.stop] = res[cc][key][NLc * 128:NLc * 128 + CR]
        return hb

    hb = gather_h(r, "hout")
    W0 = f32(e_w_in[0])
    cwT = f32(e_conv_w[0])
    rope = _rope_table(S)
    Mc = np.ascontiguousarray(np.concatenate([M4.transpose(1, 0, 2).reshape(128, 512),
                                              rope.reshape(NLT, 128, 64).transpose(1, 0, 2).reshape(128, NLT * 64)], axis=1))
    ims = []
    for cc in range(8):
        b, j = cc // 4, cc % 4
        base = 1536
        cols = np.concatenate([np.arange(j * 256, (j + 1) * 256), 1024 + np.arange(j * 64, (j + 1) * 64),
                               1280 + np.arange(j * 64, (j + 1) * 64),
                               base + np.arange(2 * j * 128, (2 * j + 2) * 128),
                               base + 1024 + np.arange(2 * j * 128, (2 * j + 2) * 128),
                               base + 2048 + np.arange(2 * j * 128, (2 * j + 2) * 128),
                               base + 3072 + np.arange(2 * j * 128, (2 * j + 2) * 128),
                               5632 + np.arange(2 * j, 2 * j + 2), 5640 + np.arange(2 * j, 2 * j + 2),
                               5648 + np.arange(2 * j, 2 * j + 2), 5656 + np.arange(2 * j, 2 * j + 2)])
        ccols = np.concatenate([np.arange(2 * j * 128, (2 * j + 2) * 128), 1024 + np.arange(2 * j * 128, (2 * j + 2) * 128),
                                2048 + np.arange(2 * j * 128, (2 * j + 2) * 128)])
        cw = np.ascontiguousarray(cwT[:, ccols].T.reshape(6, 128, 5).transpose(1, 0, 2).reshape(128, 30))
        gpar = np.concatenate([np.asarray(e_a_log, np.float32)[0, 0, 2 * j:2 * j + 2], np.asarray(e_a_log, np.float32)[0, 1, 2 * j:2 * j + 2],
                               np.asarray(e_dt_bias, np.float32)[0, 0, 2 * j:2 * j + 2], np.asarray(e_dt_bias, np.float32)[0, 1, 2 * j:2 * j + 2]])
        ims.append(dict(h=np.ascontiguousarray(hb[b]), wc=np.ascontiguousarray(W0[:, cols]),
                        qkg=_bc(np.concatenate([np.asarray(e_q_gain, np.float32)[0]] * 4 + [np.asarray(e_k_gain, np.float32)[0]])),
                        sinks=_bc(np.asarray(e_sinks, np.float32)[0, 4 * j:4 * j + 4]), cw=cw, gpar=_bc(gpar),
                        dnn=_bc(np.asarray(e_dn_norm, np.float32)[0]), M=Mc, identb=identb, identf=identf))
    r = _run(build_mix0(NLT, NCT), ims)
    mixf = np.zeros((B, TB, D), NPBF)
    for cc in range(8):
        b, j = cc // 4, cc % 4
        mixf[b, :, j * 256:(j + 1) * 256] = r[cc]["mixp"][:, 0:256]
        mixf[b, :, 1024 + j * 256:1024 + (j + 1) * 256] = r[cc]["mixp"][:, 256:512]

    def post(layer, last, xin_list, mixfull, wout):
        NT = NLc if last else NLc + 1
        wr = f32(w_router[layer]); br = f32(b_router[layer]).reshape(1, 32)
        wgu = f32(w_gu[layer]); bgu = f32(b_gu[layer]); wdn = f32(w_down[layer]); bdn = f32(b_down[layer])
        wo = f32(wout)
        ims = []
        for cc in range(8):
            b, ls, cs = tok_rows(cc)
            mix = np.zeros((NT * 128, D), NPBF)
            mix[:NLc * 128] = mixfull[b, ls]
            if not last:
                mix[NLc * 128:NLc * 128 + CR] = mixfull[b, S + cs.start:S + cs.stop]
            modv = np.ascontiguousarray(np.stack([mod[layer, b], mod[layer, 2]]))
            im = dict(xin=xin_list[cc], mix=mix, modv=modv, g2=norm_g[layer, 1], wout=wo, wr=wr, br=br, wgu=wgu, bgu=bgu,
                      wdn=wdn, bdn=bdn, identb=identb, identf=identf)
            if not last:
                im["gn"] = norm_g[layer + 1, 0]
                im["modn"] = np.ascontiguousarray(np.stack([mod[layer + 1, b, 0:2], mod[layer + 1, 2, 0:2]]))
            ims.append(im)
        return _run(build_post(NT, DFF, last, 0 if last else CR), ims)

    r = post(0, False, xins, mixf, e_w_out[0])
    x1 = [np.ascontiguousarray(r[cc]["xout"][:NLc * 128]) for cc in range(8)]
    hb = gather_h(r, "hnext")
    W1 = f32(o_w_in[0]); gw2 = f32(o_gate_w2[0]); gb = f32(o_gate_b[0])
    U2 = np.ascontiguousarray(M4[0:2])
    ims = []
    for cc in range(8):
        b, j = cc // 4, cc % 4
        cols = np.concatenate([np.arange(j * 256, (j + 1) * 256), 1024 + np.arange(j * 256, (j + 1) * 256),
                               2048 + np.arange(j * 512, (j + 1) * 512), 4096 + np.arange(j * 512, (j + 1) * 512),
                               np.arange(6144, 6176)])
        w2 = np.ascontiguousarray(np.concatenate([gw2[:, :, j * 256:(j + 1) * 256], gb[:, None, j * 256:(j + 1) * 256]], axis=1))
        ims.append(dict(h=np.ascontiguousarray(hb[b]), wc=np.ascontiguousarray(W1[:, cols]), w2=w2,
                        gnorm=f32(o_gla_norm[0]), identb=identb, U=U2))
    r = _run(build_gla(NLT, NCT), ims)
    mixf = np.zeros((B, TB, D), NPBF)
    for cc in range(8):
        b, j = cc // 4, cc % 4
        mixf[b, :, j * 512:(j + 1) * 512] = r[cc]["mixp"]
    r = post(1, True, x1, mixf, o_w_out[0])
    out = np.zeros((B, S, D), np.float32)
    for cc in range(8):
        b, ls, cs = tok_rows(cc)
        out[b, ls] = r[cc]["xout"][:NLc * 128]
    return out
```

```python
import os
import numpy as np
import ml_dtypes
from contextlib import ExitStack
import concourse.bass as bass
import concourse.mybir as mybir
from concourse.bass_utils import run_bass_kernel_spmd

F32 = mybir.dt.float32
BF16 = mybir.dt.bfloat16
I32 = mybir.dt.int32
U32 = mybir.dt.uint32
AF = mybir.ActivationFunctionType
ALU = mybir.AluOpType
AX = mybir.AxisListType
NPBF = ml_dtypes.bfloat16

SEM_ROT = 20000


class _Op:
    __slots__ = ("stream", "fn", "r", "w", "dma", "deps", "sig", "need")

    def __init__(self, stream, fn, r, w, dma):
        self.stream = stream
        self.fn = fn
        self.r = r
        self.w = w
        self.dma = dma
        self.deps = None
        self.sig = None
        self.need = False


class Prog:
    def __init__(self):
        self.nc = bass.Bass("TRN2", target_bir_lowering=False)
        self.es = ExitStack()
        self.ops = []
        self.out_dma = []
        self.keymap = {}
        self.ksuf = ""
        self.ksuf_keys = set()

    def din(self, name, shape, dt):
        return self.nc.dram_tensor(name, list(shape), dt, kind="ExternalInput").ap()

    def dout(self, name, shape, dt):
        return self.nc.dram_tensor(name, list(shape), dt, kind="ExternalOutput").ap()

    def dtmp(self, name, shape, dt):
        return self.nc.dram_tensor(name, list(shape), dt, kind="Internal").ap()

    def sb(self, name, shape, dt):
        return self.es.enter_context(self.nc.sbuf_tensor(name, list(shape), dt))

    def ps(self, name, shape, dt):
        return self.es.enter_context(self.nc.psum_tensor(name, list(shape), dt))

    def _km(self, keys):
        out = []
        for k in keys:
            if self.ksuf and k in self.ksuf_keys:
                k = k + self.ksuf
            k = self.keymap.get(k, k)
            if k not in out:
                out.append(k)
        return tuple(out)

    def op(self, stream, fn, r=(), w=()):
        self.ops.append(_Op(stream, fn, self._km(r), self._km(w), None))

    def dma(self, stream, out, in_, r=(), w=(), key=None, **kw):
        assert key is not None
        self.ops.append(_Op(stream, lambda e: e.dma_start(out=out, in_=in_, **kw), self._km(r), self._km(w), key))

    def dmaf(self, stream, fn, r=(), w=(), key=None):
        assert key is not None
        self.ops.append(_Op(stream, fn, self._km(r), self._km(w), key))

    def mm(self, out, lhsT, rhs, start=True, stop=True, r=(), w=(), **kw):
        self.op("pe", lambda e: e.matmul(out, lhsT, rhs, start=start, stop=stop, **kw), r, w)

    def tr(self, out, in_, ident, r=(), w=()):
        self.op("pe", lambda e: e.transpose(out, in_, ident), r, w)

    def act(self, out, in_, func, r=(), w=(), stream="act", **kw):
        self.op(stream, lambda e: e.activation(out=out, in_=in_, func=func, **kw), r, w)

    def tt(self, out, in0, in1, op, r=(), w=(), stream="dve"):
        self.op(stream, lambda e: e.tensor_tensor(out, in0, in1, op), r, w)

    def ts(self, out, in0, s1, s2, op0, op1=None, r=(), w=(), stream="dve", **kw):
        if op1 is None:
            self.op(stream, lambda e: e.tensor_scalar(out, in0, s1, None, op0, **kw), r, w)
        else:
            self.op(stream, lambda e: e.tensor_scalar(out, in0, s1, s2, op0, op1, **kw), r, w)

    def stt(self, out, in0, scalar, in1, op0, op1, r=(), w=(), stream="dve", **kw):
        self.op(stream, lambda e: e.scalar_tensor_tensor(out, in0, scalar, in1, op0, op1, **kw), r, w)

    def cp(self, out, in_, r=(), w=(), stream="dve"):
        if stream == "act":
            self.op(stream, lambda e: e.copy(out, in_), r, w)
        else:
            self.op(stream, lambda e: e.tensor_copy(out, in_), r, w)

    def memset(self, ap, val, w=(), stream="dve"):
        self.op(stream, lambda e: e.memset(ap, val), (), w)

    def finalize(self):
        nc = self.nc
        ops = self.ops
        state = {}

        def dom(o):
            return ("dma", o.dma) if o.dma is not None else o.stream

        for i, o in enumerate(ops):
            deps = set()
            d_i = dom(o)
            for k in o.r:
                st = state.setdefault(k, ({}, {}))
                deps.update(st[0].values())
                st[1][d_i] = i
            for k in o.w:
                st = state.setdefault(k, ({}, {}))
                wr, rd = st
                others_r = {d: j for d, j in rd.items() if j != i}
                if others_r:
                    for d, j in others_r.items():
                        deps.add(j)
                    for d, j in wr.items():
                        if d != d_i or (o.dma is None and d_i != "pe"):
                            deps.add(j)
                    wr.clear()
                    rd.clear()
                    wr[d_i] = i
                else:
                    for d, j in wr.items():
                        if d != d_i or (o.dma is None and d_i != "pe"):
                            deps.add(j)
                    rd.clear()
                    wr[d_i] = i
            deps.discard(i)
            o.deps = deps
            for j in deps:
                ops[j].need = True
        eng_cnt = {}
        sem_of = {}

        def get_sem(key):
            if key not in sem_of:
                sem_of[key] = self.es.enter_context(nc.semaphore("s%d" % len(sem_of)))
            return sem_of[key]

        dma_cum = {}
        for o in ops:
            if o.dma is not None:
                dma_cum[o.dma] = dma_cum.get(o.dma, 0) + 16
                o.sig = (("dma", o.dma), dma_cum[o.dma])
            elif o.need:
                c = eng_cnt.get(o.stream, 0) + 1
                eng_cnt[o.stream] = c
                o.sig = ((o.stream, (c - 1) // SEM_ROT), (c - 1) % SEM_ROT + 1)
        streams = {}
        for i, o in enumerate(ops):
            streams.setdefault(o.stream, []).append(i)
        waits = [None] * len(ops)
        waited = {}
        for i, o in enumerate(ops):
            wl = {}
            for j in o.deps:
                sk, val = ops[j].sig
                if wl.get(sk, 0) < val:
                    wl[sk] = val
            ws = waited.setdefault(o.stream, {})
            out = []
            for sk, val in wl.items():
                if ws.get(sk, 0) < val:
                    ws[sk] = val
                    out.append((sk, val))
            waits[i] = out
        for sk in set(s for w in waits for s, _ in w):
            get_sem(sk)
        for o in ops:
            if o.sig is not None:
                get_sem(o.sig[0])
        final = [(("dma", k), dma_cum[k]) for k in self.out_dma if k in dma_cum]

        def emit(stream, e):
            for i in streams.get(stream, []):
                o = ops[i]
                for sk, val in waits[i]:
                    e.wait_ge(sem_of[sk], val)
                ins = o.fn(e)
                if o.sig is not None:
                    sk, _ = o.sig
                    ins.then_inc(sem_of[sk], 16 if o.dma is not None else 1)
            if stream == "sp":
                for sk, val in final:
                    e.wait_ge(sem_of[sk], val)

        with nc.Block() as block:
            @block.sync
            def _(e):
                emit("sp", e)

            @block.tensor
            def _(e):
                emit("pe", e)

            @block.vector
            def _(e):
                emit("dve", e)

            @block.scalar
            def _(e):
                emit("act", e)

            @block.gpsimd
            def _(e):
                emit("pool", e)
        self.es.close()
        self.n_sems = len(sem_of)
        return nc


D = 2048
KD = 16
EPS = 1e-6


def build_mod():
    P = Prog()
    cT = P.din("cT", [128, KD * 3], F32)
    wm = P.din("wm", [2, D, 1536], F32)
    bm = P.din("bm", [1, 2 * 1536], F32)
    out = P.dout("modo", [2, 3, 1536], F32)
    sc = P.sb("sc", [128, KD, 3], F32)
    wms = P.sb("wms", [128, KD, 1536], F32)
    bms = P.sb("bms", [1, 2, 1536], F32)
    ones3 = P.sb("ones3", [1, 3], F32)
    res = P.sb("res", [3, 1536], F32)
    ps = P.ps("pm", [128, 512], F32)
    P.dma("sp", sc[:].rearrange("p k r -> p (k r)"), cT, w=["sc"], key="sc")
    P.act(sc[:].rearrange("p k r -> p (k r)"), sc[:].rearrange("p k r -> p (k r)"), AF.Silu, r=["sc"], w=["sc"])
    P.dma("sp", bms[:].rearrange("o l c -> o (l c)"), bm, w=["bms"], key="bms")
    P.memset(ones3[:], 1.0, w=["ones3"])
    for l in range(2):
        for k in range(KD):
            P.dma("sp", wms[:, k, :], wm[l, k * 128:(k + 1) * 128, :], w=["wms"], key="wms")
        for cc in range(3):
            cs = slice(cc * 512, (cc + 1) * 512)
            for k in range(KD):
                P.mm(ps[0:3, :], sc[:, k, :], wms[:, k, cs], start=(k == 0), stop=False, r=["sc", "wms"], w=["pm"])
            P.mm(ps[0:3, :], ones3[:, :], bms[:, l, cs], start=False, stop=True, r=["ones3", "bms"], w=["pm"])
            P.cp(res[:, cs], ps[0:3, :], r=["pm"], w=["res"])
        P.dma("sp", out[l], res[:], r=["res"], key="modo")
    P.out_dma.append("modo")
    return P.finalize()


def build_pre(NT, ctx_rows):
    P = Prog()
    R = NT * 128
    xin = P.din("xin", [R, D], F32)
    g = P.din("g", [D], F32)
    modn = P.din("modn", [2, 2, D], F32)
    hout = P.dout("hout", [R, D], BF16)
    bcB = P.sb("bcB", [128, D], F32)
    bcC = P.sb("bcC", [128, D], F32)
    xt = P.sb("xt", [128, D], F32)
    tmp = P.sb("tmp", [128, D], F32)
    hn = P.sb("hn", [128, D], BF16)
    sm = P.sb("sm", [128, 2], F32)

    def set_mod(kind):
        P.dma("sp", bcB[:], modn[kind, 1, :].partition_broadcast(128), w=["bcB"], key="bcB")
        P.dma("sp", bcC[:], g.partition_broadcast(128), w=["bcC"], key="bcC")
        P.stt(bcB[:], bcB[:], 1.0, bcC[:], ALU.add, ALU.mult, r=["bcB", "bcC"], w=["bcB"])
        P.dma("sp", bcC[:], modn[kind, 0, :].partition_broadcast(128), w=["bcC"], key="bcC")

    set_mod(0)
    for t in range(NT):
        if ctx_rows > 0 and t == NT - 1:
            set_mod(1)
        rows = slice(t * 128, (t + 1) * 128)
        P.dma("sp", xt[:], xin[rows, :], w=["xt"], key="xt")
        P.memset(sm[:, 0:1], 0.0, w=["sm"])
        P.act(tmp[:], xt[:], AF.Square, r=["xt"], w=["tmp", "sm"], accum_out=sm[:, 0:1])
        P.ts(sm[:, 0:1], sm[:, 0:1], 1.0 / D, EPS, ALU.mult, ALU.add, r=["sm"], w=["sm"])
        P.act(sm[:, 0:1], sm[:, 0:1], AF.Sqrt, r=["sm"], w=["sm"])
        P.op("dve", lambda e: e.reciprocal(sm[:, 0:1], sm[:, 0:1]), r=["sm"], w=["sm"])
        P.stt(tmp[:], xt[:], sm[:, 0:1], bcB[:], ALU.mult, ALU.mult, r=["xt", "sm", "bcB"], w=["tmp"])
        P.tt(hn[:], tmp[:], bcC[:], ALU.add, r=["tmp", "bcC"], w=["hn"])
        P.dma("sp", hout[rows, :], hn[:], r=["hn"], key="hout")
    P.out_dma.append("hout")
    return P.finalize()


D = 2048
KD = 16
NE = 32
EPS = 1e-6


def bc_row(ap_row, n):
    return ap_row.partition_broadcast(128)


def build_post(NT, DFF, last, ctx_rows):
    P = Prog()
    R = NT * 128
    KF = DFF // 128
    NCG = DFF // 256
    xin = P.din("xin", [R, D], F32)
    mix = P.din("mix", [R, D], BF16)
    modv = P.din("modv", [2, 6, D], F32)
    g2 = P.din("g2", [D], F32)
    wout = P.din("wout", [D, D], F32)
    wr = P.din("wr", [D, NE], F32)
    br = P.din("br", [1, NE], F32)
    wgu = P.din("wgu", [NE, D, 2 * DFF], F32)
    bgu = P.din("bgu", [NE, 2 * DFF], F32)
    wdn = P.din("wdn", [NE, DFF, D], F32)
    bdn = P.din("bdn", [NE, D], F32)
    identb_d = P.din("identb", [128, 128], BF16)
    identf_d = P.din("identf", [128, 128], F32)
    if not last:
        gn = P.din("gn", [D], F32)
        modn = P.din("modn", [2, 2, D], F32)
        hnext = P.dout("hnext", [R, D], BF16)
    xout = P.dout("xout", [R, D], F32)
    wo_s = P.dtmp("wo_s", [4, 128, KD * 512], BF16)
    wg_l = [P.dtmp("wg_s%d" % e, [NCG, 128, KD * 512], BF16) for e in range(NE)]
    wd_l = [P.dtmp("wd_s%d" % e, [4, 128, KF * 512], BF16) for e in range(NE)]
    x1_s = P.dtmp("x1_s", [R, D], F32)
    h2_s = P.dtmp("h2_s", [R, D], BF16)
    wbuf = P.sb("wbuf", [128, 2, KD * 512], BF16)
    BA = P.sb("BA", [128, 32768], BF16)
    FA = P.sb("FA", [128, 8192], F32)
    bcA = P.sb("bcA", [128, D], F32)
    bcB = P.sb("bcB", [128, D], F32)
    bcC = P.sb("bcC", [128, D], F32)
    xt3 = P.sb("xt3", [128, D], F32)
    hn = P.sb("hn", [128, D], BF16)
    bgT = P.sb("bgT", [128, 2 * (DFF // 128), NE], F32)
    Bd = P.sb("Bd", [NE, D], F32)
    G = P.sb("G", [128, NT, NE], F32)
    GT = P.sb("GT", [NE, 512], F32)
    GTb = P.sb("GTb", [NE, 512], BF16)
    identb = P.sb("identb_s", [128, 128], BF16)
    identf = P.sb("identf_s", [128, 128], F32)
    wrs = P.sb("wrs", [128, KD, NE], F32)
    brs = P.sb("brs", [1, NE], F32)
    ones1 = P.sb("ones1", [1, 128], F32)
    sm = P.sb("sm", [128, 16], F32)
    mx8 = P.sb("mx8", [128, 8], F32)
    lg = P.sb("lg", [128, NE], F32)
    msk = P.sb("msk", [128, NE], F32)
    gl = P.sb("gl", [128, 512], F32)
    sg = P.sb("sg", [128, 512], F32)
    ln = P.sb("ln", [128, 512], F32)
    ptb = [P.ps("ptb%d" % i, [128, 1024], BF16) for i in range(2)]
    pf = [P.ps("pf%d" % i, [128, 512], F32) for i in range(2)]
    pg = [P.ps("pg%d" % i, [128, 512], F32) for i in range(2)]
    py = [P.ps("py%d" % i, [128, 512], F32) for i in range(2)]

    mixt = BA[:, 0:2048]
    mixT = BA[:, 2048:4096]
    h2b = BA[:, 4096:6144]
    xt = FA[:, 0:2048]
    h2 = FA[:, 2048:4096]
    h2T32 = FA[:, 4096:6144]
    P1KEYS = ["mixt", "mixT", "h2b", "xt", "h2", "h2T32"]

    P.dma("sp", identb[:], identb_d, w=["identb"], key="identb")
    P.dma("sp", identf[:], identf_d, w=["identf"], key="identf")
    P.dma("sp", wrs[:], wr.rearrange("(k p) e -> p k e", p=128), w=["wrs"], key="wrs")
    P.dma("sp", brs[:], br, w=["brs"], key="brs")
    P.dma("sp", Bd[:], bdn, w=["Bd"], key="Bd")
    P.dma("sp", FA[0:NE, 0:2 * DFF], bgu, w=["xt", "h2"], key="bgst")
    for c_ in range(2 * (DFF // 128)):
        P.tr(pf[c_ % 2][:, 0:NE], FA[0:NE, c_ * 128:(c_ + 1) * 128], identf[0:NE, 0:NE], r=["xt", "h2", "identf"], w=["pf%d" % (c_ % 2)])
        P.cp(bgT[:, c_, :], pf[c_ % 2][:, 0:NE], r=["pf%d" % (c_ % 2)], w=["bgT"])
    P.memset(ones1[:], 1.0, w=["ones1"])
    P.memset(G[:], 0.0, w=["G"])

    stg = 0

    def cast_piece(dst, srcs):
        nonlocal stg
        s = stg % 2
        stg += 1
        key = "wp%d" % s
        for (lo, hi, src) in srcs:
            P.dma("pool", wbuf[:, s, :].rearrange("p (k c) -> p k c", c=512)[:, :src.shape[1], lo:hi], src,
                  w=[key], key=key + "l")
        P.dma("sp", dst, wbuf[:, s, 0:dst.shape[1]], r=[key], w=["wscr"], key=key + "s")

    for dc in range(4):
        cast_piece(wo_s[dc], [(0, 512, wout[:, dc * 512:(dc + 1) * 512].rearrange("(k p) c -> p k c", p=128))])
    for e in range(NE):
        for cg in range(NCG):
            cast_piece(wg_l[e][cg], [
                (0, 256, wgu[e, :, cg * 256:(cg + 1) * 256].rearrange("(k p) c -> p k c", p=128)),
                (256, 512, wgu[e, :, DFF + cg * 256:DFF + (cg + 1) * 256].rearrange("(k p) c -> p k c", p=128))])
        for dc in range(4):
            cast_piece(wd_l[e][dc], [(0, 512, wdn[e, :, dc * 512:(dc + 1) * 512].rearrange("(k p) c -> p k c", p=128))])

    wslot = [0]

    def load_piece(src, extra_w=()):
        s = wslot[0] % 2
        wslot[0] += 1
        key = "wp%d" % s
        P.dma("sp", wbuf[:, s, 0:src.shape[1]], src, r=["wscr"], w=[key] + list(extra_w), key=key + "l")
        return s, key

    def load_bc(tile, row_ap, key):
        P.dma("sp", tile[:], row_ap.partition_broadcast(128), w=[key], key=key)

    def set_mod(kind):
        load_bc(bcA, modv[kind, 2, :], "bcA")
        load_bc(bcB, modv[kind, 4, :], "bcB")
        load_bc(bcC, g2, "bcC")
        P.stt(bcB[:], bcB[:], 1.0, bcC[:], ALU.add, ALU.mult, r=["bcB", "bcC"], w=["bcB"])
        load_bc(bcC, modv[kind, 3, :], "bcC")

    def rstd_from(src, key_src, col):
        P.memset(sm[:, col:col + 1], 0.0, w=["sm%d" % col])
        P.act(h2[:], src, AF.Square, r=[key_src], w=["h2", "sm%d" % col], accum_out=sm[:, col:col + 1])
        P.ts(sm[:, col:col + 1], sm[:, col:col + 1], 1.0 / D, EPS, ALU.mult, ALU.add, r=["sm%d" % col], w=["sm%d" % col])
        P.act(sm[:, col:col + 1], sm[:, col:col + 1], AF.Sqrt, r=["sm%d" % col], w=["sm%d" % col])
        P.op("dve", lambda e: e.reciprocal(sm[:, col:col + 1], sm[:, col:col + 1]), r=["sm%d" % col], w=["sm%d" % col])

    set_mod(0)
    for t in range(NT):
        is_ctx = ctx_rows > 0 and t == NT - 1
        if is_ctx:
            set_mod(1)
        rows = slice(t * 128, (t + 1) * 128)
        P.dma("sp", mixt, mix[rows, :], w=["mixt"], key="mixt")
        P.dma("sp", xt, xin[rows, :], w=["xt"], key="xt")
        for j in range(2):
            for i in range(8):
                k = j * 8 + i
                P.tr(ptb[j][:, i * 128:(i + 1) * 128], mixt[:, k * 128:(k + 1) * 128], identb[:],
                     r=["mixt", "identb"], w=["ptb%d" % j])
            P.cp(mixT[:, j * 1024:(j + 1) * 1024], ptb[j][:], r=["ptb%d" % j], w=["mixT"], stream="act" if j else "dve")
        for dc in range(4):
            s, key = load_piece(wo_s[dc])
            wv = wbuf[:, s, :].rearrange("p (k c) -> p k c", c=512)
            ps = pf[dc % 2]
            pk = "pf%d" % (dc % 2)
            for k in range(KD):
                P.mm(ps[:], mixT[:, k * 128:(k + 1) * 128], wv[:, k, :], start=(k == 0), stop=(k == KD - 1),
                     r=["mixT", key], w=[pk])
            cs = slice(dc * 512, (dc + 1) * 512)
            P.tt(h2[:, cs], ps[:], bcA[:, cs], ALU.mult, r=[pk, "bcA"], w=["h2"])
            P.tt(xt[:, cs], h2[:, cs], xt[:, cs], ALU.add, r=["h2", "xt"], w=["xt"])
        P.dma("sp", x1_s[rows, :], xt, r=["xt"], w=["x1_s"], key="xt_st")
        rstd_from(xt, "xt", 0)
        P.stt(h2[:], xt, sm[:, 0:1], bcB[:], ALU.mult, ALU.mult, r=["xt", "sm0", "bcB"], w=["h2"])
        P.tt(h2[:], h2[:], bcC[:], ALU.add, r=["h2", "bcC"], w=["h2"])
        P.cp(h2b, h2[:], r=["h2"], w=["h2b"], stream="act")
        P.dma("sp", h2_s[rows, :], h2b, r=["h2b"], w=["h2_s"], key="h2b_st")
        if last and False:
            pass
        for j in range(4):
            for i in range(4):
                k = j * 4 + i
                P.tr(pf[j % 2][:, i * 128:(i + 1) * 128], h2[:, k * 128:(k + 1) * 128], identf[:],
                     r=["h2", "identf"], w=["pf%d" % (j % 2)])
            P.cp(h2T32[:, j * 512:(j + 1) * 512], pf[j % 2][:], r=["pf%d" % (j % 2)], w=["h2T32"],
                 stream="act" if j % 2 else "dve")
        for k in range(KD):
            P.mm(pg[0][:, 0:NE], h2T32[:, k * 128:(k + 1) * 128], wrs[:, k, :], start=(k == 0), stop=False,
                 r=["h2T32", "wrs"], w=["pg0"])
        P.mm(pg[0][:, 0:NE], ones1[:, :], brs[:, :], start=False, stop=True, r=["ones1", "brs"], w=["pg0"])
        P.cp(lg[:], pg[0][:, 0:NE], r=["pg0"], w=["lg"])
        P.op("dve", lambda e: e.max(mx8[:], lg[:]), r=["lg"], w=["mx8"])
        P.ts(msk[:], lg[:], mx8[:, 3:4], None, ALU.is_ge, r=["lg", "mx8"], w=["msk"])
        P.ts(sm[:, 2:3], mx8[:, 0:1], -1.0, None, ALU.mult, r=["mx8"], w=["sm2"])
        P.act(lg[:], lg[:], AF.Exp, r=["lg", "sm2"], w=["lg"], bias=sm[:, 2:3], scale=1.0)
        P.tt(lg[:], lg[:], msk[:], ALU.mult, r=["lg", "msk"], w=["lg"])
        P.op("dve", lambda e: e.reduce_sum(sm[:, 3:4], lg[:], AX.X), r=["lg"], w=["sm3"])
        P.op("dve", lambda e: e.reciprocal(sm[:, 3:4], sm[:, 3:4]), r=["sm3"], w=["sm3"])
        nv = ctx_rows if is_ctx else 128
        P.ts(G[0:nv, t, :], lg[0:nv, :], sm[0:nv, 3:4], None, ALU.mult, r=["lg", "sm3"], w=["G"])

    h2g = BA[:, 0:8192]
    h2T = BA[:, 8192:16384]
    actb = BA[:, 16384:24576]
    actT = BA[:, 24576:32768]
    acc = FA[:, 0:8192]
    first = True
    ngroups = (NT + 3) // 4
    for g in range(ngroups):
        t0 = g * 4
        nt = min(4, NT - t0)
        NTOK = nt * 128
        extra = P1KEYS if first else []
        for tt in range(nt):
            P.dma("sp", h2g[:, tt * 2048:(tt + 1) * 2048], h2_s[(t0 + tt) * 128:(t0 + tt + 1) * 128, :],
                  r=["h2_s"], w=["h2g"] + (extra if tt == 0 else []), key="h2g")
        h2Tv = h2T.rearrange("p (k t) -> p k t", t=512)
        for tt in range(nt):
            for j in range(2):
                for i in range(8):
                    k = j * 8 + i
                    P.tr(ptb[j][:, i * 128:(i + 1) * 128], h2g[:, tt * 2048 + k * 128: tt * 2048 + (k + 1) * 128],
                         identb[:], r=["h2g", "identb"], w=["ptb%d" % j])
                P.cp(h2Tv[:, j * 8:(j + 1) * 8, tt * 128:(tt + 1) * 128],
                     ptb[j][:].rearrange("p (k t) -> p k t", t=128), r=["ptb%d" % j],
                     w=["h2T"] + (extra if (tt == 0 and j == 0) else []), stream="act" if j else "dve")
        for tt in range(nt):
            P.tr(pf[0][0:NE, tt * 128:(tt + 1) * 128], G[:, t0 + tt, :], identf[:], r=["G", "identf"], w=["pf0"])
        P.cp(GT[:, 0:NTOK], pf[0][0:NE, 0:NTOK], r=["pf0"], w=["GT"])
        for tt in range(nt):
            for dc in range(4):
                ps = py[dc % 2]
                pk = "py%d" % (dc % 2)
                P.mm(ps[:], GT[:, tt * 128:(tt + 1) * 128], Bd[:, dc * 512:(dc + 1) * 512], r=["GT", "Bd"], w=[pk])
                P.cp(acc[:, tt * 2048 + dc * 512: tt * 2048 + (dc + 1) * 512], ps[:], r=[pk],
                     w=["acc"] + (extra if (tt == 0 and dc == 0) else []), stream="act" if dc % 2 else "dve")
        first = False
        for e in range(NE):
            actTv = actT.rearrange("p (k t) -> p k t", t=512)
            for cg in range(NCG):
                s, key = load_piece(wg_l[e][cg])
                wv = wbuf[:, s, :].rearrange("p (k c) -> p k c", c=512)
                for jj in range(2):
                    j = cg * 2 + jj
                    pgl, kgl = pg[j % 2], "pg%d" % (j % 2)
                    pln, kln = pf[j % 2], "pf%d" % (j % 2)
                    for k in range(KD):
                        P.mm(pgl[:, 0:NTOK], wv[:, k, jj * 128:(jj + 1) * 128], h2Tv[:, k, 0:NTOK], start=(k == 0),
                             stop=(k == KD - 1), r=["h2T", key], w=[kgl])
                    for k in range(KD):
                        P.mm(pln[:, 0:NTOK], wv[:, k, 256 + jj * 128:256 + (jj + 1) * 128], h2Tv[:, k, 0:NTOK], start=(k == 0),
                             stop=(k == KD - 1), r=["h2T", key], w=[kln])
                    P.ts(gl[:, 0:NTOK], pgl[:, 0:NTOK], bgT[:, j, e:e + 1], 7.0, ALU.add, ALU.min, r=[kgl, "bgT"], w=["gl"])
                    P.act(sg[:, 0:NTOK], gl[:, 0:NTOK], AF.Sigmoid, r=["gl"], w=["sg"], scale=1.702)
                    P.ts(ln[:, 0:NTOK], pln[:, 0:NTOK], bgT[:, KF + j, e:e + 1], 7.0, ALU.add, ALU.min, r=[kln, "bgT"], w=["ln"])
                    P.ts(ln[:, 0:NTOK], ln[:, 0:NTOK], -7.0, 1.0, ALU.max, ALU.add, r=["ln"], w=["ln"])
                    P.tt(gl[:, 0:NTOK], gl[:, 0:NTOK], sg[:, 0:NTOK], ALU.mult, r=["gl", "sg"], w=["gl"])
                    P.tt(actTv[:, j, 0:NTOK], gl[:, 0:NTOK], ln[:, 0:NTOK], ALU.mult, r=["gl", "ln"], w=["actT"])
            for dc in range(4):
                s, key = load_piece(wd_l[e][dc])
                wv = wbuf[:, s, 0:KF * 512].rearrange("p (k c) -> p k c", c=512)
                for tt in range(nt):
                    ps = py[tt % 2]
                    pk = "py%d" % (tt % 2)
                    for kf in range(KF):
                        P.mm(ps[:], actTv[:, kf, tt * 128:(tt + 1) * 128], wv[:, kf, :], start=(kf == 0), stop=(kf == KF - 1),
                             r=["actT", key], w=[pk])
                    a = acc[:, tt * 2048 + dc * 512: tt * 2048 + (dc + 1) * 512]
                    P.stt(a, ps[:], G[:, t0 + tt, e:e + 1], a, ALU.mult, ALU.add, r=[pk, "G", "acc"], w=["acc"])
        for tt in range(nt):
            t = t0 + tt
            is_ctx = ctx_rows > 0 and t == NT - 1
            kind = 1 if is_ctx else 0
            rows = slice(t * 128, (t + 1) * 128)
            if tt == 0 or is_ctx:
                load_bc(bcA, modv[kind, 5, :], "bcA")
                if not last:
                    load_bc(bcB, modn[kind, 1, :], "bcB")
                    load_bc(bcC, gn, "bcC")
                    P.stt(bcB[:], bcB[:], 1.0, bcC[:], ALU.add, ALU.mult, r=["bcB", "bcC"], w=["bcB"])
                    load_bc(bcC, modn[kind, 0, :], "bcC")
            P.dma("sp", xt3[:], x1_s[rows, :], r=["x1_s"], w=["xt3"], key="xt3")
            a = acc[:, tt * 2048:(tt + 1) * 2048]
            P.tt(a, a, bcA[:], ALU.mult, r=["acc", "bcA"], w=["acc"])
            P.tt(xt3[:], xt3[:], a, ALU.add, r=["xt3", "acc"], w=["xt3"])
            P.dma("sp", xout[rows, :], xt3[:], r=["xt3"], key="xout")
            if not last:
                P.memset(sm[:, 5:6], 0.0, w=["sm5"])
                P.act(a, xt3[:], AF.Square, r=["xt3"], w=["acc", "sm5"], accum_out=sm[:, 5:6])
                P.ts(sm[:, 5:6], sm[:, 5:6], 1.0 / D, EPS, ALU.mult, ALU.add, r=["sm5"], w=["sm5"])
                P.act(sm[:, 5:6], sm[:, 5:6], AF.Sqrt, r=["sm5"], w=["sm5"])
                P.op("dve", lambda e: e.reciprocal(sm[:, 5:6], sm[:, 5:6]), r=["sm5"], w=["sm5"])
                P.stt(a, xt3[:], sm[:, 5:6], bcB[:], ALU.mult, ALU.mult, r=["xt3", "sm5", "bcB"], w=["acc"])
                P.tt(hn[:], a, bcC[:], ALU.add, r=["acc", "bcC"], w=["hn"])
                P.dma("sp", hnext[rows, :], hn[:], r=["hn"], key="hnext")
    P.out_dma.append("xout")
    if not last:
        P.out_dma.append("hnext")
    return P.finalize()


D = 2048
KD = 16


import os
GSTAGE = int(os.environ.get('GSTAGE', '99'))
GSUB = int(os.environ.get('GSUB', '9'))


def build_gla(NLT, NCT):
    P = Prog()
    TB = (NLT + NCT) * 128
    NC = 1568
    h = P.din("h", [TB, D], BF16)
    wc = P.din("wc", [D, NC], F32)
    w2 = P.din("w2", [2, 17, 256], F32)
    gnorm = P.din("gnorm", [512], F32)
    identb_d = P.din("identb", [128, 128], BF16)
    U_d = P.din("U", [2, 128, 128], F32)
    mixp = P.dout("mixp", [TB, 512], BF16)
    o_s = P.dtmp("o_s", [TB, 512], F32)

    W = P.sb("W", [128, KD, NC], BF16)
    identb = P.sb("identb_s", [128, 128], BF16)
    U = P.sb("U_s", [128, 2, 128], F32)
    w2s = P.sb("w2s", [17, 2, 256], F32)
    gbc = P.sb("gbc", [128, 512], F32)
    ones = P.sb("ones", [128, 128], F32)
    ht = P.sb("ht", [128, D], BF16)
    hT = P.sb("hT", [128, KD, 128], BF16)
    qT = P.sb("qT", [128, 2, 128], F32)
    kT = P.sb("kT", [128, 2, 128], F32)
    vb = P.sb("vb", [128, 512], BF16)
    r17 = P.sb("r17", [17, 128], F32)
    gk = P.sb("gk", [128, 256], F32)
    tmpa = P.sb("tmpa", [128, 256], F32)
    tmpb = P.sb("tmpb", [128, 256], F32)
    bTs = P.sb("bTs", [128, 2, 128], F32)
    ee = P.sb("ee", [128, 3, 2, 128], F32)
    qs = P.sb("qs", [128, 2, 128], BF16)
    ks = P.sb("ks", [128, 2, 128], BF16)
    qd = P.sb("qd", [128, 2, 128], BF16)
    attnT = P.sb("attnT", [128, 128], BF16)
    kd = P.sb("kd", [128, 256], BF16)
    S = P.sb("S", [128, 2, 512], F32)
    Sb = P.sb("Sb", [128, 2, 512], BF16)
    ot = P.sb("ot", [128, 512], F32)
    ot2 = P.sb("ot2", [128, 512], F32)
    go = P.sb("go", [128, 512], F32)
    yb = P.sb("yb", [128, 512], BF16)
    sm = P.sb("sm", [128, 8], F32)
    ptb = [P.ps("ptb%d" % i, [128, 1024], BF16) for i in range(2)]
    p_qk = P.ps("p_qk", [128, 512], F32)
    p_v = P.ps("p_v", [128, 512], F32)
    p_kr = P.ps("p_kr", [128, 512], F32)
    p_b = P.ps("p_b", [128, 512], F32)
    p_bT = P.ps("p_bT", [128, 512], F32)
    p_o = P.ps("p_o", [128, 512], F32)

    for k in range(KD):
        P.dma("pool", W[:, k, :], wc[k * 128:(k + 1) * 128, :], w=["W"], key="W")
    P.dma("sp", identb[:], identb_d, w=["identb"], key="identb")
    P.dma("sp", U[:], U_d.rearrange("d m c -> m d c"), w=["U"], key="U")
    P.dma("sp", w2s[:], w2.rearrange("d r c -> r d c"), w=["w2s"], key="w2s")
    P.dma("sp", gbc[:], gnorm.partition_broadcast(128), w=["gbc"], key="gbc")
    P.memset(ones[:], 1.0, w=["ones"])
    P.memset(r17[:], 1.0, w=["r17"])

    for d in range(2):
        P.memset(S[:], 0.0, w=["S"])
        P.memset(Sb[:], 0.0, w=["Sb"])
        last = 127 if d == 0 else 0
        order = [("c", i) for i in range(NCT)] + [("l", i) for i in range(NLT)]
        if d == 1:
            order = [("c", i) for i in reversed(range(NCT))] + [("l", i) for i in reversed(range(NLT))]
        for kind, i in order:
            r0 = (i if kind == "l" else NLT + i) * 128
            rows = slice(r0, r0 + 128)
            need_o = kind == "l"
            if GSTAGE < -3:
                continue
            P.dma("sp", ht[:], h[rows, :], w=["ht"], key="ht")
            for j in range(2):
                for q in range(8):
                    k = j * 8 + q
                    P.tr(ptb[j][:, q * 128:(q + 1) * 128], ht[:, k * 128:(k + 1) * 128], identb[:],
                         r=["ht", "identb"], w=["ptb%d" % j])
                P.cp(hT[:, j * 8:(j + 1) * 8, :], ptb[j][:].rearrange("p (k t) -> p k t", t=128), r=["ptb%d" % j],
                     w=["hT"], stream="act" if j else "dve")
            if GSUB < 1:
                continue
            for f in range(4):
                for k in range(KD):
                    P.mm(p_qk[:, f * 128:(f + 1) * 128], W[:, k, f * 128:(f + 1) * 128], hT[:, k, :],
                         start=(k == 0), stop=(k == KD - 1), r=["W", "hT"], w=["p_qk"])
            if GSUB < 2:
                continue
            P.ts(qT[:], p_qk[:, 0:256].rearrange("p (f t) -> p f t", t=128), 1.0 / 16, None, ALU.mult, r=["p_qk"], w=["qT"])
            P.cp(kT[:], p_qk[:, 256:512].rearrange("p (f t) -> p f t", t=128), r=["p_qk"], w=["kT"])
            if GSTAGE < -2:
                continue
            for k in range(KD):
                P.mm(p_v[:], hT[:, k, :], W[:, k, 512:1024], start=(k == 0), stop=(k == KD - 1), r=["W", "hT"], w=["p_v"])
            P.cp(vb[:], p_v[:], r=["p_v"], w=["vb"], stream="act")
            for k in range(KD):
                P.mm(p_kr[:, 0:256], hT[:, k, :], W[:, k, 256:512], start=(k == 0), stop=(k == KD - 1),
                     r=["W", "hT"], w=["p_kr"])
            if GSTAGE < -1:
                continue
            for k in range(KD):
                P.mm(p_bT[0:16, 384:512], W[:, k, 1536 + 16 * d:1552 + 16 * d], hT[:, k, :], start=(k == 0),
                     stop=(k == KD - 1), r=["W", "hT"], w=["p_rT"])
            P.cp(r17[0:16, :], p_bT[0:16, 384:512], r=["p_rT"], w=["r17"])
            P.mm(p_kr[:, 256:512], r17[:, :], w2s[:, d, :], r=["r17", "w2s"], w=["p_gk"])
            P.act(tmpa[:], p_kr[:, 256:512], AF.Exp, r=["p_gk"], w=["tmpa"], scale=-1.0)
            P.act(tmpa[:], tmpa[:], AF.Ln, r=["tmpa"], w=["tmpa"], bias=1.0)
            P.ts(gk[:], tmpa[:], -1.0 / 16, None, ALU.mult, r=["tmpa"], w=["gk"])
            if GSTAGE < 1:
                continue
            P.mm(p_b[:, 0:256], U[:, d, :], gk[:], r=["U", "gk"], w=["p_btok"])
            P.mm(p_b[:, 256:512], ones[:], gk[:], r=["ones", "gk"], w=["p_blast"])
            for f in range(2):
                P.mm(p_bT[:, f * 128:(f + 1) * 128], gk[:, f * 128:(f + 1) * 128], U[:, d, :], r=["gk", "U"], w=["p_bT"])
            P.cp(bTs[:], p_bT[:, 0:256].rearrange("p (f t) -> p f t", t=128), r=["p_bT"], w=["bTs"])
            P.ts(sm[:, 0:2], bTs[:, :, 64], -1.0, None, ALU.mult, r=["bTs"], w=["sm01"])
            for f in range(2):
                P.act(ee[:, 0, f, :], bTs[:, f, :], AF.Exp, r=["bTs", "sm01"], w=["ee"], bias=sm[:, f:f + 1], scale=1.0)
                P.act(ee[:, 1, f, :], bTs[:, f, :], AF.Exp, r=["bTs"], w=["ee"], bias=bTs[:, f, 64:65], scale=-1.0)
                P.act(ee[:, 2, f, :], bTs[:, f, :], AF.Exp, r=["bTs"], w=["ee"])
                P.act(sm[:, 2 + f:3 + f], bTs[:, f, last:last + 1], AF.Exp, r=["bTs"], w=["sm23"])
            P.tt(qs[:], qT[:], ee[:, 0], ALU.mult, r=["qT", "ee"], w=["qs"])
            P.tt(ks[:], kT[:], ee[:, 1], ALU.mult, r=["kT", "ee"], w=["ks"])
            P.tt(qd[:], qT[:], ee[:, 2], ALU.mult, r=["qT", "ee"], w=["qd"])
            if GSTAGE < 2:
                continue
            for f in range(2):
                P.mm(p_bT[:, 256:384], ks[:, f, :], qs[:, f, :], start=(f == 0), stop=(f == 1), r=["ks", "qs"], w=["p_at"])
            P.tt(attnT[:], p_bT[:, 256:384], U[:, d, :], ALU.mult, r=["p_at", "U"], w=["attnT"])
            P.cp(tmpb[:], p_b[:, 256:512], r=["p_blast"], w=["tmpb"], stream="act")
            P.tt(tmpb[:], tmpb[:], p_b[:, 0:256], ALU.subtract, r=["tmpb", "p_btok"], w=["tmpb"])
            P.act(tmpb[:], tmpb[:], AF.Exp, r=["tmpb"], w=["tmpb"])
            P.tt(kd[:], p_kr[:, 0:256], tmpb[:], ALU.mult, r=["p_kr", "tmpb"], w=["kd"])
            if GSTAGE < 3:
                continue
            if need_o:
                P.mm(p_o[:], attnT[:], vb[:], start=True, stop=False, r=["attnT", "vb"], w=["p_o"])
                for f in range(2):
                    P.mm(p_o[:], qd[:, f, :], Sb[:, f, :], start=False, stop=(f == 1), r=["qd", "Sb"], w=["p_o"])
                if d == 0:
                    P.cp(ot[:], p_o[:], r=["p_o"], w=["ot"])
                    P.dma("sp", o_s[rows, :], ot[:], r=["ot"], w=["o_s"], key="ot_st")
                else:
                    P.dma("sp", ot2[:], o_s[rows, :], r=["o_s"], w=["ot2"], key="ot2")
                    P.tt(ot[:], p_o[:], ot2[:], ALU.add, r=["p_o", "ot2"], w=["ot"])
                    for k in range(KD):
                        P.mm(p_v[:], hT[:, k, :], W[:, k, 1024:1536], start=(k == 0), stop=(k == KD - 1),
                             r=["W", "hT"], w=["p_v"])
                    P.act(go[:], p_v[:], AF.Silu, r=["p_v"], w=["go"])
                    P.memset(sm[:, 4:5], 0.0, w=["sm4"])
                    P.act(ot2[:], ot[:], AF.Square, r=["ot"], w=["ot2", "sm4"], accum_out=sm[:, 4:5])
                    P.ts(sm[:, 4:5], sm[:, 4:5], 1.0 / 512, 1e-6, ALU.mult, ALU.add, r=["sm4"], w=["sm4"])
                    P.act(sm[:, 4:5], sm[:, 4:5], AF.Sqrt, r=["sm4"], w=["sm4"])
                    P.op("dve", lambda e: e.reciprocal(sm[:, 4:5], sm[:, 4:5]), r=["sm4"], w=["sm4"])
                    P.stt(ot[:], ot[:], sm[:, 4:5], gbc[:], ALU.mult, ALU.mult, r=["ot", "sm4", "gbc"], w=["ot"])
                    P.tt(yb[:], ot[:], go[:], ALU.mult, r=["ot", "go"], w=["yb"])
                    P.dma("sp", mixp[rows, :], yb[:], r=["yb"], key="mixp")
            if GSTAGE < 4:
                continue
            for f in range(2):
                P.mm(p_o[:], kd[:, f * 128:(f + 1) * 128], vb[:], r=["kd", "vb"], w=["p_o"])
                P.stt(S[:, f, :], S[:, f, :], sm[:, 2 + f:3 + f], p_o[:], ALU.mult, ALU.add, r=["S", "sm23", "p_o"], w=["S"])
                P.cp(Sb[:, f, :], S[:, f, :], r=["S"], w=["Sb"], stream="act")
    P.memset(yb[:], 0.0, w=["yb"])
    for i in range(NCT):
        r0 = (NLT + i) * 128
        P.dma("sp", mixp[r0:r0 + 128, :], yb[:], r=["yb"], key="mixp")
    P.out_dma.append("mixp")
    return P.finalize()


D = 2048
KD = 16


import os
STAGE = int(os.environ.get('STAGE', '9'))
VAR = os.environ.get('VAR', '')
SUB = float(os.environ.get('SUB', '9'))


def build_mix0(NLT, NCT):
    P = Prog()
    P.keymap = {"B0a": "B0", "B0x": "B0", "B1z": "B1", "B2b": "B2", "B2c": "B2", "B2d": "B2", "B3a": "B3", "B3b": "B3",
                "B3c": "B3", "B4c": "B4", "B4d": "B4", "B5a": "B5", "B5b": "B5", "B5c": "B5", "B5d": "B5"}
    HKEYS = ["gbc", "gct", "egs0", "egs1", "egs2", "egs3", "t1", "t2", "Dcm", "DTs", "DTi", "kb", "kbT", "Mb0", "Mb1", "MTb0", "MTb1",
             "attnT", "R", "wT", "vn", "o2", "kdk", "Sst", "od", "B2", "B2b", "B2c", "B2d", "B3a", "B3b", "B3c", "B4", "B4c", "B4d",
             "B5a", "B5b", "B5c", "B5d"]
    P.ksuf_keys = set(HKEYS)
    for kk_ in ("B2", "B2b", "B2c", "B2d"):
        P.keymap[kk_ + "_0"] = "B2"; P.keymap[kk_ + "_1"] = "B0"
    for kk_ in ("B3a", "B3b", "B3c"):
        P.keymap[kk_ + "_0"] = "B3"; P.keymap[kk_ + "_1"] = "B1"
    for kk_ in ("B4", "B4c", "B4d"):
        P.keymap[kk_ + "_0"] = "B4"; P.keymap[kk_ + "_1"] = "ptb0"
    for kk_ in ("B5a", "B5b", "B5c", "B5d"):
        P.keymap[kk_ + "_0"] = "B5"; P.keymap[kk_ + "_1"] = "ptb1"
    TB = (NLT + NCT) * 128
    S = NLT * 128
    CT = NCT * 128
    TBP = S + 4 + CT + 4
    NC = 1416
    NTT = NLT + NCT
    h = P.din("h", [TB, D], BF16)
    wc = P.din("wc", [D, NC], F32)
    qkg = P.din("qkg", [128, 320], F32)
    sinks = P.din("sinks", [128, 4], F32)
    cw = P.din("cw", [128, 30], F32)
    gpar = P.din("gpar", [128, 8], F32)
    dnn = P.din("dnn", [128, 128], F32)
    M_d = P.din("M", [128, 512 + NLT * 64], F32)
    identb_d = P.din("identb", [128, 128], BF16)
    identf_d = P.din("identf", [128, 128], F32)
    mixp = P.dout("mixp", [TB, 512], BF16)
    xs = P.dtmp("xs", [6, 128, TBP], F32)
    zs = P.dtmp("zs", [TB, 256], F32)
    gs = P.dtmp("gs", [TB, 8], F32)
    o_s = P.dtmp("o_s", [TB, 256], F32)

    W = P.sb("W", [128, KD, NC], BF16)
    identb = P.sb("identb_s", [128, 128], BF16)
    identf = P.sb("identf_s", [128, 128], F32)
    Mm = P.sb("Mm", [128, 4, 128], F32)
    gq = P.sb("gq", [128, 320], F32)
    esink = P.sb("esink", [128, 4], F32)
    cws = P.sb("cws", [128, 6, 5], F32)
    gp = P.sb("gp", [128, 8], F32)
    dnbc = P.sb("dnbc", [128, 128], F32)
    ones = P.sb("ones", [128, 128], F32)
    zero6 = P.sb("zero6", [128, 6, 2], F32)
    ht = P.sb("ht", [128, D], BF16)
    hT = P.sb("hT", [128, KD, 128], BF16)
    qkv = P.sb("qkv", [128, 384], F32)
    sq = P.sb("sq", [128, 320], F32)
    ss = P.sb("ss", [128, 8], F32)
    qkn = P.sb("qkn", [128, 320], F32)
    qkr = P.sb("qkr", [128, 320], F32)
    rt = P.sb("ropetmp", [128, 64], F32)
    rpa = P.sb("rpa", [128, NLT, 64], F32)
    qrb = P.sb("qrb", [128, 320], BF16)
    qTr = [P.sb("qTr%d" % i, [64, 512], BF16) for i in range(2)]
    kTa = P.sb("kTa", [64, NTT * 128], BF16)
    V1 = P.sb("V1", [128, NTT, 66], BF16)
    xst = P.sb("xst", [128, 6, 128], F32)
    zt = P.sb("zt", [128, 256], F32)
    gts = P.sb("gts", [128, 8], F32)
    gtm = P.sb("gtm", [128, 4], F32)
    eT = P.sb("eT", [128, 5, 512], BF16)
    den = P.sb("den", [128, 4], F32)
    aout = P.sb("aout", [128, 256], BF16)
    xw = P.sb("xw", [128, 6, 132], F32)
    yc = P.sb("yc", [128, 6, 128], F32)
    sq4 = P.sb("sq4", [128, 4, 128], F32)
    rs4 = P.sb("rs4", [128, 4, 128], F32)
    qk = P.sb("qk", [128, 4, 128], F32)
    ktv = P.sb("ktv", [128, 4, 128], F32)
    gt8 = P.sb("gt8", [128, 8], F32)
    gbc = P.sb("gbc", [128, 128], F32)
    gct = P.sb("gct", [128, 1], F32)
    egs = P.sb("egs", [128, 4], F32)
    t1 = P.sb("t1", [128, 128], F32)
    t2 = P.sb("t2", [128, 128], F32)
    Dcm = P.sb("Dcm", [128, 128], F32)
    DTs = P.sb("DTs", [128, 128], F32)
    DTi = P.sb("DTi", [128, 128], F32)
    kb = P.sb("kb", [128, 128], F32)
    kbT = P.sb("kbT", [128, 128], F32)
    Mb = [P.sb("Mb%d" % i, [128, 128], F32) for i in range(2)]
    MTb = [P.sb("MTb%d" % i, [128, 128], F32) for i in range(2)]
    attnT = P.sb("attnT", [128, 128], F32)
    R = P.sb("R", [128, 256], F32)
    wT = P.sb("wT", [128, 128], F32)
    vn = P.sb("vn", [128, 128], F32)
    o2 = P.sb("o2", [128, 128], F32)
    od = P.sb("od", [128, 256], F32)
    oprev = P.sb("oprev", [128, 256], F32)
    kdk = P.sb("kdk", [128, 128], F32)
    Sst = P.sb("Sst", [128, 2, 128], F32)
    yb = P.sb("yb", [128, 256], BF16)

    HT_NAMES = ["gbc", "gct", "egs", "t1", "t2", "Dcm", "DTs", "DTi", "kb", "kbT", "Mb0", "Mb1", "MTb0", "MTb1", "attnT", "R",
                "wT", "vn", "o2", "kdk"]
    HT0 = dict(gbc=gbc, gct=gct, egs=egs, t1=t1, t2=t2, Dcm=Dcm, DTs=DTs, DTi=DTi, kb=kb, kbT=kbT, Mb0=Mb[0], Mb1=Mb[1],
               MTb0=MTb[0], MTb1=MTb[1], attnT=attnT, R=R, wT=wT, vn=vn, o2=o2, kdk=kdk)
    HT1 = {}
    for nm_ in HT_NAMES:
        shp_ = [128, 256] if nm_ == "R" else ([128, 1] if nm_ == "gct" else ([128, 4] if nm_ == "egs" else [128, 128]))
        HT1[nm_] = P.sb(nm_ + "_h1", shp_, F32)
    ptb = [P.ps("ptb%d" % i, [128, 1024], BF16) for i in range(2)]
    B0 = P.ps("B0", [128, 512], F32)
    B1 = P.ps("B1", [128, 512], F32)
    B2 = P.ps("B2", [128, 512], F32)
    B3 = P.ps("B3", [128, 512], F32)
    B4 = P.ps("B4", [128, 512], F32)
    B5 = P.ps("B5", [128, 512], F32)
    PB2, PB3, PB4, PB5 = B2, B3, B4, B5
    ptbf = [ptb[i][:].bitcast(F32) for i in range(2)]

    for k in range(KD):
        P.dma("pool", W[:, k, :], wc[k * 128:(k + 1) * 128, :], w=["W"], key="W")
    P.dma("sp", identb[:], identb_d, w=["identb"], key="identb")
    P.dma("sp", identf[:], identf_d, w=["identf"], key="identf")
    P.dma("sp", Mm[:].rearrange("p d c -> p (d c)"), M_d[:, 0:512], w=["Mm"], key="Mm")
    P.dma("sp", gq[:], qkg, w=["gq"], key="gq")
    P.dma("sp", esink[:], sinks, w=["esink"], key="esink")
    P.act(esink[:], esink[:], AF.Exp, r=["esink"], w=["esink"])
    P.dma("sp", cws[:].rearrange("p c j -> p (c j)"), cw, w=["cws"], key="cws")
    P.dma("sp", gp[:], gpar, w=["gp"], key="gp")
    P.act(gp[:, 0:4], gp[:, 0:4], AF.Exp, r=["gp"], w=["gp"])
    P.ts(gp[:, 0:4], gp[:, 0:4], -1.0, None, ALU.mult, r=["gp"], w=["gp"])
    P.dma("sp", dnbc[:], dnn, w=["dnbc"], key="dnbc")
    P.dma("sp", rpa[:].rearrange("p n c -> p (n c)"), M_d[:, 512:512 + NLT * 64], w=["rp"], key="rp")
    P.memset(ones[:], 1.0, w=["ones"])
    P.memset(zero6[:], 0.0, w=["zero6"])
    P.memset(V1[:], 1.0, w=["V1"])

    def tile_pos(kind, i):
        if kind == "l":
            return i * 128, 2 + i * 128, i
        return S + i * 128, S + 4 + 2 + i * 128, NLT + i

    def proj_tile(kind, i, qslot):
        if STAGE < 1:
            return
        r0, col0, gt = tile_pos(kind, i)
        rows = slice(r0, r0 + 128)
        P.dma("sp", ht[:], h[rows, :], r=["identb", "identf", "Mm", "gq", "esink", "cws", "gp", "dnbc", "rp"], w=["ht"], key="ht")
        for j in range(2):
            for q in range(8):
                k = j * 8 + q
                P.tr(ptb[j][:, q * 128:(q + 1) * 128], ht[:, k * 128:(k + 1) * 128], identb[:],
                     r=["ht", "identb"], w=["ptb%d" % j])
            P.cp(hT[:, j * 8:(j + 1) * 8, :], ptb[j][:].rearrange("p (k t) -> p k t", t=128), r=["ptb%d" % j],
                 w=["hT"], stream="act" if j else "dve")
        if SUB < 2:
            return
        for k in range(KD):
            P.mm(B0[:, 0:384], hT[:, k, :], W[:, k, 0:384], start=(k == 0), stop=(k == KD - 1), r=["W", "hT"], w=["B0a"])
        P.cp(qkv[:], B0[:, 0:384], r=["B0a"], w=["qkv"])
        P.tt(sq[:], qkv[:, 0:320], qkv[:, 0:320], ALU.mult, r=["qkv"], w=["sq"])
        P.op("dve", lambda e: e.reduce_sum(ss[:, 0:5], sq[:].rearrange("p (h f) -> p h f", f=64), AX.X), r=["sq"], w=["ss"])
        P.ts(ss[:, 0:5], ss[:, 0:5], 1.0 / 64, 1e-6, ALU.mult, ALU.add, r=["ss"], w=["ss"])
        P.act(ss[:, 0:5], ss[:, 0:5], AF.Sqrt, r=["ss"], w=["ss"])
        P.op("dve", lambda e: e.reciprocal(ss[:, 0:5], ss[:, 0:5]), r=["ss"], w=["ss"])
        for hh in range(5):
            cs = slice(hh * 64, (hh + 1) * 64)
            P.stt(qkn[:, cs], qkv[:, cs], ss[:, hh:hh + 1], gq[:, cs], ALU.mult, ALU.mult, r=["qkv", "ss", "gq"], w=["qkn"])
        if SUB < 3:
            return
        src = qkn
        if kind == "l":
            rp = rpa[:, i, :]
            if VAR == 'xdma':
                P.dma("sp", qkr[:, 0:64], M_d[0, :, 0:64], w=["qkr"], key="xd")
            for hh in range(0 if VAR == "noops" else 5):
                for a in range(2):
                    b0 = hh * 64 + a * 32
                    x1 = qkn[:, b0:b0 + 16]
                    x2 = qkn[:, b0 + 16:b0 + 32]
                    cv = rp[:, a * 16:(a + 1) * 16]
                    sv = rp[:, 32 + a * 16:32 + (a + 1) * 16]
                    P.tt(rt[:, 0:16], x1, cv, ALU.mult, r=["qkn", "rp"], w=["rt"])
                    P.tt(rt[:, 16:32], x2, sv, ALU.mult, r=["qkn", "rp"], w=["rt"])
                    P.tt(rt[:, 32:48], x2, cv, ALU.mult, r=["qkn", "rp"], w=["rt"])
                    P.tt(rt[:, 48:64], x1, sv, ALU.mult, r=["qkn", "rp"], w=["rt"])
                    P.tt(qkr[:, b0:b0 + 16], rt[:, 0:16], rt[:, 16:32], ALU.subtract, r=["rt"], w=["qkr"])
                    P.tt(qkr[:, b0 + 16:b0 + 32], rt[:, 32:48], rt[:, 48:64], ALU.add, r=["rt"], w=["qkr"])
            src = qkr
        if SUB < 4.1:
            return
        P.cp(qrb[:], src[:], r=["qkn", "qkr"], w=["qrb"], stream="act")
        for hh in range(5):
            P.tr(ptb[1][0:64, hh * 128:(hh + 1) * 128], qrb[:, hh * 64:(hh + 1) * 64], identb[:],
                 r=["qrb", "identb"], w=["ptb1"])
        if SUB < 4.2:
            return
        P.cp(qTr[qslot][:, :], ptb[1][0:64, 0:512], r=["ptb1"], w=["qT%d" % qslot])
        if SUB < 4.3:
            return
        P.cp(kTa[:, gt * 128:(gt + 1) * 128], ptb[1][0:64, 512:640], r=["ptb1"], w=["kTa"])
        if SUB < 4.4:
            return
        P.cp(V1[:, gt, 0:64], qkv[:, 320:384], r=["qkv"], w=["V1"])
        if SUB < 5:
            return
        for c in range(6):
            for k in range(KD):
                P.mm(B0[:, 384:512], W[:, k, 384 + c * 128:384 + (c + 1) * 128], hT[:, k, :], start=(k == 0),
                     stop=(k == KD - 1), r=["W", "hT"], w=["B0x"])
            P.cp(xst[:, c, :], B0[:, 384:512], r=["B0x"], w=["xst"], stream="act" if c % 2 else "dve")
        P.dma("sp", xs[:, :, col0:col0 + 128].rearrange("c p t -> p c t"), xst[:], r=["xst"], w=["xs"], key="xst_st")
        if SUB < 6:
            return
        for k in range(KD):
            P.mm(B1[:, 0:264], hT[:, k, :], W[:, k, 1152:1416], start=(k == 0), stop=(k == KD - 1), r=["W", "hT"], w=["B1z"])
        P.act(zt[:], B1[:, 0:256], AF.Silu, r=["B1z"], w=["zt"])
        P.dma("sp", zs[rows, :], zt[:], r=["zt"], w=["zs"], key="zt_st")
        P.tt(gtm[:], B1[:, 256:260], gp[:, 4:8], ALU.add, r=["B1z", "gp"], w=["gtm"])
        P.act(gtm[:], gtm[:], AF.Exp, r=["gtm"], w=["gtm"])
        P.act(gtm[:], gtm[:], AF.Ln, r=["gtm"], w=["gtm"], bias=1.0)
        P.tt(gts[:, 0:4], gtm[:], gp[:, 0:4], ALU.mult, r=["gtm", "gp"], w=["gts"])
        P.act(gts[:, 4:8], B1[:, 260:264], AF.Sigmoid, r=["B1z"], w=["gts"])
        P.dma("sp", gs[rows, :], gts[:], r=["gts"], w=["gs"], key="gts_st")

    def attn_block(kind, i, qslot):
        if STAGE < 2:
            return
        r0, col0, gt = tile_pos(kind, i)
        kbs = []
        if kind == "l":
            if i > 0:
                kbs.append((i - 1, 1))
            kbs.append((i, None))
            if i < NLT - 1:
                kbs.append((i + 1, 0))
        for c in range(NCT):
            kbs.append((NLT + c, None))
        for idx, (kg, mk) in enumerate(kbs):
            ps = B2 if idx % 2 == 0 else B3
            pk = "B2" if idx % 2 == 0 else "B3"
            P.mm(ps[:], kTa[:, kg * 128:(kg + 1) * 128], qTr[qslot][:, :], r=["kTa", "qT%d" % qslot], w=[pk])
            P.act(eT[:, idx, :], ps[:], AF.Exp, r=[pk], w=["eT"], scale=0.125)
            if mk is not None:
                for hh in range(4):
                    P.tt(eT[:, idx, hh * 128:(hh + 1) * 128], eT[:, idx, hh * 128:(hh + 1) * 128], Mm[:, mk, :], ALU.mult,
                         r=["eT", "Mm"], w=["eT"])
        for hh in range(4):
            for idx, (kg, mk) in enumerate(kbs):
                P.mm(B4[:, hh * 128:hh * 128 + 65], eT[:, idx, hh * 128:(hh + 1) * 128], V1[:, kg, 0:65], start=(idx == 0),
                     stop=(idx == len(kbs) - 1), r=["eT", "V1"], w=["B4"])
        pv = B4[:, :].rearrange("p (h f) -> p h f", f=128)
        P.tt(den[:], pv[:, :, 64], esink[:], ALU.add, r=["B4", "esink"], w=["den"])
        P.op("dve", lambda e: e.reciprocal(den[:], den[:]), r=["den"], w=["den"])
        for hh in range(4):
            P.ts(aout[:, hh * 64:(hh + 1) * 64], B4[:, hh * 128:hh * 128 + 64], den[:, hh:hh + 1], None, ALU.mult,
                 r=["B4", "den"], w=["aout"])
        P.dma("sp", mixp[r0:r0 + 128, 0:256], aout[:], r=["aout"], key="mixp")

    for c in range(NCT):
        proj_tile("c", c, c % 2)
    for c in range(NCT):
        attn_block("c", c, c % 2)
    for i in range(NLT):
        proj_tile("l", i, i % 2)
        if i >= 1:
            attn_block("l", i - 1, (i - 1) % 2)
    attn_block("l", NLT - 1, (NLT - 1) % 2)

    PSK = ["B0a", "B0x", "B1z", "B2", "B3", "B4", "B2b", "B2c", "B2d", "B3a", "B3b", "B3c", "B4c", "B4d",
           "B5a", "B5b", "B5c", "B5d"]
    P.memset(ss[:, 7:8], 0.0, w=PSK + ["ss7"])
    for d in range(2 if STAGE >= 3 else 0):
        P.memset(Sst[:], 0.0, w=["Sst_0", "Sst_1"])
        iMin = 0 if d == 0 else 1
        iMst = 2 if d == 0 else 3
        iLst = 3 if d == 0 else 2
        last = 127 if d == 0 else 0
        order = [("c", i) for i in range(NCT)] + [("l", i) for i in range(NLT)]
        if d == 1:
            order = [("c", i) for i in reversed(range(NCT))] + [("l", i) for i in reversed(range(NLT))]
        for kind, i in order:
            r0, col0, gt = tile_pos(kind, i)
            rows = slice(r0, r0 + 128)
            nlast = (NLT if kind == "l" else NCT) - 1
            lo = 2 if i == 0 else 0
            hi = 130 if i == nlast else 132
            if lo:
                P.memset(xw[:, :, 0:2], 0.0, w=["xw"])
            if hi < 132:
                P.memset(xw[:, :, 130:132], 0.0, w=["xw"])
            P.dma("sp", xw[:, :, lo:hi], xs[:, :, col0 - 2 + lo:col0 - 2 + hi].rearrange("c p t -> p c t"), r=["xs"], w=["xw"], key="xw")
            P.dma("sp", gt8[:], gs[rows, :], r=["gs"], w=["gt8"], key="gt8")
            for c in range(6):
                P.ts(yc[:, c, :], xw[:, c, 0:128], cws[:, c, 0:1], None, ALU.mult, r=["xw", "cws"], w=["yc"])
                for j in range(1, 5):
                    P.stt(yc[:, c, :], xw[:, c, j:j + 128], cws[:, c, j:j + 1], yc[:, c, :], ALU.mult, ALU.add,
                          r=["xw", "cws", "yc"], w=["yc"])
            P.act(yc[:], yc[:], AF.Silu, r=["yc"], w=["yc"])
            P.tt(sq4[:], yc[:, 0:4, :], yc[:, 0:4, :], ALU.mult, r=["yc"], w=["sq4"])
            P.mm(B0[:], ones[:], sq4[:].rearrange("p c t -> p (c t)"), r=["ones", "sq4"], w=["B0a", "B0x"])
            P.ts(rs4[:].rearrange("p c t -> p (c t)"), B0[:], 1e-6, None, ALU.add, r=["B0a"], w=["rs4"])
            P.act(rs4[:], rs4[:], AF.Sqrt, r=["rs4"], w=["rs4"])
            P.op("dve", lambda e: e.reciprocal(rs4[:], rs4[:]), r=["rs4"], w=["rs4"])
            P.ts(rs4[:, 0:2, :], rs4[:, 0:2, :], 128 ** -0.5, None, ALU.mult, r=["rs4"], w=["rs4"])
            P.tt(qk[:], yc[:, 0:4, :], rs4[:], ALU.mult, r=["yc", "rs4"], w=["qk"])
            for hh in range(2):
                P.tr(B1[:, hh * 128:(hh + 1) * 128], qk[:, 2 + hh, :], identf[:], r=["qk", "identf"], w=["B1z"])
                P.tr(B1[:, 256 + hh * 128:256 + (hh + 1) * 128], yc[:, 4 + hh, :], identf[:], r=["yc", "identf"], w=["B1z"])
            P.cp(ktv[:].rearrange("p c t -> p (c t)"), B1[:], r=["B1z"], w=["ktv"])
            segs = []
            for hh in range(2 if STAGE >= 4 else 0):
                seg_start = len(P.ops)
                P.ksuf = "_%d" % hh
                HB = HT0 if hh == 0 else HT1
                gbc, gct, egs, t1, t2, Dcm, DTs, DTi, kb, kbT = (HB[n_] for n_ in HT_NAMES[:10])
                Mb = [HB["Mb0"], HB["Mb1"]]
                MTb = [HB["MTb0"], HB["MTb1"]]
                attnT, R, wT, vn, o2, kdk = (HB[n_] for n_ in HT_NAMES[14:])
                if hh == 0:
                    B2, B3, B4, B5 = PB2, PB3, PB4, PB5
                else:
                    B2, B3, B4, B5 = B0, B1, ptbf[0], ptbf[1]
                g = gt8[:, 2 * d + hh:2 * d + hh + 1]
                beta = gt8[:, 4 + 2 * d + hh:4 + 2 * d + hh + 1]
                qT = qk[:, hh, :]
                kT = qk[:, 2 + hh, :]
                k_tok = ktv[:, hh, :]
                v_tok = ktv[:, 2 + hh, :]
                P.ts(gbc[:], ones[:], g, None, ALU.mult, r=["ones", "gt8"], w=["gbc"])
                P.mm(B2[:, 0:128], gbc[:], Mm[:, iMin, :], r=["gbc", "Mm"], w=["B2"])
                P.mm(B3[:, 256:384], Mm[:, iMin, :], gbc[:], r=["Mm", "gbc"], w=["B3c"])
                P.cp(gct[:], B3[:, 256:257], r=["B3c"], w=["gct"])
                P.act(egs[:, 0:1], gct[:, 0:1], AF.Exp, r=["gct"], w=["egs0"])
                P.cp(egs[:, 3:4], B2[:, last:last + 1], r=["B2"], w=["egs3"])
                P.act(egs[:, 1:2], egs[:, 3:4], AF.Exp, r=["egs3"], w=["egs1"])
                P.ts(egs[:, 2:3], B2[:, last:last + 1], gct[:, 0:1], None, ALU.subtract, r=["B2", "gct"], w=["egs2"])
                P.act(egs[:, 2:3], egs[:, 2:3], AF.Exp, r=["egs2"], w=["egs2"])
                P.ts(t1[:], B2[:, 0:128], gct[:, 0:1], 0.0, ALU.subtract, ALU.max, r=["B2", "gct"], w=["t1"])
                P.act(Dcm[:], t1[:], AF.Exp, r=["t1"], w=["Dcm"], scale=-1.0)
                P.tt(Dcm[:], Dcm[:], Mm[:, iLst, :], ALU.mult, r=["Dcm", "Mm"], w=["Dcm"])
                P.ts(t2[:], B2[:, 0:128], gct[:, 0:1], 0.0, ALU.subtract, ALU.min, r=["B2", "gct"], w=["t2"])
                P.act(t2[:], t2[:], AF.Exp, r=["t2"], w=["t2"])
                P.tt(DTs[:], t2[:], Mm[:, iMst, :], ALU.mult, r=["t2", "Mm"], w=["DTs"])
                P.tt(DTi[:], t2[:], Mm[:, iMin, :], ALU.mult, r=["t2", "Mm"], w=["DTi"])
                P.ts(kb[:], k_tok, beta, None, ALU.mult, r=["ktv", "gt8"], w=["kb"])
                P.tr(B3[:, 0:128], kb[:], identf[:], r=["kb", "identf"], w=["B3a"])
                P.cp(kbT[:], B3[:, 0:128], r=["B3a"], w=["kbT"])
                P.mm(B2[:, 128:256], kbT[:], kT, r=["kbT", "qk"], w=["B2b"])
                P.mm(B2[:, 256:384], kT, kbT[:], r=["kbT", "qk"], w=["B2c"])
                P.mm(B2[:, 384:512], kT, qT, r=["qk"], w=["B2d"])
                P.stt(Mb[0][:], B2[:, 128:256], -1.0, Dcm[:], ALU.mult, ALU.mult, r=["B2b", "Dcm"], w=["Mb0"])
                P.stt(MTb[0][:], B2[:, 256:384], -1.0, DTs[:], ALU.mult, ALU.mult, r=["B2c", "DTs"], w=["MTb0"])
                P.tt(attnT[:], B2[:, 384:512], DTi[:], ALU.mult, r=["B2d", "DTi"], w=["attnT"])
                P.ts(R[:, 0:128], v_tok, beta, None, ALU.mult, r=["ktv", "gt8"], w=["R"])
                P.ts(R[:, 128:256], kb[:], egs[:, 0:1], None, ALU.mult, r=["kb", "egs0"], w=["R"])
                cur = 0
                for it in range(7):
                    P.mm(B4[:, 0:256], MTb[cur][:], R[:], r=["MTb%d" % cur, "R"], w=["B4"])
                    if it < 6:
                        P.mm(B5[:, 0:128], MTb[cur][:], Mb[cur][:], r=["MTb%d" % cur, "Mb%d" % cur], w=["B5a"])
                        P.mm(B5[:, 128:256], Mb[cur][:], MTb[cur][:], r=["MTb%d" % cur, "Mb%d" % cur], w=["B5b"])
                    P.tt(R[:], B4[:, 0:256], R[:], ALU.add, r=["B4", "R"], w=["R"])
                    if it < 6:
                        nx = 1 - cur
                        P.cp(Mb[nx][:], B5[:, 0:128], r=["B5a"], w=["Mb%d" % nx])
                        P.cp(MTb[nx][:], B5[:, 128:256], r=["B5b"], w=["MTb%d" % nx])
                        cur = nx
                P.tr(B3[:, 128:256], R[:, 128:256], identf[:], r=["R", "identf"], w=["B3b"])
                P.cp(wT[:], B3[:, 128:256], r=["B3b"], w=["wT"])
                P.mm(B5[:, 256:384], wT[:], Sst[:, hh, :], r=["wT", "Sst"], w=["B5c"])
                P.tt(vn[:], R[:, 0:128], B5[:, 256:384], ALU.subtract, r=["R", "B5c"], w=["vn"])
                P.mm(B5[:, 384:512], qT, Sst[:, hh, :], r=["qk", "Sst"], w=["B5d"])
                P.mm(B4[:, 256:384], attnT[:], vn[:], r=["attnT", "vn"], w=["B4c"])
                P.cp(o2[:], B4[:, 256:384], r=["B4c"], w=["o2"])
                P.stt(od[:, hh * 128:(hh + 1) * 128], B5[:, 384:512], egs[:, 0:1], o2[:], ALU.mult, ALU.add,
                      r=["B5d", "egs0", "o2"], w=["od"])
                P.ts(kdk[:], k_tok, egs[:, 2:3], None, ALU.mult, r=["ktv", "egs2"], w=["kdk"])
                P.mm(B4[:, 384:512], kdk[:], vn[:], r=["kdk", "vn"], w=["B4d"])
                P.stt(Sst[:, hh, :], Sst[:, hh, :], egs[:, 1:2], B4[:, 384:512], ALU.mult, ALU.add,
                      r=["Sst", "egs1", "B4d"], w=["Sst"])
                segs.append(P.ops[seg_start:])
                del P.ops[seg_start:]
                P.ksuf = ""
            if segs:
                B2, B3, B4, B5 = PB2, PB3, PB4, PB5
                for i_ in range(max(len(s_) for s_ in segs)):
                    for s_ in segs:
                        if i_ < len(s_):
                            P.ops.append(s_[i_])
            if d == 0:
                P.dma("sp", o_s[rows, :], od[:], r=["od_0", "od_1"], w=["o_s"], key="od_st")
            else:
                P.dma("sp", oprev[:], o_s[rows, :], r=["o_s"], w=["oprev"], key="oprev")
                P.dma("sp", zt[:], zs[rows, :], r=["zs"], w=["zt"], key="zt_ld")
                P.tt(od[:], od[:], oprev[:], ALU.add, r=["od_0", "od_1", "oprev"], w=["od_0", "od_1", "odf"])
                P.tt(oprev[:], od[:], od[:], ALU.mult, r=["odf"], w=["oprev"])
                P.op("dve", lambda e: e.reduce_sum(ss[:, 5:7], oprev[:].rearrange("p (h f) -> p h f", f=128), AX.X),
                     r=["oprev"], w=["ss57"])
                P.ts(ss[:, 5:7], ss[:, 5:7], 1.0 / 128, 1e-6, ALU.mult, ALU.add, r=["ss57"], w=["ss57"])
                P.act(ss[:, 5:7], ss[:, 5:7], AF.Sqrt, r=["ss57"], w=["ss57"])
                P.op("dve", lambda e: e.reciprocal(ss[:, 5:7], ss[:, 5:7]), r=["ss57"], w=["ss57"])
                for hh in range(2):
                    cs = slice(hh * 128, (hh + 1) * 128)
                    P.stt(od[:, cs], od[:, cs], ss[:, 5 + hh:6 + hh], dnbc[:], ALU.mult, ALU.mult, r=["odf", "ss57", "dnbc"], w=["odf", "od_0", "od_1"])
                P.tt(yb[:], od[:], zt[:], ALU.mult, r=["odf", "zt"], w=["yb"])
                P.dma("sp", mixp[rows, 256:512], yb[:], r=["yb"], key="mixp")
    P.out_dma.append("mixp")
    return P.finalize()


D = 2048


def _run(nc, in_maps):
    return run_bass_kernel_spmd(nc, in_maps, core_ids=list(range(8))).results


def _bc(v):
    v = np.asarray(v, np.float32).reshape(1, -1)
    return np.ascontiguousarray(np.broadcast_to(v, (128, v.shape[1])))


def _rope_table(S):
    GRID_W = 64
    rows = S // GRID_W
    row = np.repeat(np.arange(rows, dtype=np.float32), GRID_W)
    col = np.tile(np.arange(GRID_W, dtype=np.float32), rows)
    inv = (np.float32(10000.0) ** (-np.arange(16, dtype=np.float32) / np.float32(16))).astype(np.float32)
    ang = np.stack([row[:, None] * inv, col[:, None] * inv], axis=1).astype(np.float32)
    return np.concatenate([np.cos(ang).reshape(S, 32), np.sin(ang).reshape(S, 32)], axis=1).astype(np.float32)


def kernel(x, c, ctx, c_ctx, w_mod, b_mod, norm_g, e_w_in, e_w_out, e_q_gain, e_k_gain, e_sinks, e_conv_w,
           e_a_log, e_dt_bias, e_dn_norm, o_w_in, o_gate_w2, o_gate_b, o_gla_norm, o_w_out,
           w_router, b_router, w_gu, b_gu, w_down, b_down):
    f32 = lambda a: np.ascontiguousarray(np.asarray(a, dtype=np.float32))
    x, c, ctx, c_ctx, w_mod, b_mod, norm_g = map(f32, (x, c, ctx, c_ctx, w_mod, b_mod, norm_g))
    B, S, _ = x.shape
    CTXL = ctx.shape[1]
    DFF = w_gu.shape[-1] // 2
    NLc = S // 4 // 128
    CR = CTXL // 4
    NLT, NCT = S // 128, CTXL // 128
    TB = S + CTXL
    identb = np.eye(128).astype(NPBF)
    identf = np.eye(128, dtype=np.float32)
    o = np.ones((128, 128), np.float32)
    M4 = np.stack([np.triu(o), np.tril(o), np.triu(o, 1), np.tril(o, -1)])
    cv = np.stack([c[0], c[1], c_ctx])
    cT = np.ascontiguousarray(cv.reshape(3, 16, 128).transpose(2, 1, 0).reshape(128, 48))
    ims = []
    for cc in range(8):
        cs = slice(cc * 1536, (cc + 1) * 1536)
        ims.append(dict(cT=cT, wm=np.ascontiguousarray(w_mod[:, :, cs]), bm=np.ascontiguousarray(b_mod[:, cs]).reshape(1, 2 * 1536)))
    r = _run(build_mod(), ims)
    mod = np.concatenate([r[cc]["modo"] for cc in range(8)], axis=2).reshape(2, 3, 6, D)

    def tok_rows(cc):
        b, q = cc // 4, cc % 4
        return b, slice(q * (S // 4), (q + 1) * (S // 4)), slice(q * CR, (q + 1) * CR)

    NT0 = NLc + 1
    xins, ims = [], []
    for cc in range(8):
        b, ls, cs = tok_rows(cc)
        xin = np.zeros((NT0 * 128, D), np.float32)
        xin[:NLc * 128] = x[b, ls]
        xin[NLc * 128:NLc * 128 + CR] = ctx[b, cs]
        xins.append(xin)
        modn = np.ascontiguousarray(np.stack([mod[0, b, 0:2], mod[0, 2, 0:2]]))
        ims.append(dict(xin=xin, g=norm_g[0, 0], modn=modn))
    r = _run(build_pre(NT0, CR), ims)

    def gather_h(res, key):
        hb = np.zeros((B, TB, D), NPBF)
        for cc in range(8):
            b, ls, cs = tok_rows(cc)
            hb[b, ls] = res[cc][key][:NLc * 128]
            hb[b, S + cs.start:S + cs.stop] = res[cc][key][NLc * 128:NLc * 128 + CR]
        return hb

    hb = gather_h(r, "hout")
    W0 = f32(e_w_in[0])
    cwT = f32(e_conv_w[0])
    rope = _rope_table(S)
    Mc = np.ascontiguousarray(np.concatenate([M4.transpose(1, 0, 2).reshape(128, 512),
                                              rope.reshape(NLT, 128, 64).transpose(1, 0, 2).reshape(128, NLT * 64)], axis=1))
    ims = []
    for cc in range(8):
        b, j = cc // 4, cc % 4
        base = 1536
        cols = np.concatenate([np.arange(j * 256, (j + 1) * 256), 1024 + np.arange(j * 64, (j + 1) * 64),
                               1280 + np.arange(j * 64, (j + 1) * 64),
                               base + np.arange(2 * j * 128, (2 * j + 2) * 128),
                               base + 1024 + np.arange(2 * j * 128, (2 * j + 2) * 128),
                               base + 2048 + np.arange(2 * j * 128, (2 * j + 2) * 128),
                               base + 3072 + np.arange(2 * j * 128, (2 * j + 2) * 128),
                               5632 + np.arange(2 * j, 2 * j + 2), 5640 + np.arange(2 * j, 2 * j + 2),
                               5648 + np.arange(2 * j, 2 * j + 2), 5656 + np.arange(2 * j, 2 * j + 2)])
        ccols = np.concatenate([np.arange(2 * j * 128, (2 * j + 2) * 128), 1024 + np.arange(2 * j * 128, (2 * j + 2) * 128),
                                2048 + np.arange(2 * j * 128, (2 * j + 2) * 128)])
        cw = np.ascontiguousarray(cwT[:, ccols].T.reshape(6, 128, 5).transpose(1, 0, 2).reshape(128, 30))
        gpar = np.concatenate([np.asarray(e_a_log, np.float32)[0, 0, 2 * j:2 * j + 2], np.asarray(e_a_log, np.float32)[0, 1, 2 * j:2 * j + 2],
                               np.asarray(e_dt_bias, np.float32)[0, 0, 2 * j:2 * j + 2], np.asarray(e_dt_bias, np.float32)[0, 1, 2 * j:2 * j + 2]])
        ims.append(dict(h=np.ascontiguousarray(hb[b]), wc=np.ascontiguousarray(W0[:, cols]),
                        qkg=_bc(np.concatenate([np.asarray(e_q_gain, np.float32)[0]] * 4 + [np.asarray(e_k_gain, np.float32)[0]])),
                        sinks=_bc(np.asarray(e_sinks, np.float32)[0, 4 * j:4 * j + 4]), cw=cw, gpar=_bc(gpar),
                        dnn=_bc(np.asarray(e_dn_norm, np.float32)[0]), M=Mc, identb=identb, identf=identf))
    r = _run(build_mix0(NLT, NCT), ims)
    mixf = np.zeros((B, TB, D), NPBF)
    for cc in range(8):
        b, j = cc // 4, cc % 4
        mixf[b, :, j * 256:(j + 1) * 256] = r[cc]["mixp"][:, 0:256]
        mixf[b, :, 1024 + j * 256:1024 + (j + 1) * 256] = r[cc]["mixp"][:, 256:512]

    def post(layer, last, xin_list, mixfull, wout):
        NT = NLc if last else NLc + 1
        wr = f32(w_router[layer]); br = f32(b_router[layer]).reshape(1, 32)
        wgu = f32(w_gu[layer]); bgu = f32(b_gu[layer]); wdn = f32(w_down[layer]); bdn = f32(b_down[layer])
        wo = f32(wout)
        ims = []
        for cc in range(8):
            b, ls, cs = tok_rows(cc)
            mix = np.zeros((NT * 128, D), NPBF)
            mix[:NLc * 128] = mixfull[b, ls]
            if not last:
                mix[NLc * 128:NLc * 128 + CR] = mixfull[b, S + cs.start:S + cs.stop]
            modv = np.ascontiguousarray(np.stack([mod[layer, b], mod[layer, 2]]))
            im = dict(xin=xin_list[cc], mix=mix, modv=modv, g2=norm_g[layer, 1], wout=wo, wr=wr, br=br, wgu=wgu, bgu=bgu,
                      wdn=wdn, bdn=bdn, identb=identb, identf=identf)
            if not last:
                im["gn"] = norm_g[layer + 1, 0]
                im["modn"] = np.ascontiguousarray(np.stack([mod[layer + 1, b, 0:2], mod[layer + 1, 2, 0:2]]))
            ims.append(im)
        return _run(build_post(NT, DFF, last, 0 if last else CR), ims)

    r = post(0, False, xins, mixf, e_w_out[0])
    x1 = [np.ascontiguousarray(r[cc]["xout"][:NLc * 128]) for cc in range(8)]
    hb = gather_h(r, "hnext")
    W1 = f32(o_w_in[0]); gw2 = f32(o_gate_w2[0]); gb = f32(o_gate_b[0])
    U2 = np.ascontiguousarray(M4[0:2])
    ims = []
    for cc in range(8):
        b, j = cc // 4, cc % 4
        cols = np.concatenate([np.arange(j * 256, (j + 1) * 256), 1024 + np.arange(j * 256, (j + 1) * 256),
                               2048 + np.arange(j * 512, (j + 1) * 512), 4096 + np.arange(j * 512, (j + 1) * 512),
                               np.arange(6144, 6176)])
        w2 = np.ascontiguousarray(np.concatenate([gw2[:, :, j * 256:(j + 1) * 256], gb[:, None, j * 256:(j + 1) * 256]], axis=1))
        ims.append(dict(h=np.ascontiguousarray(hb[b]), wc=np.ascontiguousarray(W1[:, cols]), w2=w2,
                        gnorm=f32(o_gla_norm[0]), identb=identb, U=U2))
    r = _run(build_gla(NLT, NCT), ims)
    mixf = np.zeros((B, TB, D), NPBF)
    for cc in range(8):
        b, j = cc // 4, cc % 4
        mixf[b, :, j * 512:(j + 1) * 512] = r[cc]["mixp"]
    r = post(1, True, x1, mixf, o_w_out[0])
    out = np.zeros((B, S, D), np.float32)
    for cc in range(8):
        b, ls, cs = tok_rows(cc)
        out[b, ls] = r[cc]["xout"][:NLc * 128]
    return out
```

```python
import os
import numpy as np
import ml_dtypes
from contextlib import ExitStack
import concourse.bass as bass
import concourse.mybir as mybir
from concourse.bass_utils import run_bass_kernel_spmd

F32 = mybir.dt.float32
BF16 = mybir.dt.bfloat16
I32 = mybir.dt.int32
U32 = mybir.dt.uint32
AF = mybir.ActivationFunctionType
ALU = mybir.AluOpType
AX = mybir.AxisListType
NPBF = ml_dtypes.bfloat16

SEM_ROT = 20000


class _Op:
    __slots__ = ("stream", "fn", "r", "w", "dma", "deps", "sig", "need")

    def __init__(self, stream, fn, r, w, dma):
        self.stream = stream
        self.fn = fn
        self.r = r
        self.w = w
        self.dma = dma
        self.deps = None
        self.sig = None
        self.need = False


class Prog:
    def __init__(self):
        self.nc = bass.Bass("TRN2", target_bir_lowering=False)
        self.es = ExitStack()
        self.ops = []
        self.out_dma = []
        self.keymap = {}
        self.ksuf = ""
        self.ksuf_keys = set()

    def din(self, name, shape, dt):
        return self.nc.dram_tensor(name, list(shape), dt, kind="ExternalInput").ap()

    def dout(self, name, shape, dt):
        return self.nc.dram_tensor(name, list(shape), dt, kind="ExternalOutput").ap()

    def dtmp(self, name, shape, dt):
        return self.nc.dram_tensor(name, list(shape), dt, kind="Internal").ap()

    def sb(self, name, shape, dt):
        return self.es.enter_context(self.nc.sbuf_tensor(name, list(shape), dt))

    def ps(self, name, shape, dt):
        return self.es.enter_context(self.nc.psum_tensor(name, list(shape), dt))

    def _km(self, keys):
        out = []
        for k in keys:
            if self.ksuf and k in self.ksuf_keys:
                k = k + self.ksuf
            k = self.keymap.get(k, k)
            if k not in out:
                out.append(k)
        return tuple(out)

    def op(self, stream, fn, r=(), w=()):
        self.ops.append(_Op(stream, fn, self._km(r), self._km(w), None))

    def dma(self, stream, out, in_, r=(), w=(), key=None, **kw):
        assert key is not None
        self.ops.append(_Op(stream, lambda e: e.dma_start(out=out, in_=in_, **kw), self._km(r), self._km(w), key))

    def dmaf(self, stream, fn, r=(), w=(), key=None):
        assert key is not None
        self.ops.append(_Op(stream, fn, self._km(r), self._km(w), key))

    def mm(self, out, lhsT, rhs, start=True, stop=True, r=(), w=(), **kw):
        self.op("pe", lambda e: e.matmul(out, lhsT, rhs, start=start, stop=stop, **kw), r, w)

    def tr(self, out, in_, ident, r=(), w=()):
        self.op("pe", lambda e: e.transpose(out, in_, ident), r, w)

    def act(self, out, in_, func, r=(), w=(), stream="act", **kw):
        self.op(stream, lambda e: e.activation(out=out, in_=in_, func=func, **kw), r, w)

    def tt(self, out, in0, in1, op, r=(), w=(), stream="dve"):
        self.op(stream, lambda e: e.tensor_tensor(out, in0, in1, op), r, w)

    def ts(self, out, in0, s1, s2, op0, op1=None, r=(), w=(), stream="dve", **kw):
        if op1 is None:
            self.op(stream, lambda e: e.tensor_scalar(out, in0, s1, None, op0, **kw), r, w)
        else:
            self.op(stream, lambda e: e.tensor_scalar(out, in0, s1, s2, op0, op1, **kw), r, w)

    def stt(self, out, in0, scalar, in1, op0, op1, r=(), w=(), stream="dve", **kw):
        self.op(stream, lambda e: e.scalar_tensor_tensor(out, in0, scalar, in1, op0, op1, **kw), r, w)

    def cp(self, out, in_, r=(), w=(), stream="dve"):
        if stream == "act":
            self.op(stream, lambda e: e.copy(out, in_), r, w)
        else:
            self.op(stream, lambda e: e.tensor_copy(out, in_), r, w)

    def memset(self, ap, val, w=(), stream="dve"):
        self.op(stream, lambda e: e.memset(ap, val), (), w)

    def finalize(self):
        nc = self.nc
        ops = self.ops
        state = {}

        def dom(o):
            return ("dma", o.dma) if o.dma is not None else o.stream

        for i, o in enumerate(ops):
            deps = set()
            d_i = dom(o)
            for k in o.r:
                st = state.setdefault(k, ({}, {}))
                deps.update(st[0].values())
                st[1][d_i] = i
            for k in o.w:
                st = state.setdefault(k, ({}, {}))
                wr, rd = st
                others_r = {d: j for d, j in rd.items() if j != i}
                if others_r:
                    for d, j in others_r.items():
                        deps.add(j)
                    for d, j in wr.items():
                        if d != d_i or (o.dma is None and d_i != "pe"):
                            deps.add(j)
                    wr.clear()
                    rd.clear()
                    wr[d_i] = i
                else:
                    for d, j in wr.items():
                        if d != d_i or (o.dma is None and d_i != "pe"):
                            deps.add(j)
                    rd.clear()
                    wr[d_i] = i
            deps.discard(i)
            o.deps = deps
            for j in deps:
                ops[j].need = True
        eng_cnt = {}
        sem_of = {}

        def get_sem(key):
            if key not in sem_of:
                sem_of[key] = self.es.enter_context(nc.semaphore("s%d" % len(sem_of)))
            return sem_of[key]

        dma_cum = {}
        for o in ops:
            if o.dma is not None:
                dma_cum[o.dma] = dma_cum.get(o.dma, 0) + 16
                o.sig = (("dma", o.dma), dma_cum[o.dma])
            elif o.need:
                c = eng_cnt.get(o.stream, 0) + 1
                eng_cnt[o.stream] = c
                o.sig = ((o.stream, (c - 1) // SEM_ROT), (c - 1) % SEM_ROT + 1)
        streams = {}
        for i, o in enumerate(ops):
            streams.setdefault(o.stream, []).append(i)
        waits = [None] * len(ops)
        waited = {}
        for i, o in enumerate(ops):
            wl = {}
            for j in o.deps:
                sk, val = ops[j].sig
                if wl.get(sk, 0) < val:
                    wl[sk] = val
            ws = waited.setdefault(o.stream, {})
            out = []
            for sk, val in wl.items():
                if ws.get(sk, 0) < val:
                    ws[sk] = val
                    out.append((sk, val))
            waits[i] = out
        for sk in set(s for w in waits for s, _ in w):
            get_sem(sk)
        for o in ops:
            if o.sig is not None:
                get_sem(o.sig[0])
        final = [(("dma", k), dma_cum[k]) for k in self.out_dma if k in dma_cum]

        def emit(stream, e):
            for i in streams.get(stream, []):
                o = ops[i]
                for sk, val in waits[i]:
                    e.wait_ge(sem_of[sk], val)
                ins = o.fn(e)
                if o.sig is not None:
                    sk, _ = o.sig
                    ins.then_inc(sem_of[sk], 16 if o.dma is not None else 1)
            if stream == "sp":
                for sk, val in final:
                    e.wait_ge(sem_of[sk], val)

        with nc.Block() as block:
            @block.sync
            def _(e):
                emit("sp", e)

            @block.tensor
            def _(e):
                emit("pe", e)

            @block.vector
            def _(e):
                emit("dve", e)

            @block.scalar
            def _(e):
                emit("act", e)

            @block.gpsimd
            def _(e):
                emit("pool", e)
        self.es.close()
        self.n_sems = len(sem_of)
        return nc


D = 2048
KD = 16
EPS = 1e-6


def build_mod():
    P = Prog()
    cT = P.din("cT", [128, KD * 3], F32)
    wm = P.din("wm", [2, D, 1536], F32)
    bm = P.din("bm", [1, 2 * 1536], F32)
    out = P.dout("modo", [2, 3, 1536], F32)
    sc = P.sb("sc", [128, KD, 3], F32)
    wms = P.sb("wms", [128, KD, 1536], F32)
    bms = P.sb("bms", [1, 2, 1536], F32)
    ones3 = P.sb("ones3", [1, 3], F32)
    res = P.sb("res", [3, 1536], F32)
    ps = P.ps("pm", [128, 512], F32)
    P.dma("sp", sc[:].rearrange("p k r -> p (k r)"), cT, w=["sc"], key="sc")
    P.act(sc[:].rearrange("p k r -> p (k r)"), sc[:].rearrange("p k r -> p (k r)"), AF.Silu, r=["sc"], w=["sc"])
    P.dma("sp", bms[:].rearrange("o l c -> o (l c)"), bm, w=["bms"], key="bms")
    P.memset(ones3[:], 1.0, w=["ones3"])
    for l in range(2):
        for k in range(KD):
            P.dma("sp", wms[:, k, :], wm[l, k * 128:(k + 1) * 128, :], w=["wms"], key="wms")
        for cc in range(3):
            cs = slice(cc * 512, (cc + 1) * 512)
            for k in range(KD):
                P.mm(ps[0:3, :], sc[:, k, :], wms[:, k, cs], start=(k == 0), stop=False, r=["sc", "wms"], w=["pm"])
            P.mm(ps[0:3, :], ones3[:, :], bms[:, l, cs], start=False, stop=True, r=["ones3", "bms"], w=["pm"])
            P.cp(res[:, cs], ps[0:3, :], r=["pm"], w=["res"])
        P.dma("sp", out[l], res[:], r=["res"], key="modo")
    P.out_dma.append("modo")
    return P.finalize()


def build_pre(NT, ctx_rows):
    P = Prog()
    R = NT * 128
    xin = P.din("xin", [R, D], F32)
    g = P.din("g", [D], F32)
    modn = P.din("modn", [2, 2, D], F32)
    hout = P.dout("hout", [R, D], BF16)
    bcB = P.sb("bcB", [128, D], F32)
    bcC = P.sb("bcC", [128, D], F32)
    xt = P.sb("xt", [128, D], F32)
    tmp = P.sb("tmp", [128, D], F32)
    hn = P.sb("hn", [128, D], BF16)
    sm = P.sb("sm", [128, 2], F32)

    def set_mod(kind):
        P.dma("sp", bcB[:], modn[kind, 1, :].partition_broadcast(128), w=["bcB"], key="bcB")
        P.dma("sp", bcC[:], g.partition_broadcast(128), w=["bcC"], key="bcC")
        P.stt(bcB[:], bcB[:], 1.0, bcC[:], ALU.add, ALU.mult, r=["bcB", "bcC"], w=["bcB"])
        P.dma("sp", bcC[:], modn[kind, 0, :].partition_broadcast(128), w=["bcC"], key="bcC")

    set_mod(0)
    for t in range(NT):
        if ctx_rows > 0 and t == NT - 1:
            set_mod(1)
        rows = slice(t * 128, (t + 1) * 128)
        P.dma("sp", xt[:], xin[rows, :], w=["xt"], key="xt")
        P.memset(sm[:, 0:1], 0.0, w=["sm"])
        P.act(tmp[:], xt[:], AF.Square, r=["xt"], w=["tmp", "sm"], accum_out=sm[:, 0:1])
        P.ts(sm[:, 0:1], sm[:, 0:1], 1.0 / D, EPS, ALU.mult, ALU.add, r=["sm"], w=["sm"])
        P.act(sm[:, 0:1], sm[:, 0:1], AF.Sqrt, r=["sm"], w=["sm"])
        P.op("dve", lambda e: e.reciprocal(sm[:, 0:1], sm[:, 0:1]), r=["sm"], w=["sm"])
        P.stt(tmp[:], xt[:], sm[:, 0:1], bcB[:], ALU.mult, ALU.mult, r=["xt", "sm", "bcB"], w=["tmp"])
        P.tt(hn[:], tmp[:], bcC[:], ALU.add, r=["tmp", "bcC"], w=["hn"])
        P.dma("sp", hout[rows, :], hn[:], r=["hn"], key="hout")
    P.out_dma.append("hout")
    return P.finalize()


D = 2048
KD = 16
NE = 32
EPS = 1e-6


def bc_row(ap_row, n):
    return ap_row.partition_broadcast(128)


def build_post(NT, DFF, last, ctx_rows):
    P = Prog()
    R = NT * 128
    KF = DFF // 128
    NCG = DFF // 256
    xin = P.din("xin", [R, D], F32)
    mix = P.din("mix", [R, D], BF16)
    modv = P.din("modv", [2, 6, D], F32)
    g2 = P.din("g2", [D], F32)
    wout = P.din("wout", [D, D], F32)
    wr = P.din("wr", [D, NE], F32)
    br = P.din("br", [1, NE], F32)
    wgu = P.din("wgu", [NE, D, 2 * DFF], F32)
    bgu = P.din("bgu", [NE, 2 * DFF], F32)
    wdn = P.din("wdn", [NE, DFF, D], F32)
    bdn = P.din("bdn", [NE, D], F32)
    identb_d = P.din("identb", [128, 128], BF16)
    identf_d = P.din("identf", [128, 128], F32)
    if not last:
        gn = P.din("gn", [D], F32)
        modn = P.din("modn", [2, 2, D], F32)
        hnext = P.dout("hnext", [R, D], BF16)
    xout = P.dout("xout", [R, D], F32)
    wo_s = P.dtmp("wo_s", [4, 128, KD * 512], BF16)
    wg_l = [P.dtmp("wg_s%d" % e, [NCG, 128, KD * 512], BF16) for e in range(NE)]
    wd_l = [P.dtmp("wd_s%d" % e, [4, 128, KF * 512], BF16) for e in range(NE)]
    x1_s = P.dtmp("x1_s", [R, D], F32)
    h2_s = P.dtmp("h2_s", [R, D], BF16)
    wbuf = P.sb("wbuf", [128, 2, KD * 512], BF16)
    BA = P.sb("BA", [128, 32768], BF16)
    FA = P.sb("FA", [128, 8192], F32)
    bcA = P.sb("bcA", [128, D], F32)
    bcB = P.sb("bcB", [128, D], F32)
    bcC = P.sb("bcC", [128, D], F32)
    xt3 = P.sb("xt3", [128, D], F32)
    hn = P.sb("hn", [128, D], BF16)
    bgT = P.sb("bgT", [128, 2 * (DFF // 128), NE], F32)
    Bd = P.sb("Bd", [NE, D], F32)
    G = P.sb("G", [128, NT, NE], F32)
    GT = P.sb("GT", [NE, 512], F32)
    GTb = P.sb("GTb", [NE, 512], BF16)
    identb = P.sb("identb_s", [128, 128], BF16)
    identf = P.sb("identf_s", [128, 128], F32)
    wrs = P.sb("wrs", [128, KD, NE], F32)
    brs = P.sb("brs", [1, NE], F32)
    ones1 = P.sb("ones1", [1, 128], F32)
    sm = P.sb("sm", [128, 16], F32)
    mx8 = P.sb("mx8", [128, 8], F32)
    lg = P.sb("lg", [128, NE], F32)
    msk = P.sb("msk", [128, NE], F32)
    gl = P.sb("gl", [128, 512], F32)
    sg = P.sb("sg", [128, 512], F32)
    ln = P.sb("ln", [128, 512], F32)
    ptb = [P.ps("ptb%d" % i, [128, 1024], BF16) for i in range(2)]
    pf = [P.ps("pf%d" % i, [128, 512], F32) for i in range(2)]
    pg = [P.ps("pg%d" % i, [128, 512], F32) for i in range(2)]
    py = [P.ps("py%d" % i, [128, 512], F32) for i in range(2)]

    mixt = BA[:, 0:2048]
    mixT = BA[:, 2048:4096]
    h2b = BA[:, 4096:6144]
    xt = FA[:, 0:2048]
    h2 = FA[:, 2048:4096]
    h2T32 = FA[:, 4096:6144]
    P1KEYS = ["mixt", "mixT", "h2b", "xt", "h2", "h2T32"]

    P.dma("sp", identb[:], identb_d, w=["identb"], key="identb")
    P.dma("sp", identf[:], identf_d, w=["identf"], key="identf")
    P.dma("sp", wrs[:], wr.rearrange("(k p) e -> p k e", p=128), w=["wrs"], key="wrs")
    P.dma("sp", brs[:], br, w=["brs"], key="brs")
    P.dma("sp", Bd[:], bdn, w=["Bd"], key="Bd")
    P.dma("sp", FA[0:NE, 0:2 * DFF], bgu, w=["xt", "h2"], key="bgst")
    for c_ in range(2 * (DFF // 128)):
        P.tr(pf[c_ % 2][:, 0:NE], FA[0:NE, c_ * 128:(c_ + 1) * 128], identf[0:NE, 0:NE], r=["xt", "h2", "identf"], w=["pf%d" % (c_ % 2)])
        P.cp(bgT[:, c_, :], pf[c_ % 2][:, 0:NE], r=["pf%d" % (c_ % 2)], w=["bgT"])
    P.memset(ones1[:], 1.0, w=["ones1"])
    P.memset(G[:], 0.0, w=["G"])

    stg = 0

    def cast_piece(dst, srcs):
        nonlocal stg
        s = stg % 2
        stg += 1
        key = "wp%d" % s
        for (lo, hi, src) in srcs:
            P.dma("pool", wbuf[:, s, :].rearrange("p (k c) -> p k c", c=512)[:, :src.shape[1], lo:hi], src,
                  w=[key], key=key + "l")
        P.dma("sp", dst, wbuf[:, s, 0:dst.shape[1]], r=[key], w=["wscr"], key=key + "s")

    for dc in range(4):
        cast_piece(wo_s[dc], [(0, 512, wout[:, dc * 512:(dc + 1) * 512].rearrange("(k p) c -> p k c", p=128))])
    for e in range(NE):
        for cg in range(NCG):
            cast_piece(wg_l[e][cg], [
                (0, 256, wgu[e, :, cg * 256:(cg + 1) * 256].rearrange("(k p) c -> p k c", p=128)),
                (256, 512, wgu[e, :, DFF + cg * 256:DFF + (cg + 1) * 256].rearrange("(k p) c -> p k c", p=128))])
        for dc in range(4):
            cast_piece(wd_l[e][dc], [(0, 512, wdn[e, :, dc * 512:(dc + 1) * 512].rearrange("(k p) c -> p k c", p=128))])

    wslot = [0]

    def slot(s):
        return wbuf[:, s, :] if s < 2 else BA[:, 16384:24576]

    def load_piece(src, extra_w=()):
        s = wslot[0] % 3
        wslot[0] += 1
        key = "wp%d" % s
        P.dma("sp", slot(s)[:, 0:src.shape[1]], src, r=["wscr"], w=[key] + list(extra_w), key=key + "l")
        return s, key

    def load_bc(tile, row_ap, key):
        P.dma("sp", tile[:], row_ap.partition_broadcast(128), w=[key], key=key)

    def set_mod(kind):
        load_bc(bcA, modv[kind, 2, :], "bcA")
        load_bc(bcB, modv[kind, 4, :], "bcB")
        load_bc(bcC, g2, "bcC")
        P.stt(bcB[:], bcB[:], 1.0, bcC[:], ALU.add, ALU.mult, r=["bcB", "bcC"], w=["bcB"])
        load_bc(bcC, modv[kind, 3, :], "bcC")

    def rstd_from(src, key_src, col):
        P.memset(sm[:, col:col + 1], 0.0, w=["sm%d" % col])
        P.act(h2[:], src, AF.Square, r=[key_src], w=["h2", "sm%d" % col], accum_out=sm[:, col:col + 1])
        P.ts(sm[:, col:col + 1], sm[:, col:col + 1], 1.0 / D, EPS, ALU.mult, ALU.add, r=["sm%d" % col], w=["sm%d" % col])
        P.act(sm[:, col:col + 1], sm[:, col:col + 1], AF.Sqrt, r=["sm%d" % col], w=["sm%d" % col])
        P.op("dve", lambda e: e.reciprocal(sm[:, col:col + 1], sm[:, col:col + 1]), r=["sm%d" % col], w=["sm%d" % col])

    set_mod(0)
    for t in range(NT):
        is_ctx = ctx_rows > 0 and t == NT - 1
        if is_ctx:
            set_mod(1)
        rows = slice(t * 128, (t + 1) * 128)
        P.dma("sp", mixt, mix[rows, :], w=["mixt"], key="mixt")
        P.dma("sp", xt, xin[rows, :], w=["xt"], key="xt")
        for j in range(2):
            for i in range(8):
                k = j * 8 + i
                P.tr(ptb[j][:, i * 128:(i + 1) * 128], mixt[:, k * 128:(k + 1) * 128], identb[:],
                     r=["mixt", "identb"], w=["ptb%d" % j])
            P.cp(mixT[:, j * 1024:(j + 1) * 1024], ptb[j][:], r=["ptb%d" % j], w=["mixT"], stream="act" if j else "dve")
        for dc in range(4):
            s, key = load_piece(wo_s[dc])
            wv = slot(s).rearrange("p (k c) -> p k c", c=512)
            ps = pf[dc % 2]
            pk = "pf%d" % (dc % 2)
            for k in range(KD):
                P.mm(ps[:], mixT[:, k * 128:(k + 1) * 128], wv[:, k, :], start=(k == 0), stop=(k == KD - 1),
                     r=["mixT", key], w=[pk])
            cs = slice(dc * 512, (dc + 1) * 512)
            P.tt(h2[:, cs], ps[:], bcA[:, cs], ALU.mult, r=[pk, "bcA"], w=["h2"])
            P.tt(xt[:, cs], h2[:, cs], xt[:, cs], ALU.add, r=["h2", "xt"], w=["xt"])
        P.dma("sp", x1_s[rows, :], xt, r=["xt"], w=["x1_s"], key="xt_st")
        rstd_from(xt, "xt", 0)
        P.stt(h2[:], xt, sm[:, 0:1], bcB[:], ALU.mult, ALU.mult, r=["xt", "sm0", "bcB"], w=["h2"])
        P.tt(h2[:], h2[:], bcC[:], ALU.add, r=["h2", "bcC"], w=["h2"])
        P.cp(h2b, h2[:], r=["h2"], w=["h2b"], stream="act")
        P.dma("sp", h2_s[rows, :], h2b, r=["h2b"], w=["h2_s"], key="h2b_st")
        if last and False:
            pass
        for j in range(4):
            for i in range(4):
                k = j * 4 + i
                P.tr(pf[j % 2][:, i * 128:(i + 1) * 128], h2[:, k * 128:(k + 1) * 128], identf[:],
                     r=["h2", "identf"], w=["pf%d" % (j % 2)])
            P.cp(h2T32[:, j * 512:(j + 1) * 512], pf[j % 2][:], r=["pf%d" % (j % 2)], w=["h2T32"],
                 stream="act" if j % 2 else "dve")
        for k in range(KD):
            P.mm(pg[0][:, 0:NE], h2T32[:, k * 128:(k + 1) * 128], wrs[:, k, :], start=(k == 0), stop=False,
                 r=["h2T32", "wrs"], w=["pg0"])
        P.mm(pg[0][:, 0:NE], ones1[:, :], brs[:, :], start=False, stop=True, r=["ones1", "brs"], w=["pg0"])
        P.cp(lg[:], pg[0][:, 0:NE], r=["pg0"], w=["lg"])
        P.op("dve", lambda e: e.max(mx8[:], lg[:]), r=["lg"], w=["mx8"])
        P.ts(msk[:], lg[:], mx8[:, 3:4], None, ALU.is_ge, r=["lg", "mx8"], w=["msk"])
        P.ts(sm[:, 2:3], mx8[:, 0:1], -1.0, None, ALU.mult, r=["mx8"], w=["sm2"])
        P.act(lg[:], lg[:], AF.Exp, r=["lg", "sm2"], w=["lg"], bias=sm[:, 2:3], scale=1.0)
        P.tt(lg[:], lg[:], msk[:], ALU.mult, r=["lg", "msk"], w=["lg"])
        P.op("dve", lambda e: e.reduce_sum(sm[:, 3:4], lg[:], AX.X), r=["lg"], w=["sm3"])
        P.op("dve", lambda e: e.reciprocal(sm[:, 3:4], sm[:, 3:4]), r=["sm3"], w=["sm3"])
        nv = ctx_rows if is_ctx else 128
        P.ts(G[0:nv, t, :], lg[0:nv, :], sm[0:nv, 3:4], None, ALU.mult, r=["lg", "sm3"], w=["G"])

    h2g = BA[:, 0:8192]
    h2T = BA[:, 8192:16384]
    actb = BA[:, 16384:24576]
    actT = BA[:, 24576:32768]
    acc = FA[:, 0:8192]
    first = True
    ngroups = (NT + 3) // 4
    for g in range(ngroups):
        t0 = g * 4
        nt = min(4, NT - t0)
        NTOK = nt * 128
        extra = P1KEYS if first else []
        for tt in range(nt):
            P.dma("sp", h2g[:, tt * 2048:(tt + 1) * 2048], h2_s[(t0 + tt) * 128:(t0 + tt + 1) * 128, :],
                  r=["h2_s"], w=["h2g"] + (extra if tt == 0 else []), key="h2g")
        h2Tv = h2T.rearrange("p (k t) -> p k t", t=512)
        for tt in range(nt):
            for j in range(2):
                for i in range(8):
                    k = j * 8 + i
                    P.tr(ptb[j][:, i * 128:(i + 1) * 128], h2g[:, tt * 2048 + k * 128: tt * 2048 + (k + 1) * 128],
                         identb[:], r=["h2g", "identb"], w=["ptb%d" % j])
                P.cp(h2Tv[:, j * 8:(j + 1) * 8, tt * 128:(tt + 1) * 128],
                     ptb[j][:].rearrange("p (k t) -> p k t", t=128), r=["ptb%d" % j],
                     w=["h2T"] + (extra if (tt == 0 and j == 0) else []), stream="act" if j else "dve")
        for tt in range(nt):
            P.tr(pf[0][0:NE, tt * 128:(tt + 1) * 128], G[:, t0 + tt, :], identf[:], r=["G", "identf"], w=["pf0"])
        P.cp(GT[:, 0:NTOK], pf[0][0:NE, 0:NTOK], r=["pf0"], w=["GT"])
        for tt in range(nt):
            for dc in range(4):
                ps = py[dc % 2]
                pk = "py%d" % (dc % 2)
                P.mm(ps[:], GT[:, tt * 128:(tt + 1) * 128], Bd[:, dc * 512:(dc + 1) * 512], r=["GT", "Bd"], w=[pk])
                P.cp(acc[:, tt * 2048 + dc * 512: tt * 2048 + (dc + 1) * 512], ps[:], r=[pk],
                     w=["acc"] + (extra if (tt == 0 and dc == 0) else []), stream="act" if dc % 2 else "dve")
        first = False
        for e in range(NE):
            actTv = actT.rearrange("p (k t) -> p k t", t=512)
            for cg in range(NCG):
                s, key = load_piece(wg_l[e][cg])
                wv = slot(s).rearrange("p (k c) -> p k c", c=512)
                for jj in range(2):
                    j = cg * 2 + jj
                    pgl, kgl = pg[j % 2], "pg%d" % (j % 2)
                    pln, kln = pf[j % 2], "pf%d" % (j % 2)
                    for k in range(KD):
                        P.mm(pgl[:, 0:NTOK], wv[:, k, jj * 128:(jj + 1) * 128], h2Tv[:, k, 0:NTOK], start=(k == 0),
                             stop=(k == KD - 1), r=["h2T", key], w=[kgl])
                    for k in range(KD):
                        P.mm(pln[:, 0:NTOK], wv[:, k, 256 + jj * 128:256 + (jj + 1) * 128], h2Tv[:, k, 0:NTOK], start=(k == 0),
                             stop=(k == KD - 1), r=["h2T", key], w=[kln])
                    P.ts(gl[:, 0:NTOK], pgl[:, 0:NTOK], bgT[:, j, e:e + 1], 7.0, ALU.add, ALU.min, r=[kgl, "bgT"], w=["gl"])
                    P.act(sg[:, 0:NTOK], gl[:, 0:NTOK], AF.Sigmoid, r=["gl"], w=["sg"], scale=1.702)
                    P.ts(ln[:, 0:NTOK], pln[:, 0:NTOK], bgT[:, KF + j, e:e + 1], 7.0, ALU.add, ALU.min, r=[kln, "bgT"], w=["ln"])
                    P.ts(ln[:, 0:NTOK], ln[:, 0:NTOK], -7.0, 1.0, ALU.max, ALU.add, r=["ln"], w=["ln"])
                    P.tt(gl[:, 0:NTOK], gl[:, 0:NTOK], sg[:, 0:NTOK], ALU.mult, r=["gl", "sg"], w=["gl"])
                    P.tt(actTv[:, j, 0:NTOK], gl[:, 0:NTOK], ln[:, 0:NTOK], ALU.mult, r=["gl", "ln"], w=["actT"])
            for dc in range(4):
                s, key = load_piece(wd_l[e][dc])
                wv = slot(s)[:, 0:KF * 512].rearrange("p (k c) -> p k c", c=512)
                for tt in range(nt):
                    ps = py[tt % 2]
                    pk = "py%d" % (tt % 2)
                    for kf in range(KF):
                        P.mm(ps[:], actTv[:, kf, tt * 128:(tt + 1) * 128], wv[:, kf, :], start=(kf == 0), stop=(kf == KF - 1),
                             r=["actT", key], w=[pk])
                    a = acc[:, tt * 2048 + dc * 512: tt * 2048 + (dc + 1) * 512]
                    P.stt(a, ps[:], G[:, t0 + tt, e:e + 1], a, ALU.mult, ALU.add, r=[pk, "G", "acc"], w=["acc"])
        for tt in range(nt):
            t = t0 + tt
            is_ctx = ctx_rows > 0 and t == NT - 1
            kind = 1 if is_ctx else 0
            rows = slice(t * 128, (t + 1) * 128)
            if tt == 0 or is_ctx:
                load_bc(bcA, modv[kind, 5, :], "bcA")
                if not last:
                    load_bc(bcB, modn[kind, 1, :], "bcB")
                    load_bc(bcC, gn, "bcC")
                    P.stt(bcB[:], bcB[:], 1.0, bcC[:], ALU.add, ALU.mult, r=["bcB", "bcC"], w=["bcB"])
                    load_bc(bcC, modn[kind, 0, :], "bcC")
            P.dma("sp", xt3[:], x1_s[rows, :], r=["x1_s"], w=["xt3"], key="xt3")
            a = acc[:, tt * 2048:(tt + 1) * 2048]
            P.tt(a, a, bcA[:], ALU.mult, r=["acc", "bcA"], w=["acc"])
            P.tt(xt3[:], xt3[:], a, ALU.add, r=["xt3", "acc"], w=["xt3"])
            P.dma("sp", xout[rows, :], xt3[:], r=["xt3"], key="xout")
            if not last:
                P.memset(sm[:, 5:6], 0.0, w=["sm5"])
                P.act(a, xt3[:], AF.Square, r=["xt3"], w=["acc", "sm5"], accum_out=sm[:, 5:6])
                P.ts(sm[:, 5:6], sm[:, 5:6], 1.0 / D, EPS, ALU.mult, ALU.add, r=["sm5"], w=["sm5"])
                P.act(sm[:, 5:6], sm[:, 5:6], AF.Sqrt, r=["sm5"], w=["sm5"])
                P.op("dve", lambda e: e.reciprocal(sm[:, 5:6], sm[:, 5:6]), r=["sm5"], w=["sm5"])
                P.stt(a, xt3[:], sm[:, 5:6], bcB[:], ALU.mult, ALU.mult, r=["xt3", "sm5", "bcB"], w=["acc"])
                P.tt(hn[:], a, bcC[:], ALU.add, r=["acc", "bcC"], w=["hn"])
                P.dma("sp", hnext[rows, :], hn[:], r=["hn"], key="hnext")
    P.out_dma.append("xout")
    if not last:
        P.out_dma.append("hnext")
    return P.finalize()


D = 2048
KD = 16


import os
GSTAGE = int(os.environ.get('GSTAGE', '99'))
GSUB = int(os.environ.get('GSUB', '9'))


def build_gla(NLT, NCT):
    P = Prog()
    TB = (NLT + NCT) * 128
    NC = 1568
    h = P.din("h", [TB, D], BF16)
    wc = P.din("wc", [D, NC], F32)
    w2 = P.din("w2", [2, 17, 256], F32)
    gnorm = P.din("gnorm", [512], F32)
    identb_d = P.din("identb", [128, 128], BF16)
    U_d = P.din("U", [2, 128, 128], F32)
    mixp = P.dout("mixp", [TB, 512], BF16)
    o_s = P.dtmp("o_s", [TB, 512], F32)

    W = P.sb("W", [128, KD, NC], BF16)
    identb = P.sb("identb_s", [128, 128], BF16)
    U = P.sb("U_s", [128, 2, 128], F32)
    w2s = P.sb("w2s", [17, 2, 256], F32)
    gbc = P.sb("gbc", [128, 512], F32)
    ones = P.sb("ones", [128, 128], F32)
    ht = P.sb("ht", [128, D], BF16)
    hT = P.sb("hT", [128, KD, 128], BF16)
    qT = P.sb("qT", [128, 2, 128], F32)
    kT = P.sb("kT", [128, 2, 128], F32)
    vb = P.sb("vb", [128, 512], BF16)
    r17 = P.sb("r17", [17, 128], F32)
    gk = P.sb("gk", [128, 256], F32)
    tmpa = P.sb("tmpa", [128, 256], F32)
    tmpb = P.sb("tmpb", [128, 256], F32)
    bTs = P.sb("bTs", [128, 2, 128], F32)
    ee = P.sb("ee", [128, 3, 2, 128], F32)
    qs = P.sb("qs", [128, 2, 128], BF16)
    ks = P.sb("ks", [128, 2, 128], BF16)
    qd = P.sb("qd", [128, 2, 128], BF16)
    attnT = P.sb("attnT", [128, 128], BF16)
    kd = P.sb("kd", [128, 256], BF16)
    S = P.sb("S", [128, 2, 512], F32)
    Sb = P.sb("Sb", [128, 2, 512], BF16)
    ot = P.sb("ot", [128, 512], F32)
    ot2 = P.sb("ot2", [128, 512], F32)
    go = P.sb("go", [128, 512], F32)
    yb = P.sb("yb", [128, 512], BF16)
    sm = P.sb("sm", [128, 8], F32)
    ptb = [P.ps("ptb%d" % i, [128, 1024], BF16) for i in range(2)]
    p_qk = P.ps("p_qk", [128, 512], F32)
    p_v = P.ps("p_v", [128, 512], F32)
    p_kr = P.ps("p_kr", [128, 512], F32)
    p_b = P.ps("p_b", [128, 512], F32)
    p_bT = P.ps("p_bT", [128, 512], F32)
    p_o = P.ps("p_o", [128, 512], F32)

    for k in range(KD):
        P.dma("pool", W[:, k, :], wc[k * 128:(k + 1) * 128, :], w=["W"], key="W")
    P.dma("sp", identb[:], identb_d, w=["identb"], key="identb")
    P.dma("sp", U[:], U_d.rearrange("d m c -> m d c"), w=["U"], key="U")
    P.dma("sp", w2s[:], w2.rearrange("d r c -> r d c"), w=["w2s"], key="w2s")
    P.dma("sp", gbc[:], gnorm.partition_broadcast(128), w=["gbc"], key="gbc")
    P.memset(ones[:], 1.0, w=["ones"])
    P.memset(r17[:], 1.0, w=["r17"])

    for d in range(2):
        P.memset(S[:], 0.0, w=["S"])
        P.memset(Sb[:], 0.0, w=["Sb"])
        last = 127 if d == 0 else 0
        order = [("c", i) for i in range(NCT)] + [("l", i) for i in range(NLT)]
        if d == 1:
            order = [("c", i) for i in reversed(range(NCT))] + [("l", i) for i in reversed(range(NLT))]
        for kind, i in order:
            r0 = (i if kind == "l" else NLT + i) * 128
            rows = slice(r0, r0 + 128)
            need_o = kind == "l"
            if GSTAGE < -3:
                continue
            P.dma("sp", ht[:], h[rows, :], w=["ht"], key="ht")
            for j in range(2):
                for q in range(8):
                    k = j * 8 + q
                    P.tr(ptb[j][:, q * 128:(q + 1) * 128], ht[:, k * 128:(k + 1) * 128], identb[:],
                         r=["ht", "identb"], w=["ptb%d" % j])
                P.cp(hT[:, j * 8:(j + 1) * 8, :], ptb[j][:].rearrange("p (k t) -> p k t", t=128), r=["ptb%d" % j],
                     w=["hT"], stream="act" if j else "dve")
            if GSUB < 1:
                continue
            for f in range(4):
                for k in range(KD):
                    P.mm(p_qk[:, f * 128:(f + 1) * 128], W[:, k, f * 128:(f + 1) * 128], hT[:, k, :],
                         start=(k == 0), stop=(k == KD - 1), r=["W", "hT"], w=["p_qk"])
            if GSUB < 2:
                continue
            P.ts(qT[:], p_qk[:, 0:256].rearrange("p (f t) -> p f t", t=128), 1.0 / 16, None, ALU.mult, r=["p_qk"], w=["qT"])
            P.cp(kT[:], p_qk[:, 256:512].rearrange("p (f t) -> p f t", t=128), r=["p_qk"], w=["kT"])
            if GSTAGE < -2:
                continue
            for k in range(KD):
                P.mm(p_v[:], hT[:, k, :], W[:, k, 512:1024], start=(k == 0), stop=(k == KD - 1), r=["W", "hT"], w=["p_v"])
            P.cp(vb[:], p_v[:], r=["p_v"], w=["vb"], stream="act")
            for k in range(KD):
                P.mm(p_kr[:, 0:256], hT[:, k, :], W[:, k, 256:512], start=(k == 0), stop=(k == KD - 1),
                     r=["W", "hT"], w=["p_kr"])
            if GSTAGE < -1:
                continue
            for k in range(KD):
                P.mm(p_bT[0:16, 384:512], W[:, k, 1536 + 16 * d:1552 + 16 * d], hT[:, k, :], start=(k == 0),
                     stop=(k == KD - 1), r=["W", "hT"], w=["p_rT"])
            P.cp(r17[0:16, :], p_bT[0:16, 384:512], r=["p_rT"], w=["r17"])
            P.mm(p_kr[:, 256:512], r17[:, :], w2s[:, d, :], r=["r17", "w2s"], w=["p_gk"])
            P.act(tmpa[:], p_kr[:, 256:512], AF.Exp, r=["p_gk"], w=["tmpa"], scale=-1.0)
            P.act(tmpa[:], tmpa[:], AF.Ln, r=["tmpa"], w=["tmpa"], bias=1.0)
            P.ts(gk[:], tmpa[:], -1.0 / 16, None, ALU.mult, r=["tmpa"], w=["gk"])
            if GSTAGE < 1:
                continue
            P.mm(p_b[:, 0:256], U[:, d, :], gk[:], r=["U", "gk"], w=["p_btok"])
            P.mm(p_b[:, 256:512], ones[:], gk[:], r=["ones", "gk"], w=["p_blast"])
            for f in range(2):
                P.mm(p_bT[:, f * 128:(f + 1) * 128], gk[:, f * 128:(f + 1) * 128], U[:, d, :], r=["gk", "U"], w=["p_bT"])
            P.cp(bTs[:], p_bT[:, 0:256].rearrange("p (f t) -> p f t", t=128), r=["p_bT"], w=["bTs"])
            P.ts(sm[:, 0:2], bTs[:, :, 64], -1.0, None, ALU.mult, r=["bTs"], w=["sm01"])
            for f in range(2):
                P.act(ee[:, 0, f, :], bTs[:, f, :], AF.Exp, r=["bTs", "sm01"], w=["ee"], bias=sm[:, f:f + 1], scale=1.0)
                P.act(ee[:, 1, f, :], bTs[:, f, :], AF.Exp, r=["bTs"], w=["ee"], bias=bTs[:, f, 64:65], scale=-1.0)
                P.act(ee[:, 2, f, :], bTs[:, f, :], AF.Exp, r=["bTs"], w=["ee"])
                P.act(sm[:, 2 + f:3 + f], bTs[:, f, last:last + 1], AF.Exp, r=["bTs"], w=["sm23"])
            P.tt(qs[:], qT[:], ee[:, 0], ALU.mult, r=["qT", "ee"], w=["qs"])
            P.tt(ks[:], kT[:], ee[:, 1], ALU.mult, r=["kT", "ee"], w=["ks"])
            P.tt(qd[:], qT[:], ee[:, 2], ALU.mult, r=["qT", "ee"], w=["qd"])
            if GSTAGE < 2:
                continue
            for f in range(2):
                P.mm(p_bT[:, 256:384], ks[:, f, :], qs[:, f, :], start=(f == 0), stop=(f == 1), r=["ks", "qs"], w=["p_at"])
            P.tt(attnT[:], p_bT[:, 256:384], U[:, d, :], ALU.mult, r=["p_at", "U"], w=["attnT"])
            P.cp(tmpb[:], p_b[:, 256:512], r=["p_blast"], w=["tmpb"], stream="act")
            P.tt(tmpb[:], tmpb[:], p_b[:, 0:256], ALU.subtract, r=["tmpb", "p_btok"], w=["tmpb"])
            P.act(tmpb[:], tmpb[:], AF.Exp, r=["tmpb"], w=["tmpb"])
            P.tt(kd[:], p_kr[:, 0:256], tmpb[:], ALU.mult, r=["p_kr", "tmpb"], w=["kd"])
            if GSTAGE < 3:
                continue
            if need_o:
                P.mm(p_o[:], attnT[:], vb[:], start=True, stop=False, r=["attnT", "vb"], w=["p_o"])
                for f in range(2):
                    P.mm(p_o[:], qd[:, f, :], Sb[:, f, :], start=False, stop=(f == 1), r=["qd", "Sb"], w=["p_o"])
                if d == 0:
                    P.cp(ot[:], p_o[:], r=["p_o"], w=["ot"])
                    P.dma("sp", o_s[rows, :], ot[:], r=["ot"], w=["o_s"], key="ot_st")
                else:
                    P.dma("sp", ot2[:], o_s[rows, :], r=["o_s"], w=["ot2"], key="ot2")
                    P.tt(ot[:], p_o[:], ot2[:], ALU.add, r=["p_o", "ot2"], w=["ot"])
                    for k in range(KD):
                        P.mm(p_v[:], hT[:, k, :], W[:, k, 1024:1536], start=(k == 0), stop=(k == KD - 1),
                             r=["W", "hT"], w=["p_v"])
                    P.act(go[:], p_v[:], AF.Silu, r=["p_v"], w=["go"])
                    P.memset(sm[:, 4:5], 0.0, w=["sm4"])
                    P.act(ot2[:], ot[:], AF.Square, r=["ot"], w=["ot2", "sm4"], accum_out=sm[:, 4:5])
                    P.ts(sm[:, 4:5], sm[:, 4:5], 1.0 / 512, 1e-6, ALU.mult, ALU.add, r=["sm4"], w=["sm4"])
                    P.act(sm[:, 4:5], sm[:, 4:5], AF.Sqrt, r=["sm4"], w=["sm4"])
                    P.op("dve", lambda e: e.reciprocal(sm[:, 4:5], sm[:, 4:5]), r=["sm4"], w=["sm4"])
                    P.stt(ot[:], ot[:], sm[:, 4:5], gbc[:], ALU.mult, ALU.mult, r=["ot", "sm4", "gbc"], w=["ot"])
                    P.tt(yb[:], ot[:], go[:], ALU.mult, r=["ot", "go"], w=["yb"])
                    P.dma("sp", mixp[rows, :], yb[:], r=["yb"], key="mixp")
            if GSTAGE < 4:
                continue
            for f in range(2):
                P.mm(p_o[:], kd[:, f * 128:(f + 1) * 128], vb[:], r=["kd", "vb"], w=["p_o"])
                P.stt(S[:, f, :], S[:, f, :], sm[:, 2 + f:3 + f], p_o[:], ALU.mult, ALU.add, r=["S", "sm23", "p_o"], w=["S"])
                P.cp(Sb[:, f, :], S[:, f, :], r=["S"], w=["Sb"], stream="act")
    P.memset(yb[:], 0.0, w=["yb"])
    for i in range(NCT):
        r0 = (NLT + i) * 128
        P.dma("sp", mixp[r0:r0 + 128, :], yb[:], r=["yb"], key="mixp")
    P.out_dma.append("mixp")
    return P.finalize()


D = 2048
KD = 16


import os
STAGE = int(os.environ.get('STAGE', '9'))
VAR = os.environ.get('VAR', '')
SUB = float(os.environ.get('SUB', '9'))


def build_mix0(NLT, NCT):
    P = Prog()
    P.keymap = {"B0a": "B0", "B0x": "B0", "B1z": "B1", "B2b": "B2", "B2c": "B2", "B2d": "B2", "B3a": "B3", "B3b": "B3",
                "B3c": "B3", "B4c": "B4", "B4d": "B4", "B5a": "B5", "B5b": "B5", "B5c": "B5", "B5d": "B5"}
    HKEYS = ["gbc", "gct", "egs0", "egs1", "egs2", "egs3", "t1", "t2", "Dcm", "DTs", "DTi", "kb", "kbT", "Mb0", "Mb1", "MTb0", "MTb1",
             "attnT", "R", "wT", "vn", "o2", "kdk", "Sst", "od", "B2", "B2b", "B2c", "B2d", "B3a", "B3b", "B3c", "B4", "B4c", "B4d",
             "B5a", "B5b", "B5c", "B5d"]
    P.ksuf_keys = set(HKEYS)
    for kk_ in ("B2", "B2b", "B2c", "B2d"):
        P.keymap[kk_ + "_0"] = "B2"; P.keymap[kk_ + "_1"] = "B0"
    for kk_ in ("B3a", "B3b", "B3c"):
        P.keymap[kk_ + "_0"] = "B3"; P.keymap[kk_ + "_1"] = "B1"
    for kk_ in ("B4", "B4c", "B4d"):
        P.keymap[kk_ + "_0"] = "B4"; P.keymap[kk_ + "_1"] = "ptb0"
    for kk_ in ("B5a", "B5b", "B5c", "B5d"):
        P.keymap[kk_ + "_0"] = "B5"; P.keymap[kk_ + "_1"] = "ptb1"
    TB = (NLT + NCT) * 128
    S = NLT * 128
    CT = NCT * 128
    TBP = S + 4 + CT + 4
    NC = 1416
    NTT = NLT + NCT
    h = P.din("h", [TB, D], BF16)
    wc = P.din("wc", [D, NC], F32)
    qkg = P.din("qkg", [128, 320], F32)
    sinks = P.din("sinks", [128, 4], F32)
    cw = P.din("cw", [128, 30], F32)
    gpar = P.din("gpar", [128, 8], F32)
    dnn = P.din("dnn", [128, 128], F32)
    M_d = P.din("M", [128, 512 + NLT * 64], F32)
    identb_d = P.din("identb", [128, 128], BF16)
    identf_d = P.din("identf", [128, 128], F32)
    mixp = P.dout("mixp", [TB, 512], BF16)
    xs = P.dtmp("xs", [6, 128, TBP], F32)
    zs = P.dtmp("zs", [TB, 256], F32)
    gs = P.dtmp("gs", [TB, 8], F32)
    o_s = P.dtmp("o_s", [TB, 256], F32)

    W = P.sb("W", [128, KD, NC], BF16)
    identb = P.sb("identb_s", [128, 128], BF16)
    identf = P.sb("identf_s", [128, 128], F32)
    Mm = P.sb("Mm", [128, 4, 128], F32)
    gq = P.sb("gq", [128, 320], F32)
    esink = P.sb("esink", [128, 4], F32)
    cws = P.sb("cws", [128, 6, 5], F32)
    gp = P.sb("gp", [128, 8], F32)
    dnbc = P.sb("dnbc", [128, 128], F32)
    ones = P.sb("ones", [128, 128], F32)
    zero6 = P.sb("zero6", [128, 6, 2], F32)
    ht = P.sb("ht", [128, D], BF16)
    hT = P.sb("hT", [128, KD, 128], BF16)
    qkv = P.sb("qkv", [128, 384], F32)
    sq = P.sb("sq", [128, 320], F32)
    ss = P.sb("ss", [128, 8], F32)
    qkn = P.sb("qkn", [128, 320], F32)
    qkr = P.sb("qkr", [128, 320], F32)
    rt = P.sb("ropetmp", [128, 64], F32)
    rpa = P.sb("rpa", [128, NLT, 64], F32)
    qrb = P.sb("qrb", [128, 320], BF16)
    qTr = [P.sb("qTr%d" % i, [64, 512], BF16) for i in range(2)]
    kTa = P.sb("kTa", [64, NTT * 128], BF16)
    V1 = P.sb("V1", [128, NTT, 66], BF16)
    xst = P.sb("xst", [128, 6, 128], F32)
    zt = P.sb("zt", [128, 256], F32)
    gts = P.sb("gts", [128, 8], F32)
    gtm = P.sb("gtm", [128, 4], F32)
    eT = P.sb("eT", [128, 5, 512], BF16)
    den = P.sb("den", [128, 4], F32)
    aout = P.sb("aout", [128, 256], BF16)
    xw = P.sb("xw", [128, 6, 132], F32)
    yc = P.sb("yc", [128, 6, 128], F32)
    sq4 = P.sb("sq4", [128, 4, 128], F32)
    rs4 = P.sb("rs4", [128, 4, 128], F32)
    qk = P.sb("qk", [128, 4, 128], F32)
    ktv = P.sb("ktv", [128, 4, 128], F32)
    gt8 = P.sb("gt8", [128, 8], F32)
    gbc = P.sb("gbc", [128, 128], F32)
    gct = P.sb("gct", [128, 1], F32)
    egs = P.sb("egs", [128, 4], F32)
    t1 = P.sb("t1", [128, 128], F32)
    t2 = P.sb("t2", [128, 128], F32)
    Dcm = P.sb("Dcm", [128, 128], F32)
    DTs = P.sb("DTs", [128, 128], F32)
    DTi = P.sb("DTi", [128, 128], F32)
    kb = P.sb("kb", [128, 128], F32)
    kbT = P.sb("kbT", [128, 128], F32)
    Mb = [P.sb("Mb%d" % i, [128, 128], F32) for i in range(2)]
    MTb = [P.sb("MTb%d" % i, [128, 128], F32) for i in range(2)]
    attnT = P.sb("attnT", [128, 128], F32)
    R = P.sb("R", [128, 256], F32)
    wT = P.sb("wT", [128, 128], F32)
    vn = P.sb("vn", [128, 128], F32)
    o2 = P.sb("o2", [128, 128], F32)
    od = P.sb("od", [128, 256], F32)
    oprev = P.sb("oprev", [128, 256], F32)
    kdk = P.sb("kdk", [128, 128], F32)
    Sst = P.sb("Sst", [128, 2, 128], F32)
    yb = P.sb("yb", [128, 256], BF16)

    HT_NAMES = ["gbc", "gct", "egs", "t1", "t2", "Dcm", "DTs", "DTi", "kb", "kbT", "Mb0", "Mb1", "MTb0", "MTb1", "attnT", "R",
                "wT", "vn", "o2", "kdk"]
    HT0 = dict(gbc=gbc, gct=gct, egs=egs, t1=t1, t2=t2, Dcm=Dcm, DTs=DTs, DTi=DTi, kb=kb, kbT=kbT, Mb0=Mb[0], Mb1=Mb[1],
               MTb0=MTb[0], MTb1=MTb[1], attnT=attnT, R=R, wT=wT, vn=vn, o2=o2, kdk=kdk)
    HT1 = {}
    for nm_ in HT_NAMES:
        shp_ = [128, 256] if nm_ == "R" else ([128, 1] if nm_ == "gct" else ([128, 4] if nm_ == "egs" else [128, 128]))
        HT1[nm_] = P.sb(nm_ + "_h1", shp_, F32)
    ptb = [P.ps("ptb%d" % i, [128, 1024], BF16) for i in range(2)]
    B0 = P.ps("B0", [128, 512], F32)
    B1 = P.ps("B1", [128, 512], F32)
    B2 = P.ps("B2", [128, 512], F32)
    B3 = P.ps("B3", [128, 512], F32)
    B4 = P.ps("B4", [128, 512], F32)
    B5 = P.ps("B5", [128, 512], F32)
    PB2, PB3, PB4, PB5 = B2, B3, B4, B5
    ptbf = [ptb[i][:].bitcast(F32) for i in range(2)]

    for k in range(KD):
        P.dma("pool", W[:, k, :], wc[k * 128:(k + 1) * 128, :], w=["W"], key="W")
    P.dma("sp", identb[:], identb_d, w=["identb"], key="identb")
    P.dma("sp", identf[:], identf_d, w=["identf"], key="identf")
    P.dma("sp", Mm[:].rearrange("p d c -> p (d c)"), M_d[:, 0:512], w=["Mm"], key="Mm")
    P.dma("sp", gq[:], qkg, w=["gq"], key="gq")
    P.dma("sp", esink[:], sinks, w=["esink"], key="esink")
    P.act(esink[:], esink[:], AF.Exp, r=["esink"], w=["esink"])
    P.dma("sp", cws[:].rearrange("p c j -> p (c j)"), cw, w=["cws"], key="cws")
    P.dma("sp", gp[:], gpar, w=["gp"], key="gp")
    P.act(gp[:, 0:4], gp[:, 0:4], AF.Exp, r=["gp"], w=["gp"])
    P.ts(gp[:, 0:4], gp[:, 0:4], -1.0, None, ALU.mult, r=["gp"], w=["gp"])
    P.dma("sp", dnbc[:], dnn, w=["dnbc"], key="dnbc")
    P.dma("sp", rpa[:].rearrange("p n c -> p (n c)"), M_d[:, 512:512 + NLT * 64], w=["rp"], key="rp")
    P.memset(ones[:], 1.0, w=["ones"])
    P.memset(zero6[:], 0.0, w=["zero6"])
    P.memset(V1[:], 1.0, w=["V1"])

    def tile_pos(kind, i):
        if kind == "l":
            return i * 128, 2 + i * 128, i
        return S + i * 128, S + 4 + 2 + i * 128, NLT + i

    def proj_tile(kind, i, qslot):
        if STAGE < 1:
            return
        r0, col0, gt = tile_pos(kind, i)
        rows = slice(r0, r0 + 128)
        P.dma("sp", ht[:], h[rows, :], r=["identb", "identf", "Mm", "gq", "esink", "cws", "gp", "dnbc", "rp"], w=["ht"], key="ht")
        for j in range(2):
            for q in range(8):
                k = j * 8 + q
                P.tr(ptb[j][:, q * 128:(q + 1) * 128], ht[:, k * 128:(k + 1) * 128], identb[:],
                     r=["ht", "identb"], w=["ptb%d" % j])
            P.cp(hT[:, j * 8:(j + 1) * 8, :], ptb[j][:].rearrange("p (k t) -> p k t", t=128), r=["ptb%d" % j],
                 w=["hT"], stream="act" if j else "dve")
        if SUB < 2:
            return
        for k in range(KD):
            P.mm(B0[:, 0:384], hT[:, k, :], W[:, k, 0:384], start=(k == 0), stop=(k == KD - 1), r=["W", "hT"], w=["B0a"])
        P.cp(qkv[:], B0[:, 0:384], r=["B0a"], w=["qkv"])
        P.tt(sq[:], qkv[:, 0:320], qkv[:, 0:320], ALU.mult, r=["qkv"], w=["sq"])
        P.op("dve", lambda e: e.reduce_sum(ss[:, 0:5], sq[:].rearrange("p (h f) -> p h f", f=64), AX.X), r=["sq"], w=["ss"])
        P.ts(ss[:, 0:5], ss[:, 0:5], 1.0 / 64, 1e-6, ALU.mult, ALU.add, r=["ss"], w=["ss"])
        P.act(ss[:, 0:5], ss[:, 0:5], AF.Sqrt, r=["ss"], w=["ss"])
        P.op("dve", lambda e: e.reciprocal(ss[:, 0:5], ss[:, 0:5]), r=["ss"], w=["ss"])
        for hh in range(5):
            cs = slice(hh * 64, (hh + 1) * 64)
            P.stt(qkn[:, cs], qkv[:, cs], ss[:, hh:hh + 1], gq[:, cs], ALU.mult, ALU.mult, r=["qkv", "ss", "gq"], w=["qkn"])
        if SUB < 3:
            return
        src = qkn
        if kind == "l":
            rp = rpa[:, i, :]
            if VAR == 'xdma':
                P.dma("sp", qkr[:, 0:64], M_d[0, :, 0:64], w=["qkr"], key="xd")
            for hh in range(0 if VAR == "noops" else 5):
                for a in range(2):
                    b0 = hh * 64 + a * 32
                    x1 = qkn[:, b0:b0 + 16]
                    x2 = qkn[:, b0 + 16:b0 + 32]
                    cv = rp[:, a * 16:(a + 1) * 16]
                    sv = rp[:, 32 + a * 16:32 + (a + 1) * 16]
                    P.tt(rt[:, 0:16], x1, cv, ALU.mult, r=["qkn", "rp"], w=["rt"])
                    P.tt(rt[:, 16:32], x2, sv, ALU.mult, r=["qkn", "rp"], w=["rt"])
                    P.tt(rt[:, 32:48], x2, cv, ALU.mult, r=["qkn", "rp"], w=["rt"])
                    P.tt(rt[:, 48:64], x1, sv, ALU.mult, r=["qkn", "rp"], w=["rt"])
                    P.tt(qkr[:, b0:b0 + 16], rt[:, 0:16], rt[:, 16:32], ALU.subtract, r=["rt"], w=["qkr"])
                    P.tt(qkr[:, b0 + 16:b0 + 32], rt[:, 32:48], rt[:, 48:64], ALU.add, r=["rt"], w=["qkr"])
            src = qkr
        if SUB < 4.1:
            return
        P.cp(qrb[:], src[:], r=["qkn", "qkr"], w=["qrb"], stream="act")
        for hh in range(5):
            P.tr(ptb[1][0:64, hh * 128:(hh + 1) * 128], qrb[:, hh * 64:(hh + 1) * 64], identb[:],
                 r=["qrb", "identb"], w=["ptb1"])
        if SUB < 4.2:
            return
        P.cp(qTr[qslot][:, :], ptb[1][0:64, 0:512], r=["ptb1"], w=["qT%d" % qslot])
        if SUB < 4.3:
            return
        P.cp(kTa[:, gt * 128:(gt + 1) * 128], ptb[1][0:64, 512:640], r=["ptb1"], w=["kTa"])
        if SUB < 4.4:
            return
        P.cp(V1[:, gt, 0:64], qkv[:, 320:384], r=["qkv"], w=["V1"])
        if SUB < 5:
            return
        for c in range(6):
            for k in range(KD):
                P.mm(B0[:, 384:512], W[:, k, 384 + c * 128:384 + (c + 1) * 128], hT[:, k, :], start=(k == 0),
                     stop=(k == KD - 1), r=["W", "hT"], w=["B0x"])
            P.cp(xst[:, c, :], B0[:, 384:512], r=["B0x"], w=["xst"], stream="act" if c % 2 else "dve")
        P.dma("sp", xs[:, :, col0:col0 + 128].rearrange("c p t -> p c t"), xst[:], r=["xst"], w=["xs"], key="xst_st")
        if SUB < 6:
            return
        for k in range(KD):
            P.mm(B1[:, 0:264], hT[:, k, :], W[:, k, 1152:1416], start=(k == 0), stop=(k == KD - 1), r=["W", "hT"], w=["B1z"])
        P.act(zt[:], B1[:, 0:256], AF.Silu, r=["B1z"], w=["zt"])
        P.dma("sp", zs[rows, :], zt[:], r=["zt"], w=["zs"], key="zt_st")
        P.tt(gtm[:], B1[:, 256:260], gp[:, 4:8], ALU.add, r=["B1z", "gp"], w=["gtm"])
        P.act(gtm[:], gtm[:], AF.Exp, r=["gtm"], w=["gtm"])
        P.act(gtm[:], gtm[:], AF.Ln, r=["gtm"], w=["gtm"], bias=1.0)
        P.tt(gts[:, 0:4], gtm[:], gp[:, 0:4], ALU.mult, r=["gtm", "gp"], w=["gts"])
        P.act(gts[:, 4:8], B1[:, 260:264], AF.Sigmoid, r=["B1z"], w=["gts"])
        P.dma("sp", gs[rows, :], gts[:], r=["gts"], w=["gs"], key="gts_st")

    def attn_block(kind, i, qslot):
        if STAGE < 2:
            return
        r0, col0, gt = tile_pos(kind, i)
        kbs = []
        if kind == "l":
            if i > 0:
                kbs.append((i - 1, 1))
            kbs.append((i, None))
            if i < NLT - 1:
                kbs.append((i + 1, 0))
        for c in range(NCT):
            kbs.append((NLT + c, None))
        for idx, (kg, mk) in enumerate(kbs):
            ps = B2 if idx % 2 == 0 else B3
            pk = "B2" if idx % 2 == 0 else "B3"
            P.mm(ps[:], kTa[:, kg * 128:(kg + 1) * 128], qTr[qslot][:, :], r=["kTa", "qT%d" % qslot], w=[pk])
            P.act(eT[:, idx, :], ps[:], AF.Exp, r=[pk], w=["eT"], scale=0.125)
            if mk is not None:
                for hh in range(4):
                    P.tt(eT[:, idx, hh * 128:(hh + 1) * 128], eT[:, idx, hh * 128:(hh + 1) * 128], Mm[:, mk, :], ALU.mult,
                         r=["eT", "Mm"], w=["eT"])
        for hh in range(4):
            for idx, (kg, mk) in enumerate(kbs):
                P.mm(B4[:, hh * 128:hh * 128 + 65], eT[:, idx, hh * 128:(hh + 1) * 128], V1[:, kg, 0:65], start=(idx == 0),
                     stop=(idx == len(kbs) - 1), r=["eT", "V1"], w=["B4"])
        pv = B4[:, :].rearrange("p (h f) -> p h f", f=128)
        P.tt(den[:], pv[:, :, 64], esink[:], ALU.add, r=["B4", "esink"], w=["den"])
        P.op("dve", lambda e: e.reciprocal(den[:], den[:]), r=["den"], w=["den"])
        for hh in range(4):
            P.ts(aout[:, hh * 64:(hh + 1) * 64], B4[:, hh * 128:hh * 128 + 64], den[:, hh:hh + 1], None, ALU.mult,
                 r=["B4", "den"], w=["aout"])
        P.dma("sp", mixp[r0:r0 + 128, 0:256], aout[:], r=["aout"], key="mixp")

    for c in range(NCT):
        proj_tile("c", c, c % 2)
    for c in range(NCT):
        attn_block("c", c, c % 2)
    for i in range(NLT):
        proj_tile("l", i, i % 2)
        if i >= 1:
            attn_block("l", i - 1, (i - 1) % 2)
    attn_block("l", NLT - 1, (NLT - 1) % 2)

    PSK = ["B0a", "B0x", "B1z", "B2", "B3", "B4", "B2b", "B2c", "B2d", "B3a", "B3b", "B3c", "B4c", "B4d",
           "B5a", "B5b", "B5c", "B5d"]
    P.memset(ss[:, 7:8], 0.0, w=PSK + ["ss7"])
    for d in range(2 if STAGE >= 3 else 0):
        P.memset(Sst[:], 0.0, w=["Sst_0", "Sst_1"])
        iMin = 0 if d == 0 else 1
        iMst = 2 if d == 0 else 3
        iLst = 3 if d == 0 else 2
        last = 127 if d == 0 else 0
        order = [("c", i) for i in range(NCT)] + [("l", i) for i in range(NLT)]
        if d == 1:
            order = [("c", i) for i in reversed(range(NCT))] + [("l", i) for i in reversed(range(NLT))]
        for kind, i in order:
            r0, col0, gt = tile_pos(kind, i)
            rows = slice(r0, r0 + 128)
            nlast = (NLT if kind == "l" else NCT) - 1
            lo = 2 if i == 0 else 0
            hi = 130 if i == nlast else 132
            if lo:
                P.memset(xw[:, :, 0:2], 0.0, w=["xw"])
            if hi < 132:
                P.memset(xw[:, :, 130:132], 0.0, w=["xw"])
            P.dma("sp", xw[:, :, lo:hi], xs[:, :, col0 - 2 + lo:col0 - 2 + hi].rearrange("c p t -> p c t"), r=["xs"], w=["xw"], key="xw")
            P.dma("sp", gt8[:], gs[rows, :], r=["gs"], w=["gt8"], key="gt8")
            for c in range(6):
                P.ts(yc[:, c, :], xw[:, c, 0:128], cws[:, c, 0:1], None, ALU.mult, r=["xw", "cws"], w=["yc"])
                for j in range(1, 5):
                    P.stt(yc[:, c, :], xw[:, c, j:j + 128], cws[:, c, j:j + 1], yc[:, c, :], ALU.mult, ALU.add,
                          r=["xw", "cws", "yc"], w=["yc"])
            P.act(yc[:], yc[:], AF.Silu, r=["yc"], w=["yc"])
            P.tt(sq4[:], yc[:, 0:4, :], yc[:, 0:4, :], ALU.mult, r=["yc"], w=["sq4"])
            P.mm(B0[:], ones[:], sq4[:].rearrange("p c t -> p (c t)"), r=["ones", "sq4"], w=["B0a", "B0x"])
            P.ts(rs4[:].rearrange("p c t -> p (c t)"), B0[:], 1e-6, None, ALU.add, r=["B0a"], w=["rs4"])
            P.act(rs4[:], rs4[:], AF.Sqrt, r=["rs4"], w=["rs4"])
            P.op("dve", lambda e: e.reciprocal(rs4[:], rs4[:]), r=["rs4"], w=["rs4"])
            P.ts(rs4[:, 0:2, :], rs4[:, 0:2, :], 128 ** -0.5, None, ALU.mult, r=["rs4"], w=["rs4"])
            P.tt(qk[:], yc[:, 0:4, :], rs4[:], ALU.mult, r=["yc", "rs4"], w=["qk"])
            for hh in range(2):
                P.tr(B1[:, hh * 128:(hh + 1) * 128], qk[:, 2 + hh, :], identf[:], r=["qk", "identf"], w=["B1z"])
                P.tr(B1[:, 256 + hh * 128:256 + (hh + 1) * 128], yc[:, 4 + hh, :], identf[:], r=["yc", "identf"], w=["B1z"])
            P.cp(ktv[:].rearrange("p c t -> p (c t)"), B1[:], r=["B1z"], w=["ktv"])
            segs = []
            for hh in range(2 if STAGE >= 4 else 0):
                seg_start = len(P.ops)
                P.ksuf = "_%d" % hh
                HB = HT0 if hh == 0 else HT1
                gbc, gct, egs, t1, t2, Dcm, DTs, DTi, kb, kbT = (HB[n_] for n_ in HT_NAMES[:10])
                Mb = [HB["Mb0"], HB["Mb1"]]
                MTb = [HB["MTb0"], HB["MTb1"]]
                attnT, R, wT, vn, o2, kdk = (HB[n_] for n_ in HT_NAMES[14:])
                if hh == 0:
                    B2, B3, B4, B5 = PB2, PB3, PB4, PB5
                else:
                    B2, B3, B4, B5 = B0, B1, ptbf[0], ptbf[1]
                g = gt8[:, 2 * d + hh:2 * d + hh + 1]
                beta = gt8[:, 4 + 2 * d + hh:4 + 2 * d + hh + 1]
                qT = qk[:, hh, :]
                kT = qk[:, 2 + hh, :]
                k_tok = ktv[:, hh, :]
                v_tok = ktv[:, 2 + hh, :]
                P.ts(gbc[:], ones[:], g, None, ALU.mult, r=["ones", "gt8"], w=["gbc"])
                P.mm(B2[:, 0:128], gbc[:], Mm[:, iMin, :], r=["gbc", "Mm"], w=["B2"])
                P.mm(B3[:, 256:384], Mm[:, iMin, :], gbc[:], r=["Mm", "gbc"], w=["B3c"])
                P.cp(gct[:], B3[:, 256:257], r=["B3c"], w=["gct"])
                P.act(egs[:, 0:1], gct[:, 0:1], AF.Exp, r=["gct"], w=["egs0"])
                P.cp(egs[:, 3:4], B2[:, last:last + 1], r=["B2"], w=["egs3"])
                P.act(egs[:, 1:2], egs[:, 3:4], AF.Exp, r=["egs3"], w=["egs1"])
                P.ts(egs[:, 2:3], B2[:, last:last + 1], gct[:, 0:1], None, ALU.subtract, r=["B2", "gct"], w=["egs2"])
                P.act(egs[:, 2:3], egs[:, 2:3], AF.Exp, r=["egs2"], w=["egs2"])
                P.ts(t1[:], B2[:, 0:128], gct[:, 0:1], 0.0, ALU.subtract, ALU.max, r=["B2", "gct"], w=["t1"])
                P.act(Dcm[:], t1[:], AF.Exp, r=["t1"], w=["Dcm"], scale=-1.0)
                P.tt(Dcm[:], Dcm[:], Mm[:, iLst, :], ALU.mult, r=["Dcm", "Mm"], w=["Dcm"])
                P.ts(t2[:], B2[:, 0:128], gct[:, 0:1], 0.0, ALU.subtract, ALU.min, r=["B2", "gct"], w=["t2"])
                P.act(t2[:], t2[:], AF.Exp, r=["t2"], w=["t2"])
                P.tt(DTs[:], t2[:], Mm[:, iMst, :], ALU.mult, r=["t2", "Mm"], w=["DTs"])
                P.tt(DTi[:], t2[:], Mm[:, iMin, :], ALU.mult, r=["t2", "Mm"], w=["DTi"])
                P.ts(kb[:], k_tok, beta, None, ALU.mult, r=["ktv", "gt8"], w=["kb"])
                P.tr(B3[:, 0:128], kb[:], identf[:], r=["kb", "identf"], w=["B3a"])
                P.cp(kbT[:], B3[:, 0:128], r=["B3a"], w=["kbT"])
                P.mm(B2[:, 128:256], kbT[:], kT, r=["kbT", "qk"], w=["B2b"])
                P.mm(B2[:, 256:384], kT, kbT[:], r=["kbT", "qk"], w=["B2c"])
                P.mm(B2[:, 384:512], kT, qT, r=["qk"], w=["B2d"])
                P.stt(Mb[0][:], B2[:, 128:256], -1.0, Dcm[:], ALU.mult, ALU.mult, r=["B2b", "Dcm"], w=["Mb0"])
                P.stt(MTb[0][:], B2[:, 256:384], -1.0, DTs[:], ALU.mult, ALU.mult, r=["B2c", "DTs"], w=["MTb0"])
                P.tt(attnT[:], B2[:, 384:512], DTi[:], ALU.mult, r=["B2d", "DTi"], w=["attnT"])
                P.ts(R[:, 0:128], v_tok, beta, None, ALU.mult, r=["ktv", "gt8"], w=["R"])
                P.ts(R[:, 128:256], kb[:], egs[:, 0:1], None, ALU.mult, r=["kb", "egs0"], w=["R"])
                cur = 0
                for it in range(7):
                    P.mm(B4[:, 0:256], MTb[cur][:], R[:], r=["MTb%d" % cur, "R"], w=["B4"])
                    if it < 6:
                        P.mm(B5[:, 0:128], MTb[cur][:], Mb[cur][:], r=["MTb%d" % cur, "Mb%d" % cur], w=["B5a"])
                        P.mm(B5[:, 128:256], Mb[cur][:], MTb[cur][:], r=["MTb%d" % cur, "Mb%d" % cur], w=["B5b"])
                    P.tt(R[:], B4[:, 0:256], R[:], ALU.add, r=["B4", "R"], w=["R"])
                    if it < 6:
                        nx = 1 - cur
                        P.cp(Mb[nx][:], B5[:, 0:128], r=["B5a"], w=["Mb%d" % nx])
                        P.cp(MTb[nx][:], B5[:, 128:256], r=["B5b"], w=["MTb%d" % nx])
                        cur = nx
                P.tr(B3[:, 128:256], R[:, 128:256], identf[:], r=["R", "identf"], w=["B3b"])
                P.cp(wT[:], B3[:, 128:256], r=["B3b"], w=["wT"])
                P.mm(B5[:, 256:384], wT[:], Sst[:, hh, :], r=["wT", "Sst"], w=["B5c"])
                P.tt(vn[:], R[:, 0:128], B5[:, 256:384], ALU.subtract, r=["R", "B5c"], w=["vn"])
                P.mm(B5[:, 384:512], qT, Sst[:, hh, :], r=["qk", "Sst"], w=["B5d"])
                P.mm(B4[:, 256:384], attnT[:], vn[:], r=["attnT", "vn"], w=["B4c"])
                P.cp(o2[:], B4[:, 256:384], r=["B4c"], w=["o2"])
                P.stt(od[:, hh * 128:(hh + 1) * 128], B5[:, 384:512], egs[:, 0:1], o2[:], ALU.mult, ALU.add,
                      r=["B5d", "egs0", "o2"], w=["od"])
                P.ts(kdk[:], k_tok, egs[:, 2:3], None, ALU.mult, r=["ktv", "egs2"], w=["kdk"])
                P.mm(B4[:, 384:512], kdk[:], vn[:], r=["kdk", "vn"], w=["B4d"])
                P.stt(Sst[:, hh, :], Sst[:, hh, :], egs[:, 1:2], B4[:, 384:512], ALU.mult, ALU.add,
                      r=["Sst", "egs1", "B4d"], w=["Sst"])
                segs.append(P.ops[seg_start:])
                del P.ops[seg_start:]
                P.ksuf = ""
            if segs:
                B2, B3, B4, B5 = PB2, PB3, PB4, PB5
                for i_ in range(max(len(s_) for s_ in segs)):
                    for s_ in segs:
                        if i_ < len(s_):
                            P.ops.append(s_[i_])
            if d == 0:
                P.dma("sp", o_s[rows, :], od[:], r=["od_0", "od_1"], w=["o_s"], key="od_st")
            else:
                P.dma("sp", oprev[:], o_s[rows, :], r=["o_s"], w=["oprev"], key="oprev")
                P.dma("sp", zt[:], zs[rows, :], r=["zs"], w=["zt"], key="zt_ld")
                P.tt(od[:], od[:], oprev[:], ALU.add, r=["od_0", "od_1", "oprev"], w=["od_0", "od_1", "odf"])
                P.tt(oprev[:], od[:], od[:], ALU.mult, r=["odf"], w=["oprev"])
                P.op("dve", lambda e: e.reduce_sum(ss[:, 5:7], oprev[:].rearrange("p (h f) -> p h f", f=128), AX.X),
                     r=["oprev"], w=["ss57"])
                P.ts(ss[:, 5:7], ss[:, 5:7], 1.0 / 128, 1e-6, ALU.mult, ALU.add, r=["ss57"], w=["ss57"])
                P.act(ss[:, 5:7], ss[:, 5:7], AF.Sqrt, r=["ss57"], w=["ss57"])
                P.op("dve", lambda e: e.reciprocal(ss[:, 5:7], ss[:, 5:7]), r=["ss57"], w=["ss57"])
                for hh in range(2):
                    cs = slice(hh * 128, (hh + 1) * 128)
                    P.stt(od[:, cs], od[:, cs], ss[:, 5 + hh:6 + hh], dnbc[:], ALU.mult, ALU.mult, r=["odf", "ss57", "dnbc"], w=["odf", "od_0", "od_1"])
                P.tt(yb[:], od[:], zt[:], ALU.mult, r=["odf", "zt"], w=["yb"])
                P.dma("sp", mixp[rows, 256:512], yb[:], r=["yb"], key="mixp")
    P.out_dma.append("mixp")
    return P.finalize()


D = 2048


def _run(nc, in_maps):
    return run_bass_kernel_spmd(nc, in_maps, core_ids=list(range(8))).results


def _bc(v):
    v = np.asarray(v, np.float32).reshape(1, -1)
    return np.ascontiguousarray(np.broadcast_to(v, (128, v.shape[1])))


def _rope_table(S):
    GRID_W = 64
    rows = S // GRID_W
    row = np.repeat(np.arange(rows, dtype=np.float32), GRID_W)
    col = np.tile(np.arange(GRID_W, dtype=np.float32), rows)
    inv = (np.float32(10000.0) ** (-np.arange(16, dtype=np.float32) / np.float32(16))).astype(np.float32)
    ang = np.stack([row[:, None] * inv, col[:, None] * inv], axis=1).astype(np.float32)
    return np.concatenate([np.cos(ang).reshape(S, 32), np.sin(ang).reshape(S, 32)], axis=1).astype(np.float32)


def kernel(x, c, ctx, c_ctx, w_mod, b_mod, norm_g, e_w_in, e_w_out, e_q_gain, e_k_gain, e_sinks, e_conv_w,
           e_a_log, e_dt_bias, e_dn_norm, o_w_in, o_gate_w2, o_gate_b, o_gla_norm, o_w_out,
           w_router, b_router, w_gu, b_gu, w_down, b_down):
    f32 = lambda a: np.ascontiguousarray(np.asarray(a, dtype=np.float32))
    x, c, ctx, c_ctx, w_mod, b_mod, norm_g = map(f32, (x, c, ctx, c_ctx, w_mod, b_mod, norm_g))
    B, S, _ = x.shape
    CTXL = ctx.shape[1]
    DFF = w_gu.shape[-1] // 2
    NLc = S // 4 // 128
    CR = CTXL // 4
    NLT, NCT = S // 128, CTXL // 128
    TB = S + CTXL
    identb = np.eye(128).astype(NPBF)
    identf = np.eye(128, dtype=np.float32)
    o = np.ones((128, 128), np.float32)
    M4 = np.stack([np.triu(o), np.tril(o), np.triu(o, 1), np.tril(o, -1)])
    cv = np.stack([c[0], c[1], c_ctx])
    cT = np.ascontiguousarray(cv.reshape(3, 16, 128).transpose(2, 1, 0).reshape(128, 48))
    ims = []
    for cc in range(8):
        cs = slice(cc * 1536, (cc + 1) * 1536)
        ims.append(dict(cT=cT, wm=np.ascontiguousarray(w_mod[:, :, cs]), bm=np.ascontiguousarray(b_mod[:, cs]).reshape(1, 2 * 1536)))
    r = _run(build_mod(), ims)
    mod = np.concatenate([r[cc]["modo"] for cc in range(8)], axis=2).reshape(2, 3, 6, D)

    def tok_rows(cc):
        b, q = cc // 4, cc % 4
        return b, slice(q * (S // 4), (q + 1) * (S // 4)), slice(q * CR, (q + 1) * CR)

    NT0 = NLc + 1
    xins, ims = [], []
    for cc in range(8):
        b, ls, cs = tok_rows(cc)
        xin = np.zeros((NT0 * 128, D), np.float32)
        xin[:NLc * 128] = x[b, ls]
        xin[NLc * 128:NLc * 128 + CR] = ctx[b, cs]
        xins.append(xin)
        modn = np.ascontiguousarray(np.stack([mod[0, b, 0:2], mod[0, 2, 0:2]]))
        ims.append(dict(xin=xin, g=norm_g[0, 0], modn=modn))
    r = _run(build_pre(NT0, CR), ims)

    def gather_h(res, key):
        hb = np.zeros((B, TB, D), NPBF)
        for cc in range(8):
            b, ls, cs = tok_rows(cc)
            hb[b, ls] = res[cc][key][:NLc * 128]
            hb[b, S + cs.start:S + cs.stop] = res[cc][key][NLc * 128:NLc * 128 + CR]
        return hb

    hb = gather_h(r, "hout")
    W0 = f32(e_w_in[0])
    cwT = f32(e_conv_w[0])
    rope = _rope_table(S)
    Mc = np.ascontiguousarray(np.concatenate([M4.transpose(1, 0, 2).reshape(128, 512),
                                              rope.reshape(NLT, 128, 64).transpose(1, 0, 2).reshape(128, NLT * 64)], axis=1))
    ims = []
    for cc in range(8):
        b, j = cc // 4, cc % 4
        base = 1536
        cols = np.concatenate([np.arange(j * 256, (j + 1) * 256), 1024 + np.arange(j * 64, (j + 1) * 64),
                               1280 + np.arange(j * 64, (j + 1) * 64),
                               base + np.arange(2 * j * 128, (2 * j + 2) * 128),
                               base + 1024 + np.arange(2 * j * 128, (2 * j + 2) * 128),
                               base + 2048 + np.arange(2 * j * 128, (2 * j + 2) * 128),
                               base + 3072 + np.arange(2 * j * 128, (2 * j + 2) * 128),
                               5632 + np.arange(2 * j, 2 * j + 2), 5640 + np.arange(2 * j, 2 * j + 2),
                               5648 + np.arange(2 * j, 2 * j + 2), 5656 + np.arange(2 * j, 2 * j + 2)])
        ccols = np.concatenate([np.arange(2 * j * 128, (2 * j + 2) * 128), 1024 + np.arange(2 * j * 128, (2 * j + 2) * 128),
                                2048 + np.arange(2 * j * 128, (2 * j + 2) * 128)])
        cw = np.ascontiguousarray(cwT[:, ccols].T.reshape(6, 128, 5).transpose(1, 0, 2).reshape(128, 30))
        gpar = np.concatenate([np.asarray(e_a_log, np.float32)[0, 0, 2 * j:2 * j + 2], np.asarray(e_a_log, np.float32)[0, 1, 2 * j:2 * j + 2],
                               np.asarray(e_dt_bias, np.float32)[0, 0, 2 * j:2 * j + 2], np.asarray(e_dt_bias, np.float32)[0, 1, 2 * j:2 * j + 2]])
        ims.append(dict(h=np.ascontiguousarray(hb[b]), wc=np.ascontiguousarray(W0[:, cols]),
                        qkg=_bc(np.concatenate([np.asarray(e_q_gain, np.float32)[0]] * 4 + [np.asarray(e_k_gain, np.float32)[0]])),
                        sinks=_bc(np.asarray(e_sinks, np.float32)[0, 4 * j:4 * j + 4]), cw=cw, gpar=_bc(gpar),
                        dnn=_bc(np.asarray(e_dn_norm, np.float32)[0]), M=Mc, identb=identb, identf=identf))
    r = _run(build_mix0(NLT, NCT), ims)
    mixf = np.zeros((B, TB, D), NPBF)
    for cc in range(8):
        b, j = cc // 4, cc % 4
        mixf[b, :, j * 256:(j + 1) * 256] = r[cc]["mixp"][:, 0:256]
        mixf[b, :, 1024 + j * 256:1024 + (j + 1) * 256] = r[cc]["mixp"][:, 256:512]

    def post(layer, last, xin_list, mixfull, wout):
        NT = NLc if last else NLc + 1
        wr = f32(w_router[layer]); br = f32(b_router[layer]).reshape(1, 32)
        wgu = f32(w_gu[layer]); bgu = f32(b_gu[layer]); wdn = f32(w_down[layer]); bdn = f32(b_down[layer])
        wo = f32(wout)
        ims = []
        for cc in range(8):
            b, ls, cs = tok_rows(cc)
            mix = np.zeros((NT * 128, D), NPBF)
            mix[:NLc * 128] = mixfull[b, ls]
            if not last:
                mix[NLc * 128:NLc * 128 + CR] = mixfull[b, S + cs.start:S + cs.stop]
            modv = np.ascontiguousarray(np.stack([mod[layer, b], mod[layer, 2]]))
            im = dict(xin=xin_list[cc], mix=mix, modv=modv, g2=norm_g[layer, 1], wout=wo, wr=wr, br=br, wgu=wgu, bgu=bgu,
                      wdn=wdn, bdn=bdn, identb=identb, identf=identf)
            if not last:
                im["gn"] = norm_g[layer + 1, 0]
                im["modn"] = np.ascontiguousarray(np.stack([mod[layer + 1, b, 0:2], mod[layer + 1, 2, 0:2]]))
            ims.append(im)
        return _run(build_post(NT, DFF, last, 0 if last else CR), ims)

    r = post(0, False, xins, mixf, e_w_out[0])
    x1 = [np.ascontiguousarray(r[cc]["xout"][:NLc * 128]) for cc in range(8)]
    hb = gather_h(r, "hnext")
    W1 = f32(o_w_in[0]); gw2 = f32(o_gate_w2[0]); gb = f32(o_gate_b[0])
    U2 = np.ascontiguousarray(M4[0:2])
    ims = []
    for cc in range(8):
        b, j = cc // 4, cc % 4
        cols = np.concatenate([np.arange(j * 256, (j + 1) * 256), 1024 + np.arange(j * 256, (j + 1) * 256),
                               2048 + np.arange(j * 512, (j + 1) * 512), 4096 + np.arange(j * 512, (j + 1) * 512),
                               np.arange(6144, 6176)])
        w2 = np.ascontiguousarray(np.concatenate([gw2[:, :, j * 256:(j + 1) * 256], gb[:, None, j * 256:(j + 1) * 256]], axis=1))
        ims.append(dict(h=np.ascontiguousarray(hb[b]), wc=np.ascontiguousarray(W1[:, cols]), w2=w2,
                        gnorm=f32(o_gla_norm[0]), identb=identb, U=U2))
    r = _run(build_gla(NLT, NCT), ims)
    mixf = np.zeros((B, TB, D), NPBF)
    for cc in range(8):
        b, j = cc // 4, cc % 4
        mixf[b, :, j * 512:(j + 1) * 512] = r[cc]["mixp"]
    r = post(1, True, x1, mixf, o_w_out[0])
    out = np.zeros((B, S, D), np.float32)
    for cc in range(8):
        b, ls, cs = tok_rows(cc)
        out[b, ls] = r[cc]["xout"][:NLc * 128]
    return out
```
